# Optimizing a Trainium2 kernel written in Bass

```python
import jax
import jax.numpy as jnp
from jax import lax
import numpy as np

D_MODEL = 4096
BATCH = 2
SEQ = 4096
DEPTH = 2

Q_BLOCK = 128
NEG_INF = -1e30
NORM_EPS = 1e-6

D_MIX = D_MODEL
GROUP_WIDTH = D_MIX // 4

NSA_HEADS = 8
NSA_HEAD_DIM = GROUP_WIDTH // NSA_HEADS
NSA_KV_HEADS = 2
NSA_CMP_STRIDE = 16
NSA_CMP_LEN = 2 * NSA_CMP_STRIDE
NSA_SEL_BLOCK = 64
NSA_TOP_N = 16
NSA_WINDOW = 512

FOX_HEADS = 8
FOX_HEAD_DIM = GROUP_WIDTH // FOX_HEADS

MLA_HEADS = 8
MLA_Q_RANK = 768
MLA_KV_RANK = 512
MLA_NOPE_DIM = 128
MLA_ROPE_DIM = 64
MLA_V_DIM = GROUP_WIDTH // MLA_HEADS
ROPE_THETA = 10000.0

SWA_HEADS = 16
SWA_KV_HEADS = 2
SWA_HEAD_DIM = GROUP_WIDTH // SWA_HEADS
SWA_WINDOW = 128

N_GROUPS = 8
EXPERTS_PER_GROUP = 8
N_EXPERTS = N_GROUPS * EXPERTS_PER_GROUP
TOP_K = 2
D_EXPERT = 384
MOE_BLOCK = 128

IN_SPLITS = (
    NSA_HEADS * NSA_HEAD_DIM,
    3 * 2 * NSA_KV_HEADS * NSA_HEAD_DIM,
    3 * NSA_HEADS,
    3 * FOX_HEADS * FOX_HEAD_DIM,
    FOX_HEADS,
    MLA_Q_RANK,
    MLA_KV_RANK,
    MLA_ROPE_DIM,
    SWA_HEADS * SWA_HEAD_DIM,
    2 * SWA_KV_HEADS * SWA_HEAD_DIM,
)
IN_COLS = sum(IN_SPLITS)
OUT_SPLITS = (NSA_HEADS * NSA_HEAD_DIM, FOX_HEADS * FOX_HEAD_DIM, MLA_HEADS * MLA_V_DIM, SWA_HEADS * SWA_HEAD_DIM)

kernel_name = 'hybrid_nsa_fox_mla_swa_hmoe'


def split_cols(x, sizes):
    out, off = [], 0
    for s in sizes:
        out.append(x[..., off:off + s])
        off += s
    return out


def rms_norm(x, g):
    xf = x.astype(jnp.float32)
    y = xf * lax.rsqrt(jnp.mean(xf * xf, axis=-1, keepdims=True) + NORM_EPS)
    return (y * g.astype(jnp.float32)).astype(x.dtype)


def alibi_slopes(n_heads):
    return jnp.exp2(-8.0 * jnp.arange(1, n_heads + 1, dtype=jnp.float32) / n_heads)


def rope_tables(seq):
    pos = jnp.arange(seq, dtype=jnp.float32)
    inv = ROPE_THETA ** (-jnp.arange(0, MLA_ROPE_DIM, 2, dtype=jnp.float32) / MLA_ROPE_DIM)
    ang = pos[:, None] * inv[None, :]
    return jnp.cos(ang), jnp.sin(ang)


def apply_rope(x, cos, sin):
    xf = x.astype(jnp.float32)
    x1, x2 = jnp.split(xf, 2, axis=-1)
    return jnp.concatenate([x1 * cos - x2 * sin, x2 * cos + x1 * sin], axis=-1).astype(x.dtype)


def causal_attention_blocks(q, k, v, cum_log_f=None):
    bsz, seq, nh, dk = q.shape
    nb = seq // Q_BLOCK
    scale = dk ** -0.5
    k_pos = jnp.arange(seq)
    qb = q.reshape(bsz, nb, Q_BLOCK, nh, dk).swapaxes(0, 1)
    xs = (qb, jnp.arange(nb))
    if cum_log_f is not None:
        c_keys = cum_log_f.transpose(0, 2, 1)
        xs = xs + (cum_log_f.reshape(bsz, nb, Q_BLOCK, nh).swapaxes(0, 1),)

    def body(args):
        qi, blk = args[0], args[1]
        s = jnp.einsum('bqhd,bkhd->bhqk', qi, k).astype(jnp.float32) * scale
        if cum_log_f is not None:
            s = s + args[2].transpose(0, 2, 1)[..., None] - c_keys[:, :, None, :]
        q_pos = blk * Q_BLOCK + jnp.arange(Q_BLOCK)
        s = jnp.where(k_pos[None, :] <= q_pos[:, None], s, NEG_INF)
        p = jax.nn.softmax(s, axis=-1).astype(v.dtype)
        return jnp.einsum('bhqk,bkhd->bqhd', p, v)

    out = lax.map(body, xs)
    return out.swapaxes(0, 1).reshape(bsz, seq, nh, v.shape[-1])


def band_blocks(t, n_prev):
    bsz, seq = t.shape[:2]
    nb = seq // Q_BLOCK
    pad = jnp.pad(t, ((0, 0), (n_prev * Q_BLOCK, 0)) + ((0, 0),) * (t.ndim - 2))
    blocks = pad.reshape((bsz, nb + n_prev, Q_BLOCK) + t.shape[2:])
    return jnp.concatenate([blocks[:, j:j + nb] for j in range(n_prev + 1)], axis=2)


def banded_attention(q, k, v, window, slopes, sinks=None):
    bsz, seq, ng, nr, hd = q.shape
    nb = seq // Q_BLOCK
    n_prev = -(-(window - 1) // Q_BLOCK)
    kb = band_blocks(k, n_prev)
    vb = band_blocks(v, n_prev)
    kbl = kb.shape[2]
    qb = q.reshape(bsz, nb, Q_BLOCK, ng, nr, hd)
    s = jnp.einsum('bnqgrd,bnkgd->bngrqk', qb, kb).astype(jnp.float32) * (hd ** -0.5)
    q_pos = jnp.arange(nb)[:, None] * Q_BLOCK + jnp.arange(Q_BLOCK)[None, :]
    k_pos = (jnp.arange(nb)[:, None] - n_prev) * Q_BLOCK + jnp.arange(kbl)[None, :]
    dist = q_pos[:, :, None] - k_pos[:, None, :]
    mask = (dist >= 0) & (dist < window) & (k_pos[:, None, :] >= 0)
    s = s - slopes[None, None, :, :, None, None] * dist[None, :, None, None].astype(jnp.float32)
    s = jnp.where(mask[None, :, None, None], s, NEG_INF)
    if sinks is None:
        p = jax.nn.softmax(s, axis=-1)
    else:
        sk = sinks.astype(jnp.float32)[None, None, :, :, None, None]
        m = jnp.maximum(jnp.max(s, axis=-1, keepdims=True), sk)
        e = jnp.exp(s - m)
        p = e / (jnp.sum(e, axis=-1, keepdims=True) + jnp.exp(sk - m))
    out = jnp.einsum('bngrqk,bnkgd->bnqgrd', p.astype(v.dtype), vb)
    return out.reshape(bsz, seq, ng, nr, hd)


def selected_attention(q, k, v, sel_idx, slopes):
    bsz, seq, ng, nr, hd = q.shape
    n_sel = sel_idx.shape[-1]
    nb = seq // Q_BLOCK
    n_keys = n_sel * NSA_SEL_BLOCK
    kblk = k.transpose(0, 2, 1, 3).reshape(bsz, ng, seq // NSA_SEL_BLOCK, NSA_SEL_BLOCK, hd)
    vblk = v.transpose(0, 2, 1, 3).reshape(bsz, ng, seq // NSA_SEL_BLOCK, NSA_SEL_BLOCK, hd)
    qb = q.reshape(bsz, nb, Q_BLOCK, ng, nr, hd).swapaxes(0, 1)
    ib = sel_idx.reshape(bsz, ng, nb, Q_BLOCK, n_sel).transpose(2, 0, 1, 3, 4)
    b_ix = jnp.arange(bsz)[:, None, None, None]
    g_ix = jnp.arange(ng)[None, :, None, None]
    scale = hd ** -0.5

    def body(args):
        qi, ii, blk = args
        ks = kblk[b_ix, g_ix, ii].reshape(bsz, ng, Q_BLOCK, n_keys, hd)
        vs = vblk[b_ix, g_ix, ii].reshape(bsz, ng, Q_BLOCK, n_keys, hd)
        s_pos = (ii[..., None] * NSA_SEL_BLOCK + jnp.arange(NSA_SEL_BLOCK)).reshape(bsz, ng, Q_BLOCK, n_keys)
        t_pos = blk * Q_BLOCK + jnp.arange(Q_BLOCK)
        dist = t_pos[None, None, :, None] - s_pos
        s = jnp.einsum('bqgrd,bgqkd->bgrqk', qi, ks).astype(jnp.float32) * scale
        s = s - slopes[None, :, :, None, None] * dist[:, :, None].astype(jnp.float32)
        s = jnp.where(dist[:, :, None] >= 0, s, NEG_INF)
        p = jax.nn.softmax(s, axis=-1).astype(v.dtype)
        return jnp.einsum('bgrqk,bgqkd->bqgrd', p, vs)

    out = lax.map(body, (qb, ib, jnp.arange(nb)))
    return out.swapaxes(0, 1).reshape(bsz, seq, ng, nr, hd)


def compress_blocks(k, pos, w1, w2):
    bsz, seq, ng, hd = k.shape
    half = k.reshape(bsz, seq // NSA_CMP_STRIDE, NSA_CMP_STRIDE, ng, hd)
    win = jnp.concatenate([half[:, :-1], half[:, 1:]], axis=2) + pos[None, None, :, None, :]
    nc = win.shape[1]
    flat = win.transpose(0, 1, 3, 2, 4).reshape(bsz, nc, ng, NSA_CMP_LEN * hd)
    return jax.nn.gelu(flat @ w1) @ w2


def nsa_mixer(q, kv, gate_logits, kc_pos, kc_w1, kc_w2, vc_pos, vc_w1, vc_w2):
    bsz, seq = q.shape[:2]
    ng, nr, hd = NSA_KV_HEADS, NSA_HEADS // NSA_KV_HEADS, NSA_HEAD_DIM
    q = q.reshape(bsz, seq, ng, nr, hd)
    kv = kv.reshape(bsz, seq, 3, 2, ng, hd)
    slopes = alibi_slopes(NSA_HEADS).reshape(ng, nr)
    t_pos = jnp.arange(seq)
    kc = compress_blocks(kv[:, :, 0, 0], kc_pos, kc_w1, kc_w2)
    vc = compress_blocks(kv[:, :, 0, 1], vc_pos, vc_w1, vc_w2)
    nc = kc.shape[1]
    c_start = jnp.arange(nc) * NSA_CMP_STRIDE
    c_end = c_start + NSA_CMP_LEN - 1
    dist = t_pos[:, None] - c_end[None, :]
    valid = dist >= 0
    s = jnp.einsum('bsgrd,bcgd->bgrsc', q, kc).astype(jnp.float32) * (hd ** -0.5)
    s = s - slopes[:, :, None, None] * dist.astype(jnp.float32)
    s = jnp.where(valid, s, NEG_INF)
    p_cmp = jax.nn.softmax(s, axis=-1) * valid
    o_cmp = jnp.einsum('bgrsc,bcgd->bsgrd', p_cmp.astype(q.dtype), vc)
    n_slc = seq // NSA_SEL_BLOCK
    j = jnp.arange(n_slc)
    overlap = ((c_start[:, None] <= j[None, :] * NSA_SEL_BLOCK + NSA_SEL_BLOCK - 1)
               & (c_end[:, None] >= j[None, :] * NSA_SEL_BLOCK)).astype(jnp.float32)
    imp = jnp.einsum('bgrsc,cn->bgsn', p_cmp, overlap)
    cur = (t_pos // NSA_SEL_BLOCK)[:, None]
    forced = (j[None, :] == 0) | (j[None, :] == cur) | (j[None, :] == cur - 1)
    imp = jnp.where(forced, 1e6, imp)
    imp = jnp.where(j[None, :] > cur, -1e6, imp)
    _, sel_idx = lax.top_k(imp, min(NSA_TOP_N, n_slc))
    o_slc = selected_attention(q, kv[:, :, 1, 0], kv[:, :, 1, 1], sel_idx, slopes)
    o_win = banded_attention(q, kv[:, :, 2, 0], kv[:, :, 2, 1], NSA_WINDOW, slopes)
    g = jax.nn.sigmoid(gate_logits.astype(jnp.float32)).reshape(bsz, seq, 3, ng, nr, 1).astype(q.dtype)
    out = g[:, :, 0] * o_cmp + g[:, :, 1] * o_slc + g[:, :, 2] * o_win
    return out.reshape(bsz, seq, NSA_HEADS * hd)


def fox_mixer(qkv, f_logit, f_bias):
    bsz, seq = qkv.shape[:2]
    qkv = qkv.reshape(bsz, seq, 3, FOX_HEADS, FOX_HEAD_DIM)
    log_f = jax.nn.log_sigmoid(f_logit.astype(jnp.float32) + f_bias.astype(jnp.float32))
    cum = jnp.cumsum(log_f, axis=1)
    out = causal_attention_blocks(qkv[:, :, 0], qkv[:, :, 1], qkv[:, :, 2], cum)
    return out.reshape(bsz, seq, FOX_HEADS * FOX_HEAD_DIM)


def mla_mixer(c_q, c_kv, k_r, q_norm_g, kv_norm_g, w_uq, w_ukv):
    bsz, seq = c_q.shape[:2]
    q = (rms_norm(c_q, q_norm_g) @ w_uq).reshape(bsz, seq, MLA_HEADS, MLA_NOPE_DIM + MLA_ROPE_DIM)
    kv = (rms_norm(c_kv, kv_norm_g) @ w_ukv).reshape(bsz, seq, MLA_HEADS, MLA_NOPE_DIM + MLA_V_DIM)
    cos, sin = rope_tables(seq)
    q_rope = apply_rope(q[..., MLA_NOPE_DIM:], cos[:, None, :], sin[:, None, :])
    k_rope = apply_rope(k_r, cos, sin)
    q = jnp.concatenate([q[..., :MLA_NOPE_DIM], q_rope], axis=-1)
    k = jnp.concatenate([kv[..., :MLA_NOPE_DIM],
                         jnp.broadcast_to(k_rope[:, :, None, :], (bsz, seq, MLA_HEADS, MLA_ROPE_DIM))], axis=-1)
    out = causal_attention_blocks(q, k, kv[..., MLA_NOPE_DIM:])
    return out.reshape(bsz, seq, MLA_HEADS * MLA_V_DIM)


def swa_mixer(q, kv, sinks):
    bsz, seq = q.shape[:2]
    nr = SWA_HEADS // SWA_KV_HEADS
    q = q.reshape(bsz, seq, SWA_KV_HEADS, nr, SWA_HEAD_DIM)
    kv = kv.reshape(bsz, seq, 2, SWA_KV_HEADS, SWA_HEAD_DIM)
    slopes = alibi_slopes(SWA_HEADS).reshape(SWA_KV_HEADS, nr)
    out = banded_attention(q, kv[:, :, 0], kv[:, :, 1], SWA_WINDOW, slopes, sinks.reshape(SWA_KV_HEADS, nr))
    return out.reshape(bsz, seq, SWA_HEADS * SWA_HEAD_DIM)


def moe_ffn(xn, rg_w, rg_b, re_w, re_b, w_gate, w_up, w_down):
    bsz, seq, d = xn.shape
    xt = xn.reshape(-1, d)
    n_tok = xt.shape[0]
    pg = jax.nn.softmax((xt @ rg_w).astype(jnp.float32) + rg_b.astype(jnp.float32), axis=-1)
    pg_top, g_idx = lax.top_k(pg, 1)
    le = ((xt @ re_w).astype(jnp.float32) + re_b.astype(jnp.float32)).reshape(n_tok, N_GROUPS, EXPERTS_PER_GROUP)
    le_g = jnp.take_along_axis(le, g_idx[:, :, None], axis=1)[:, 0]
    ev, ei = lax.top_k(le_g, TOP_K)
    gate = pg_top * jax.nn.softmax(ev, axis=-1)
    expert = g_idx * EXPERTS_PER_GROUP + ei
    n_assign = n_tok * TOP_K
    flat_e = expert.reshape(-1)
    flat_tok = jnp.repeat(jnp.arange(n_tok, dtype=jnp.int32), TOP_K)
    flat_w = gate.reshape(-1)
    order = jnp.argsort(flat_e)
    se, st, sw = flat_e[order], flat_tok[order], flat_w[order]
    counts = jnp.zeros((N_EXPERTS,), jnp.int32).at[flat_e].add(1)
    starts = jnp.cumsum(counts) - counts
    pcounts = (counts + MOE_BLOCK - 1) // MOE_BLOCK * MOE_BLOCK
    pends = jnp.cumsum(pcounts)
    pstarts = pends - pcounts
    dest = pstarts[se] + jnp.arange(n_assign, dtype=jnp.int32) - starts[se]
    n_blocks = (n_assign + N_EXPERTS * (MOE_BLOCK - 1)) // MOE_BLOCK
    n_pad = n_blocks * MOE_BLOCK
    tok_pad = jnp.full((n_pad,), n_tok, jnp.int32).at[dest].set(st)
    w_pad = jnp.zeros((n_pad,), xt.dtype).at[dest].set(sw.astype(xt.dtype))
    blk_e = jnp.minimum(jnp.searchsorted(pends, jnp.arange(n_blocks) * MOE_BLOCK, side='right'), N_EXPERTS - 1)
    x_ext = jnp.concatenate([xt, jnp.zeros((1, d), xt.dtype)], axis=0)

    def run_block(args):
        tok, e = args
        xb = x_ext[tok]
        h = jax.nn.silu(xb @ w_gate[e]) * (xb @ w_up[e])
        return h @ w_down[e]

    yb = lax.map(run_block, (tok_pad.reshape(n_blocks, MOE_BLOCK), blk_e))
    y = jnp.zeros((n_tok + 1, d), xt.dtype).at[tok_pad].add(yb.reshape(n_pad, d) * w_pad[:, None])
    return y[:n_tok].reshape(bsz, seq, d)


def setup_inputs(seed: int = 0) -> dict:
    key = jax.random.key(seed)
    ks = jax.random.split(key, 26)
    f32 = jnp.float32
    L = DEPTH
    hd = NSA_HEAD_DIM

    def nrm(k, shape, scale):
        return jax.random.normal(k, shape, f32) * scale

    def gain(k, shape):
        return 1.0 + 0.01 * jax.random.normal(k, shape, f32)

    return {
        'x': nrm(ks[0], (BATCH, SEQ, D_MODEL), 1.0),
        'norm_mix_g': gain(ks[1], (L, D_MODEL)),
        'w_in': nrm(ks[2], (L, D_MODEL, IN_COLS), D_MODEL ** -0.5),
        'nsa_kc_pos': nrm(ks[3], (L, NSA_CMP_LEN, hd), 0.02),
        'nsa_kc_w1': nrm(ks[4], (L, NSA_CMP_LEN * hd, hd), (NSA_CMP_LEN * hd) ** -0.5),
        'nsa_kc_w2': nrm(ks[5], (L, hd, hd), hd ** -0.5),
        'nsa_vc_pos': nrm(ks[6], (L, NSA_CMP_LEN, hd), 0.02),
        'nsa_vc_w1': nrm(ks[7], (L, NSA_CMP_LEN * hd, hd), (NSA_CMP_LEN * hd) ** -0.5),
        'nsa_vc_w2': nrm(ks[8], (L, hd, hd), hd ** -0.5),
        'fox_f_bias': 3.0 + 0.1 * jax.random.normal(ks[9], (L, FOX_HEADS), f32),
        'mla_q_norm_g': gain(ks[10], (L, MLA_Q_RANK)),
        'mla_kv_norm_g': gain(ks[11], (L, MLA_KV_RANK)),
        'mla_w_uq': nrm(ks[12], (L, MLA_Q_RANK, MLA_HEADS * (MLA_NOPE_DIM + MLA_ROPE_DIM)), MLA_Q_RANK ** -0.5),
        'mla_w_ukv': nrm(ks[13], (L, MLA_KV_RANK, MLA_HEADS * (MLA_NOPE_DIM + MLA_V_DIM)), MLA_KV_RANK ** -0.5),
        'swa_sinks': nrm(ks[14], (L, SWA_HEADS), 0.5),
        'out_norm_g': gain(ks[15], (L, D_MIX)),
        'w_out': nrm(ks[16], (L, D_MIX, D_MODEL), D_MIX ** -0.5),
        'norm_ffn_g': gain(ks[17], (L, D_MODEL)),
        'router_group_w': nrm(ks[18], (L, D_MODEL, N_GROUPS), D_MODEL ** -0.5),
        'router_group_b': nrm(ks[19], (L, N_GROUPS), 0.01),
        'router_expert_w': nrm(ks[20], (L, D_MODEL, N_EXPERTS), D_MODEL ** -0.5),
        'router_expert_b': nrm(ks[21], (L, N_EXPERTS), 0.01),
        'exp_w_gate': nrm(ks[22], (L, N_EXPERTS, D_MODEL, D_EXPERT), D_MODEL ** -0.5),
        'exp_w_up': nrm(ks[23], (L, N_EXPERTS, D_MODEL, D_EXPERT), D_MODEL ** -0.5),
        'exp_w_down': nrm(ks[24], (L, N_EXPERTS, D_EXPERT, D_MODEL), D_EXPERT ** -0.5),
        'final_norm_g': gain(ks[25], (D_MODEL,)),
    }


def reference(x, norm_mix_g, w_in, nsa_kc_pos, nsa_kc_w1, nsa_kc_w2, nsa_vc_pos, nsa_vc_w1, nsa_vc_w2,
              fox_f_bias, mla_q_norm_g, mla_kv_norm_g, mla_w_uq, mla_w_ukv, swa_sinks, out_norm_g, w_out,
              norm_ffn_g, router_group_w, router_group_b, router_expert_w, router_expert_b,
              exp_w_gate, exp_w_up, exp_w_down, final_norm_g):
    for l in range(DEPTH):
        xn = rms_norm(x, norm_mix_g[l])
        proj = xn @ w_in[l]
        a_q, a_kv, a_g, b_qkv, b_f, c_q, c_kv, c_kr, d_q, d_kv = split_cols(proj, IN_SPLITS)
        outs = (
            nsa_mixer(a_q, a_kv, a_g, nsa_kc_pos[l], nsa_kc_w1[l], nsa_kc_w2[l],
                      nsa_vc_pos[l], nsa_vc_w1[l], nsa_vc_w2[l]),
            fox_mixer(b_qkv, b_f, fox_f_bias[l]),
            mla_mixer(c_q, c_kv, c_kr, mla_q_norm_g[l], mla_kv_norm_g[l], mla_w_uq[l], mla_w_ukv[l]),
            swa_mixer(d_q, d_kv, swa_sinks[l]),
        )
        gains = split_cols(out_norm_g[l], OUT_SPLITS)
        mixed = jnp.concatenate([rms_norm(o, g) for o, g in zip(outs, gains)], axis=-1)
        x = x + mixed @ w_out[l]
        x = x + moe_ffn(rms_norm(x, norm_ffn_g[l]), router_group_w[l], router_group_b[l],
                        router_expert_w[l], router_expert_b[l], exp_w_gate[l], exp_w_up[l], exp_w_down[l])
    return rms_norm(x, final_norm_g)
```

```python
import numpy as np
import ml_dtypes
import concourse.bass as bass
import concourse.mybir as mybir
from concourse.bass_utils import run_bass_kernel_spmd

F32 = mybir.dt.float32
BF16 = mybir.dt.bfloat16
I32 = mybir.dt.int32
AF = mybir.ActivationFunctionType
ALU = mybir.AluOpType
AX = mybir.AxisListType

ENGS = ("tensor", "vector", "scalar", "gpsimd", "sync")
DMA_SLOTS = 8


class Res:
    __slots__ = ("name", "lw", "rd")

    def __init__(self, name):
        self.name = name
        self.lw = None
        self.rd = {}


class Op:
    __slots__ = ("eng", "fn", "dma", "deps", "sig", "sem", "val", "idx")

    def __init__(self, eng, fn, dma):
        self.eng = eng
        self.fn = fn
        self.dma = dma
        self.deps = []
        self.sig = False
        self.sem = None
        self.val = 0


class Sched:
    def __init__(self, nc):
        self.nc = nc
        self.ops = []
        self.dma_cnt = {e: 0 for e in ENGS}
        self.dma_last = {}
        self.out_dmas = []

    def op(self, eng, fn, reads=(), writes=(), dma=False):
        o = Op(eng, fn, dma)
        o.idx = len(self.ops)
        deps = {}
        for r in reads:
            if r.lw is not None:
                deps[r.lw.idx] = (r.lw, "raw")
        for w in writes:
            if w.lw is not None and w.lw.idx not in deps:
                deps[w.lw.idx] = (w.lw, "waw")
            for rr in w.rd.values():
                if rr.idx not in deps:
                    deps[rr.idx] = (rr, "war")
        if dma:
            n = self.dma_cnt[eng]
            self.dma_cnt[eng] = n + 1
            slot = n % DMA_SLOTS
            o.sem = ("dma", eng, slot)
            o.val = 16 * (n // DMA_SLOTS + 1)
            o.sig = True
            prev = self.dma_last.get((eng, slot))
            if prev is not None and prev.idx not in deps:
                deps[prev.idx] = (prev, "slot")
            self.dma_last[(eng, slot)] = o
        for d, kind in deps.values():
            if not d.dma and d.eng == eng:
                if kind != "raw" or eng == "tensor":
                    continue
            o.deps.append(d)
            d.sig = True
        for r in reads:
            r.rd[o.sem if dma else eng] = o
        for w in writes:
            w.lw = o
            w.rd = {}
        self.ops.append(o)
        return o

    def emit(self):
        nc = self.nc
        cnt = {e: 0 for e in ENGS}
        for o in self.ops:
            if not o.dma and o.sig:
                cnt[o.eng] += 1
                o.sem = ("eng", o.eng)
                o.val = cnt[o.eng]
        semkeys = [("eng", e) for e in ENGS]
        for e in ENGS:
            for s in range(min(DMA_SLOTS, self.dma_cnt[e])):
                semkeys.append(("dma", e, s))
        import contextlib
        with contextlib.ExitStack() as st:
            sems = {k: st.enter_context(nc.semaphore("s_" + "_".join(str(x) for x in k))) for k in semkeys}
            block = st.enter_context(nc.Block())
            per = {e: [o for o in self.ops if o.eng == e] for e in ENGS}
            final = {}
            for (eng, slot), o in self.dma_last.items():
                final.setdefault(eng, []).append((sems[o.sem], o.val))

            def run(e, engobj):
                seen = {}
                for o in per[e]:
                    for d in o.deps:
                        if seen.get(d.sem, 0) < d.val:
                            engobj.wait_ge(sems[d.sem], d.val)
                            seen[d.sem] = d.val
                    ins = o.fn(engobj)
                    if o.sig:
                        ins.then_inc(sems[o.sem], 16 if o.dma else 1)
                for s, v in final.get(e, []):
                    engobj.wait_ge(s, v)

            @block.tensor
            def _(eng):
                run("tensor", eng)

            @block.vector
            def _(eng):
                run("vector", eng)

            @block.scalar
            def _(eng):
                run("scalar", eng)

            @block.gpsimd
            def _(eng):
                run("gpsimd", eng)

            @block.sync
            def _(eng):
                run("sync", eng)


class V:
    __slots__ = ("ap", "res")

    def __init__(self, ap, res):
        self.ap = ap
        self.res = res

    def __getitem__(self, idx):
        return V(self.ap[idx], self.res)

    def rr(self, s, **kw):
        return V(self.ap.rearrange(s, **kw), self.res)

    def bc(self, shape):
        return V(self.ap.broadcast_to(shape), self.res)

    def un(self, axis):
        return V(self.ap.unsqueeze(axis), self.res)


class Tile:
    def __init__(self, handle, name):
        self.h = handle
        self.name = name
        self.res = Res(name)
        self.subs = {}

    def __getitem__(self, idx):
        return V(self.h[idx], self.res)

    def sub(self, key, idx=None):
        r = self.subs.get(key)
        if r is None:
            r = self.subs[key] = Res("%s/%s" % (self.name, key))
        return V(self.h[idx] if idx is not None else self.h[:], r)


def _aps(x):
    return x.ap if isinstance(x, V) else x


class KB:
    def __init__(self, nc, st):
        self.nc = nc
        self.st = st
        self.S = Sched(nc)
        self.n = 0

    def sb(self, name, shape, dt):
        return Tile(self.st.enter_context(self.nc.sbuf_tensor(name, list(shape), dt)), name)

    def ps(self, name, shape, dt):
        return Tile(self.st.enter_context(self.nc.psum_tensor(name, list(shape), dt)), name)

    def dram(self, name, shape, dt, kind):
        ap = self.nc.dram_tensor(name, list(shape), dt, kind=kind).ap()
        return V(ap, Res(name))

    def _op(self, eng, fn, reads, writes, dma=False):
        rs = [r.res for r in reads if isinstance(r, V)]
        ws = [w.res for w in writes if isinstance(w, V)]
        return self.S.op(eng, fn, rs, ws, dma)

    def dma(self, out, in_, eng="sync", **kw):
        return self._op(eng, lambda e: e.dma_start(out=out.ap, in_=in_.ap, **kw), [in_], [out], dma=True)

    def gather(self, out, table, idx, **kw):
        return self._op("gpsimd", lambda e: e.indirect_dma_start(
            out=out.ap, out_offset=None, in_=table.ap,
            in_offset=bass.IndirectOffsetOnAxis(ap=idx.ap, axis=0), **kw), [table, idx], [out], dma=True)

    def scatter(self, table, idx, in_, **kw):
        return self._op("gpsimd", lambda e: e.indirect_dma_start(
            out=table.ap, out_offset=bass.IndirectOffsetOnAxis(ap=idx.ap, axis=0),
            in_=in_.ap, in_offset=None, **kw), [in_, idx], [table], dma=True)

    def mm(self, out, lhsT, rhs, start=True, stop=True):
        return self._op("tensor", lambda e: e.matmul(out.ap, lhsT=lhsT.ap, rhs=rhs.ap, start=start, stop=stop),
                        [lhsT, rhs], [out])

    def tr(self, out, in_, ident):
        return self._op("tensor", lambda e: e.transpose(out.ap, in_.ap, ident.ap), [in_, ident], [out])

    def act(self, out, in_, func, bias=0.0, scale=1.0, accum=None, eng="scalar"):
        kw = {}
        if accum is not None:
            kw["accum_out"] = accum.ap
        ws = [out] + ([accum] if accum is not None else [])
        return self._op(eng, lambda e: e.activation(out=out.ap, in_=in_.ap, func=func, bias=_aps(bias),
                                                    scale=_aps(scale), **kw), [in_, bias, scale], ws)

    def tt(self, out, in0, in1, op, eng="vector"):
        return self._op(eng, lambda e: e.tensor_tensor(out=out.ap, in0=in0.ap, in1=in1.ap, op=op), [in0, in1], [out])

    def ts(self, out, in0, s1, s2, op0, op1=None, accum=None, eng="vector"):
        kw = {}
        if op1 is not None:
            kw["op1"] = op1
        if accum is not None:
            kw["accum_out"] = accum.ap
        ws = [out] + ([accum] if accum is not None else [])
        return self._op(eng, lambda e: e.tensor_scalar(out=out.ap, in0=in0.ap, scalar1=_aps(s1), scalar2=_aps(s2),
                                                       op0=op0, **kw), [in0, s1, s2], ws)

    def stt(self, out, in0, scalar, in1, op0, op1, eng="vector"):
        return self._op(eng, lambda e: e.scalar_tensor_tensor(out=out.ap, in0=in0.ap, scalar=_aps(scalar), in1=in1.ap,
                                                              op0=op0, op1=op1), [in0, scalar, in1], [out])

    def copy(self, out, in_, eng="vector"):
        if eng == "scalar":
            return self._op(eng, lambda e: e.copy(out=out.ap, in_=in_.ap), [in_], [out])
        return self._op(eng, lambda e: e.tensor_copy(out=out.ap, in_=in_.ap), [in_], [out])

    def memset(self, out, val, eng="vector"):
        return self._op(eng, lambda e: e.memset(out.ap, val), [], [out])

    def recip(self, out, in_):
        return self._op("vector", lambda e: e.reciprocal(out=out.ap, in_=in_.ap), [in_], [out])

    def reduce(self, out, in_, op, axis=AX.X):
        return self._op("vector", lambda e: e.tensor_reduce(out=out.ap, in_=in_.ap, axis=axis, op=op), [in_], [out])

    def max8(self, out, in_):
        return self._op("vector", lambda e: e.max(out=out.ap, in_=in_.ap), [in_], [out])

    def match_replace(self, out, rep, vals, imm):
        return self._op("vector", lambda e: e.match_replace(out=out.ap, in_to_replace=rep.ap, in_values=vals.ap,
                                                            imm_value=imm), [rep, vals], [out])

    def rstd(self, out, ss, n, tmp):
        self.ts(tmp, ss, 1.0 / n, 1e-6, ALU.mult, ALU.add)
        self.act(tmp, tmp, AF.Sqrt)
        self.recip(out, tmp)


import contextlib

D = 4096
NT = 1024
EPS = 1e-6
BIG = 30000.0


def col_view(v, p=128):
    return v.rr("(c p) -> p c", p=p)


def build_c():
    nc = bass.Bass("TRN2", target_bir_lowering=False)
    with contextlib.ExitStack() as st:
        K = KB(nc, st)
        o_d = K.dram("o", [NT, D], F32, "ExternalInput")
        x_d = K.dram("x", [NT, D], F32, "ExternalInput")
        gout_d = K.dram("gout", [D], F32, "ExternalInput")
        wout_d = K.dram("wout", [D, D], F32, "ExternalInput")
        gffn_d = K.dram("gffn", [D], F32, "ExternalInput")
        rw_d = K.dram("rw", [D, 72], F32, "ExternalInput")
        rb_d = K.dram("rb", [72], F32, "ExternalInput")
        ident_d = K.dram("ident", [128, 128], F32, "ExternalInput")
        xmid_d = K.dram("xmid", [NT, D], F32, "ExternalOutput")
        xn2_d = K.dram("xn2", [NT, D], BF16, "ExternalOutput")
        m_d = K.dram("mroute", [NT, 64], F32, "ExternalOutput")
        gw_d = K.dram("gwroute", [NT, 64], F32, "ExternalOutput")
        rt_d = K.dram("route", [NT, 4], F32, "ExternalOutput")
        iota_d = K.dram("iota64", [128, 64], F32, "ExternalInput")
        xmid_res = [Res("xmid%d" % n) for n in range(NT // 128)]

        identf = K.sb("identf", [128, 128], F32)
        identb = K.sb("identb", [128, 128], BF16)
        gcol = K.sb("gcol", [128, 32], F32)
        g2col = K.sb("g2col", [128, 32], F32)
        wr = K.sb("wr", [128, 32, 72], F32)
        rbb = K.sb("rbb", [128, 72], F32)
        iota = K.sb("iota", [128, 64], F32)
        rto = K.sb("rto", [128, 4], F32)
        rtmp = K.sb("rtmp", [128, 64], F32)
        K.dma(iota[:], iota_d)
        K.dma(identf[:], ident_d)
        K.copy(identb[:], identf[:])
        K.dma(gcol[:], col_view(gout_d), allow_slow_non_contiguous=True)
        K.dma(g2col[:], col_view(gffn_d), allow_slow_non_contiguous=True)
        K.dma(wr[:], rw_d.rr("(c p) n -> p c n", p=128))
        K.dma(rbb[:], V(rb_d.ap.partition_broadcast(128), rb_d.res))
        K.tt(wr[:], wr[:], g2col[:].un(2).bc([128, 32, 72]), ALU.mult)

        mixedT = K.sb("mixedT", [128, 32, 512], BF16)
        wbf = [K.sb("wbf%d" % i, [128, 32, 512], BF16) for i in range(2)]
        ot = K.sb("ot", [128, D], F32)
        onb = K.sb("onb", [128, D], BF16)
        junk = K.sb("junk", [128, 1024], BF16)
        xnb = K.sb("xnb", [128, D], BF16)
        ss = K.sb("ss", [128, 4], F32)
        tmp4 = K.sb("tmp4", [128, 4], F32)
        rs4 = K.sb("rs4", [128, 4], F32)
        xt = [K.sb("xt%d" % i, [128, 4, 512], F32) for i in range(2)]
        ysb = [K.sb("ysb%d" % i, [128, 512], F32) for i in range(2)]
        xmT = K.sb("xmT", [128, 32, 128], F32)
        lg = K.sb("lg", [128, 72], F32)
        sm = K.sb("sm", [128, 16], F32)
        oh = K.sb("oh", [128, 8], F32)
        msk = K.sb("msk", [128, 64], F32)
        m8 = K.sb("m8", [128, 8], F32)
        mo = K.sb("mo", [128, 64], F32)
        gwo = K.sb("gwo", [128, 64], F32)
        tpb = [K.ps("tpb%d" % i, [128, 8, 128], BF16) for i in range(2)]
        yps = [K.ps("yps%d" % i, [128, 512], F32) for i in range(2)]
        tpf = [K.ps("tpf%d" % i, [128, 4, 128], F32) for i in range(2)]
        lgp = K.ps("lgp", [128, 72], F32)

        wv = wout_d.rr("(c p) n -> p c n", p=128)
        wcount = 0
        for hf in range(NT // 512):
            for n in range(4):
                r0 = hf * 512 + n * 128
                K.dma(ot[:], o_d[r0:r0 + 128, :])
                for gi in range(4):
                    K.act(junk[:], ot[:, gi * 1024:(gi + 1) * 1024], AF.Square, accum=ss[:, gi:gi + 1])
                K.rstd(rs4[:], ss[:], 1024.0, tmp4[:])
                for gi in range(4):
                    K.ts(onb.sub(gi, (slice(None), slice(gi * 1024, (gi + 1) * 1024))),
                         ot[:, gi * 1024:(gi + 1) * 1024], rs4[:, gi:gi + 1], None, ALU.mult)
                for k in range(4):
                    tp = tpb[k % 2]
                    for c in range(8):
                        cc = k * 8 + c
                        K.tr(tp[:, c, :], onb.sub(cc // 8, (slice(None), slice(cc * 128, (cc + 1) * 128))), identb[:])
                    K.tt(mixedT.sub((n, k), (slice(None), slice(k * 8, k * 8 + 8), slice(n * 128, (n + 1) * 128))),
                         tp[:], gcol[:, k * 8:k * 8 + 8].un(2).bc([128, 8, 128]), ALU.mult)
            for j in range(8):
                wb = wbf[wcount % 2]
                wcount += 1
                K.dma(wb[:], wv[:, :, j * 512:(j + 1) * 512], eng="gpsimd")
                xb = xt[j % 2]
                K.dma(xb[:], x_d[hf * 512:(hf + 1) * 512, j * 512:(j + 1) * 512].rr("(n p) c -> p n c", p=128))
                for n in range(4):
                    yp = yps[n % 2]
                    for c in range(32):
                        K.mm(yp[:], mixedT.sub((n, c // 8), (slice(None), c, slice(n * 128, (n + 1) * 128))),
                             wb[:, c, :], start=(c == 0), stop=(c == 31))
                    yb = ysb[n % 2]
                    K.tt(yb[:], yp[:], xb[:, n, :], ALU.add)
                    r0 = hf * 512 + n * 128
                    K.dma(V(xmid_d.ap[r0:r0 + 128, j * 512:(j + 1) * 512], xmid_res[hf * 4 + n]), yb[:])
            for n in range(4):
                r0 = hf * 512 + n * 128
                K.dma(ot[:], V(xmid_d.ap[r0:r0 + 128, :], xmid_res[hf * 4 + n]))
                K.act(xnb[:], ot[:], AF.Square, accum=ss[:, 0:1])
                K.rstd(rs4[:, 0:1], ss[:, 0:1], float(D), tmp4[:, 0:1])
                K.ts(xnb[:], ot[:], rs4[:, 0:1], None, ALU.mult)
                K.dma(xn2_d[r0:r0 + 128, :], xnb[:])
                for k in range(8):
                    tp = tpf[k % 2]
                    for c in range(4):
                        cc = k * 4 + c
                        K.tr(tp[:, c, :], ot[:, cc * 128:(cc + 1) * 128], identf[:])
                    K.copy(xmT.sub(k, (slice(None), slice(k * 4, k * 4 + 4), slice(None))), tp[:],
                           eng=("scalar" if k % 2 else "vector"))
                for c in range(32):
                    K.mm(lgp[:], xmT.sub(c // 4, (slice(None), c, slice(None))), wr[:, c, :],
                         start=(c == 0), stop=(c == 31))
                K.stt(lg[:], lgp[:], rs4[:, 0:1], rbb[:], ALU.mult, ALU.add)
                router_math(K, lg, sm, oh, msk, m8, mo, gwo, iota, rto, rtmp)
                K.dma(rt_d[r0:r0 + 128, :], rto[:])
                K.dma(m_d[r0:r0 + 128, :], mo[:])
                K.dma(gw_d[r0:r0 + 128, :], gwo[:])
        K.S.emit()
    return nc


def router_math(K, lg, sm, oh, msk, m8, mo, gwo, iota, rto, rtmp):
    K.reduce(sm[:, 0:1], lg[:, 0:8], ALU.max)
    K.ts(oh[:], lg[:, 0:8], sm[:, 0:1], None, ALU.is_ge)
    K.ts(sm[:, 1:2], sm[:, 0:1], -1.0, None, ALU.mult)
    K.act(msk[:, 0:8], lg[:, 0:8], AF.Exp, bias=sm[:, 1:2], accum=sm[:, 2:3])
    K.recip(sm[:, 3:4], sm[:, 2:3])
    K.ts(oh[:], oh[:], BIG, -BIG, ALU.mult, ALU.add)
    K.tt(msk[:].rr("p (g e) -> p g e", g=8), lg[:, 8:72].rr("p (g e) -> p g e", g=8),
         oh[:].un(2).bc([128, 8, 8]), ALU.add)
    K.max8(m8[:], msk[:])
    K.ts(mo[:], msk[:], m8[:, 1:2], None, ALU.is_ge)
    K.tt(sm[:, 4:5], m8[:, 0:1], m8[:, 1:2], ALU.add)
    K.ts(sm[:, 4:5], sm[:, 4:5], -1.0, None, ALU.mult)
    K.act(gwo[:], msk[:], AF.Sigmoid, bias=sm[:, 4:5], scale=2.0)
    K.stt(gwo[:], gwo[:], sm[:, 3:4], mo[:], ALU.mult, ALU.mult)
    K.stt(rtmp[:], iota[:], 1.0, mo[:], ALU.add, ALU.mult)
    K.reduce(rto[:, 1:2], rtmp[:], ALU.max)
    K.ts(rto[:, 1:2], rto[:, 1:2], -1.0, None, ALU.add)
    K.ts(rtmp[:], iota[:], -1.0, 64.0, ALU.mult, ALU.add)
    K.tt(rtmp[:], rtmp[:], mo[:], ALU.mult)
    K.reduce(rto[:, 0:1], rtmp[:], ALU.max)
    K.ts(rto[:, 0:1], rto[:, 0:1], -1.0, 64.0, ALU.mult, ALU.add)
    for k in range(2):
        K.ts(rtmp[:], iota[:], rto[:, k:k + 1], None, ALU.is_equal)
        K.tt(rtmp[:], rtmp[:], gwo[:], ALU.mult)
        K.reduce(rto[:, 2 + k:3 + k], rtmp[:], ALU.add)


CE = 384
NTOK = 8192
NTT = NTOK // 128
OOB = 1.0e6


def build_d1():
    nc = bass.Bass("TRN2", target_bir_lowering=False)
    with contextlib.ExitStack() as st:
        K = KB(nc, st)
        m_d = K.dram("mall", [NTOK, 64], F32, "ExternalInput")
        rt_d = K.dram("rtall", [NTOK, 4], F32, "ExternalInput")
        gb_d = K.dram("gbase", [1], F32, "ExternalInput")
        tri_d = K.dram("tri", [128, 128], F32, "ExternalInput")
        tok_d = K.dram("tokid", [128, NTT], F32, "ExternalInput")
        iota_d = K.dram("iota64", [128, 64], F32, "ExternalInput")
        idx_d = K.dram("idxlist", [8 * CE, 2], I32, "ExternalOutput")
        dg_d = K.dram("destg", [NTOK, 2], I32, "ExternalOutput")

        Mt = K.sb("Mt", [128, NTT, 64], F32)
        Mb = K.sb("Mb", [128, NTT, 64], BF16)
        Rf = K.sb("Rf", [128, NTT, 64], F32)
        Rb = K.sb("Rb", [128, NTT, 64], BF16)
        slot = K.sb("slot", [128, NTT, 64], F32)
        oh = K.sb("ohd", [128, NTT, 64], F32)
        trif = K.sb("trif", [128, 128], F32)
        trib = K.sb("trib", [128, 128], BF16)
        oneb = K.sb("oneb", [128, 128], BF16)
        iota = K.sb("iota", [128, 64], F32)
        tokf = K.sb("tokf", [128, NTT], F32)
        toki = K.sb("toki", [128, NTT, 2], I32)
        rt = K.sb("rt", [128, NTT, 4], F32)
        gb = K.sb("gb", [128, 1], F32)
        sl = K.sb("sl", [128, NTT], F32)
        dgl = K.sb("dgl", [128, NTT], F32)
        dl = K.sb("dl", [128, NTT], F32)
        ok = K.sb("ok", [128, NTT], F32)
        ok2 = K.sb("ok2", [128, NTT], F32)
        di = [K.sb("di%d" % k, [128, NTT], I32) for k in range(2)]
        dgi = K.sb("dgi", [128, NTT, 2], I32)
        zt = K.sb("zt", [128, 8 * CE // 128, 2], I32)
        pss = [K.ps("pss%d" % i, [128, 8, 64], F32) for i in range(8)]

        K.dma(Mt[:], m_d.rr("(n p) e -> p n e", p=128))
        K.dma(rt[:], rt_d.rr("(n p) k -> p n k", p=128))
        K.dma(trif[:], tri_d)
        K.dma(tokf[:], tok_d)
        K.dma(iota[:], iota_d)
        K.dma(gb[:], V(gb_d.ap.partition_broadcast(128), gb_d.res))
        K.copy(trib[:], trif[:])
        K.memset(oneb[:], 1.0)
        K.copy(toki[:, :, 0], tokf[:])
        K.copy(toki[:, :, 1], tokf[:])
        K.memset(zt[:], 0)
        K.dma(idx_d.rr("(s p) o -> p s o", p=128), zt[:])
        K.copy(Mb[:], Mt[:])
        K.memset(Rf[:, 0, :], 0.0)
        for n in range(1, NTT):
            K.tt(Rf[:, n, :], Rf[:, n - 1, :], Mt[:, n - 1, :], ALU.add)
        K.copy(Rb[:], Rf[:])
        for n in range(NTT):
            p = pss[n // 8]
            K.mm(p[:, n % 8, :], trib[:], Mb[:, n, :], start=True, stop=False)
            K.mm(p[:, n % 8, :], oneb[:], Rb[:, n, :], start=False, stop=True)
        for b in range(8):
            K.copy(slot[:, b * 8:(b + 1) * 8, :], pss[b][:], eng=("scalar" if b % 2 else "vector"))
        iota3 = iota[:].un(1).bc([128, NTT, 64])
        for k in range(2):
            K.tt(oh[:], iota3, rt[:, :, k].un(2).bc([128, NTT, 64]), ALU.is_equal)
            K.tt(oh[:], oh[:], slot[:], ALU.mult)
            K.reduce(sl[:], oh[:], ALU.add)
            K.stt(dgl[:], rt[:, :, k], float(CE), sl[:], ALU.mult, ALU.add)
            K.copy(dgi[:, :, k], dgl[:])
            K.ts(ok[:], sl[:], float(CE), None, ALU.is_lt)
            K.ts(dl[:], dgl[:], gb[:, 0:1], None, ALU.subtract)
            K.ts(ok2[:], dl[:], 0.0, None, ALU.is_ge)
            K.tt(ok[:], ok[:], ok2[:], ALU.mult)
            K.ts(ok2[:], dl[:], float(8 * CE), None, ALU.is_lt)
            K.tt(ok[:], ok[:], ok2[:], ALU.mult)
            K.ts(dl[:], dl[:], -OOB, None, ALU.add)
            K.tt(dl[:], dl[:], ok[:], ALU.mult)
            K.ts(dl[:], dl[:], OOB, None, ALU.add)
            K.copy(di[k][:], dl[:])
        K.dma(dg_d.rr("(n p) k -> p n k", p=128), dgi[:])
        for n in range(NTT):
            for k in range(2):
                K.scatter(idx_d, di[k][:, n:n + 1], toki[:, n, :], bounds_check=8 * CE - 1, oob_is_err=False)
        K.S.emit()
    return nc


NPROJ = 8288


def ot_pre(K):
    if not hasattr(K, "_otp"):
        K._otp = K.sb("otp", [128, D], F32)
    return K._otp


def norm_transpose(K, src_d, r0, width, gcols, ot, onb, ss, rs, tmp, tpb, identb, dstT, n, junk):
    nch = width // 128
    K.dma(ot[:, 0:width], src_d[r0:r0 + 128, :])
    K.act(junk[:, 0:width], ot[:, 0:width], AF.Square, accum=ss[:, 0:1])
    K.rstd(rs[:, 0:1], ss[:, 0:1], float(width), tmp[:, 0:1])
    K.ts(onb[:, 0:width], ot[:, 0:width], rs[:, 0:1], None, ALU.mult)
    for k in range((nch + 7) // 8):
        tp = tpb[k % 2]
        m = min(8, nch - k * 8)
        for c in range(m):
            cc = k * 8 + c
            K.tr(tp[:, c, :], onb[:, cc * 128:(cc + 1) * 128], identb[:])
        K.tt(dstT.sub((n, k), (slice(None), slice(k * 8, k * 8 + m), slice(n * 128, (n + 1) * 128))),
             tp[:, 0:m, :], gcols[:, k * 8:k * 8 + m].un(2).bc([128, m, 128]), ALU.mult)


def build_a():
    nc = bass.Bass("TRN2", target_bir_lowering=False)
    with contextlib.ExitStack() as st:
        K = KB(nc, st)
        x_d = K.dram("x", [NT, D], F32, "ExternalInput")
        xa_d = K.dram("xadd", [NT, D], F32, "ExternalInput")
        g_d = K.dram("g", [D], F32, "ExternalInput")
        w_d = K.dram("w", [D, NPROJ], F32, "ExternalInput")
        ident_d = K.dram("ident", [128, 128], F32, "ExternalInput")
        p_d = K.dram("proj", [NT, NPROJ], F32, "ExternalOutput")
        xs_d = K.dram("xsum", [NT, D], F32, "ExternalOutput")
        xs_res = [Res("xs%d" % n) for n in range(NT // 128)]
        xa = K.sb("xa", [128, D], F32)
        for n in range(NT // 128):
            K.dma(ot_pre(K)[:], x_d[n * 128:(n + 1) * 128, :])
            K.dma(xa[:], xa_d[n * 128:(n + 1) * 128, :])
            K.tt(xa[:], xa[:], ot_pre(K)[:], ALU.add)
            K.dma(V(xs_d.ap[n * 128:(n + 1) * 128, :], xs_res[n]), xa[:])
        identf = K.sb("identf", [128, 128], F32)
        identb = K.sb("identb", [128, 128], BF16)
        gcol = K.sb("gcol", [128, 32], F32)
        K.dma(identf[:], ident_d)
        K.copy(identb[:], identf[:])
        K.dma(gcol[:], col_view(g_d), allow_slow_non_contiguous=True)
        xT = K.sb("xT", [128, 32, 512], BF16)
        wbf = [K.sb("wbf%d" % i, [128, 32, 512], BF16) for i in range(2)]
        ot = K.sb("ot", [128, D], F32)
        onb = K.sb("onb", [128, D], BF16)
        junk = K.sb("junk", [128, D], BF16)
        ss = K.sb("ss", [128, 1], F32)
        tmp = K.sb("tmp", [128, 1], F32)
        rs = K.sb("rs", [128, 1], F32)
        ysb = [K.sb("ysb%d" % i, [128, 512], F32) for i in range(4)]
        tpb = [K.ps("tpb%d" % i, [128, 8, 128], BF16) for i in range(2)]
        yps = [K.ps("yps%d" % i, [128, 512], F32) for i in range(4)]
        wv = w_d.rr("(c p) n -> p c n", p=128)
        chunks = [(j * 512, 512) for j in range(16)] + [(8192, 96)]
        wcount = 0
        ycount = 0
        for hf in range(NT // 512):
            for n in range(4):
                norm_transpose(K, V(xs_d.ap, xs_res[hf * 4 + n]), hf * 512 + n * 128, D, gcol, ot, onb, ss, rs, tmp, tpb,
                               identb, xT, n, junk)
            for (c0, cw) in chunks:
                wb = wbf[wcount % 2]
                wcount += 1
                K.dma(wb[:, :, 0:cw], wv[:, :, c0:c0 + cw], eng="gpsimd")
                for n in range(4):
                    yp = yps[ycount % 4]
                    yb = ysb[ycount % 4]
                    for c in range(32):
                        K.mm(yp[:, 0:cw], xT.sub((n, c // 8), (slice(None), c, slice(n * 128, (n + 1) * 128))),
                             wb[:, c, 0:cw], start=(c == 0), stop=(c == 31))
                    K.copy(yb[:, 0:cw], yp[:, 0:cw], eng=("scalar" if ycount % 2 else "vector"))
                    ycount += 1
                    r0 = hf * 512 + n * 128
                    K.dma(p_d[r0:r0 + 128, c0:c0 + cw], yb[:, 0:cw])
        K.S.emit()
    return nc


def build_a2():
    nc = bass.Bass("TRN2", target_bir_lowering=False)
    with contextlib.ExitStack() as st:
        K = KB(nc, st)
        cq_d = K.dram("cq", [NT, 768], F32, "ExternalInput")
        ckv_d = K.dram("ckv", [NT, 512], F32, "ExternalInput")
        kr_d = K.dram("kr", [NT, 64], F32, "ExternalInput")
        gq_d = K.dram("gq", [768], F32, "ExternalInput")
        gkv_d = K.dram("gkv", [512], F32, "ExternalInput")
        wuq_d = K.dram("wuq", [768, 1536], F32, "ExternalInput")
        wukv_d = K.dram("wukv", [512, 2048], F32, "ExternalInput")
        cos_d = K.dram("cos", [NT, 32], F32, "ExternalInput")
        sin_d = K.dram("sin", [NT, 32], F32, "ExternalInput")
        ident_d = K.dram("ident", [128, 128], F32, "ExternalInput")
        q_d = K.dram("q", [NT, 1536], F32, "ExternalOutput")
        kv_d = K.dram("kv", [NT, 2048], F32, "ExternalOutput")
        kro_d = K.dram("kro", [NT, 64], F32, "ExternalOutput")
        identf = K.sb("identf", [128, 128], F32)
        identb = K.sb("identb", [128, 128], BF16)
        gqc = K.sb("gqc", [128, 6], F32)
        gkvc = K.sb("gkvc", [128, 4], F32)
        wuq = K.sb("wuqs", [128, 6, 1536], BF16)
        wukv = K.sb("wukvs", [128, 4, 2048], BF16)
        K.dma(identf[:], ident_d)
        K.copy(identb[:], identf[:])
        K.dma(gqc[:], col_view(gq_d), allow_slow_non_contiguous=True)
        K.dma(gkvc[:], col_view(gkv_d), allow_slow_non_contiguous=True)
        K.dma(wuq[:], wuq_d.rr("(c p) n -> p c n", p=128), eng="gpsimd")
        K.dma(wukv[:], wukv_d.rr("(c p) n -> p c n", p=128), eng="gpsimd")
        cqT = K.sb("cqT", [128, 6, 128], BF16)
        ckvT = K.sb("ckvT", [128, 4, 128], BF16)
        ot = K.sb("ot", [128, 768], F32)
        onb = K.sb("onb", [128, 768], BF16)
        junk = K.sb("junk", [128, 768], BF16)
        ss = K.sb("ss", [128, 1], F32)
        tmp = K.sb("tmp", [128, 1], F32)
        rs = K.sb("rs", [128, 1], F32)
        qsb = K.sb("qsb", [128, 1536], F32)
        kvsb = K.sb("kvsb", [128, 2048], F32)
        krs = K.sb("krs", [128, 64], F32)
        cs = K.sb("cs", [128, 32], F32)
        sn = K.sb("sn", [128, 32], F32)
        t1 = K.sb("t1", [128, 8, 32], F32)
        t2 = K.sb("t2", [128, 8, 32], F32)
        t3 = K.sb("t3", [128, 8, 32], F32)
        t4 = K.sb("t4", [128, 8, 32], F32)
        tpb = [K.ps("tpb%d" % i, [128, 8, 128], BF16) for i in range(2)]
        yps = [K.ps("yps%d" % i, [128, 512], F32) for i in range(4)]

        def rope(buf3, nh):
            x1 = buf3[:, :, 0:32]
            x2 = buf3[:, :, 32:64]
            cb = cs[:].un(1).bc([128, nh, 32])
            sb_ = sn[:].un(1).bc([128, nh, 32])
            K.tt(t1[:, 0:nh, :], x1, cb, ALU.mult)
            K.tt(t2[:, 0:nh, :], x2, sb_, ALU.mult)
            K.tt(t3[:, 0:nh, :], x2, cb, ALU.mult)
            K.tt(t4[:, 0:nh, :], x1, sb_, ALU.mult)
            K.tt(x1, t1[:, 0:nh, :], t2[:, 0:nh, :], ALU.subtract)
            K.tt(x2, t3[:, 0:nh, :], t4[:, 0:nh, :], ALU.add)

        yc = 0
        for n in range(NT // 128):
            r0 = n * 128
            norm_transpose(K, cq_d, r0, 768, gqc, ot, onb, ss, rs, tmp, tpb, identb, cqT, 0, junk)
            norm_transpose(K, ckv_d, r0, 512, gkvc, ot, onb, ss, rs, tmp, tpb, identb, ckvT, 0, junk)
            K.dma(krs[:], kr_d[r0:r0 + 128, :])
            K.dma(cs[:], cos_d[r0:r0 + 128, :])
            K.dma(sn[:], sin_d[r0:r0 + 128, :])
            for j in range(3):
                yp = yps[yc % 4]
                yc += 1
                for c in range(6):
                    K.mm(yp[:], cqT.sub((0, 0), (slice(None), c, slice(None))), wuq[:, c, j * 512:(j + 1) * 512],
                         start=(c == 0), stop=(c == 5))
                K.copy(qsb[:, j * 512:(j + 1) * 512], yp[:], eng=("scalar" if j % 2 else "vector"))
            for j in range(4):
                yp = yps[yc % 4]
                yc += 1
                for c in range(4):
                    K.mm(yp[:], ckvT.sub((0, 0), (slice(None), c, slice(None))), wukv[:, c, j * 512:(j + 1) * 512],
                         start=(c == 0), stop=(c == 3))
                K.copy(kvsb[:, j * 512:(j + 1) * 512], yp[:], eng=("scalar" if j % 2 else "vector"))
            rope(qsb[:].rr("p (h d) -> p h d", d=192)[:, :, 128:192], 8)
            rope(krs[:].rr("p (h d) -> p h d", d=64), 1)
            K.dma(q_d[r0:r0 + 128, :], qsb[:])
            K.dma(kv_d[r0:r0 + 128, :], kvsb[:])
            K.dma(kro_d[r0:r0 + 128, :], krs[:])
        K.S.emit()
    return nc


S = 4096
NQT = S // 128


def nsa_tables():
    c = np.arange(256)
    c_end = 16 * c + 31
    t = np.arange(S)
    cm = (t[None, :] >= c_end[:, None]) & (c[:, None] < 255)
    cmpmask = cm.reshape(2, 128, NQT, 128).transpose(1, 0, 2, 3).astype(np.float32)
    j = np.arange(64)
    c_start = 16 * c
    ovl = ((c_start[:, None] <= j[None, :] * 64 + 63) & (c_end[:, None] >= j[None, :] * 64) & (c[:, None] < 255))
    ovl = ovl.reshape(2, 128, 64).transpose(1, 0, 2).astype(np.float32)
    cur = (t // 64)[:, None]
    forced = (j[None, :] == 0) | (j[None, :] == cur) | (j[None, :] == cur - 1)
    future = j[None, :] > cur
    keep = (~(forced | future)).astype(np.float32)
    fill = np.where(forced, 1e6 + j[None, :] * 16.0, 0.0) + np.where(future, -1e6 - j[None, :] * 16.0, 0.0)
    keep = keep.reshape(NQT, 128, 64).transpose(1, 0, 2)
    fill = fill.reshape(NQT, 128, 64).transpose(1, 0, 2).astype(np.float32)
    E = (np.arange(S)[None, :] // 64 == j[:, None]).astype(np.float32)
    return cmpmask, ovl, np.ascontiguousarray(keep), np.ascontiguousarray(fill), E


class ACtx:
    pass


def attn_head(K, C, qparts, kparts, vaug, vw, klist_fn, scale, bias_fn, extra_fn, epilogue):
    for qt in range(NQT):
        kl = klist_fn(qt)
        po = C.ps_o[C.oc % 2]
        C.oc += 1
        for i, (kt, mk) in enumerate(kl):
            pss = C.ps_s[C.sc % 4]
            pt = C.pT[C.sc % 4]
            C.sc += 1
            ex = extra_fn(kt, qt) if extra_fn else None
            npart = len(qparts)
            for p in range(npart):
                K.mm(pss[:, 0:128], kparts[p][:, kt * 128:(kt + 1) * 128], qparts[p][:, qt * 128:(qt + 1) * 128],
                     start=(p == 0), stop=(p == npart - 1 and ex is None))
            if ex is not None:
                K.mm(pss[:, 0:128], ex[0], ex[1], start=False, stop=True)
            b = bias_fn(kt, qt) if bias_fn else 0.0
            K.act(pt[:], pss[:, 0:128], AF.Exp, bias=b, scale=scale)
            if mk is not None:
                K.tt(pt[:], pt[:], mk, ALU.mult)
            K.mm(po[:, 0:vw], pt[:], vaug[:, kt, 0:vw], start=(i == 0), stop=(i == len(kl) - 1))
        epilogue(qt, po)


def build_b():
    cmpmask_np = nsa_tables()[0]
    nc = bass.Bass("TRN2", target_bir_lowering=False)
    with contextlib.ExitStack() as st:
        K = KB(nc, st)
        I = lambda n, s: K.dram(n, s, F32, "ExternalInput")
        swa_q, swa_k, swa_v = I("swa_qT", [4, 64, S]), I("swa_kT", [64, S]), I("swa_v", [S, 64])
        swa_alb, swa_sink = I("swa_alb", [128, 4, 2]), I("swa_sink", [128, 4])
        fox_q, fox_k, fox_v = I("fox_qT", [2, 128, S]), I("fox_kT", [2, 128, S]), I("fox_v", [2, S, 128])
        fox_fl, fox_fb = I("fox_fl", [2, S]), I("fox_fb", [2, 1])
        mla_qn, mla_qr, mla_kn = I("mla_qnT", [2, 128, S]), I("mla_qrT", [2, 64, S]), I("mla_knT", [2, 128, S])
        mla_kr, mla_v = I("mla_krT", [64, S]), I("mla_v", [2, S, 128])
        nsa_q = I("nsa_qT", [4, 128, S])
        nsa_kc, nsa_vc = I("nsa_kcT", [128, S]), I("nsa_vcT", [128, S])
        nsa_ks, nsa_vs = I("nsa_ksT", [128, S]), I("nsa_vs", [S, 128])
        nsa_kw, nsa_vw = I("nsa_kwT", [128, S]), I("nsa_vw", [S, 128])
        nsa_gl = I("nsa_gl", [S, 6])
        kc_pos, kc_w1, kc_w2 = I("kc_posT", [128, 32]), I("kc_w1", [S, 128]), I("kc_w2", [128, 128])
        vc_pos, vc_w1, vc_w2 = I("vc_posT", [128, 32]), I("vc_w1", [S, 128]), I("vc_w2", [128, 128])
        nsa_alb = I("nsa_alb", [128, 4, NQT])
        cmpb_d = I("cmpb", [128, 4, 2, NQT])
        cmpmask_d = I("cmpmask", [128, 2, NQT, 128])
        ovl_d, keep_d, fill_d, E_d = I("ovl", [128, 2, 64]), I("keep", [128, NQT, 64]), I("fill", [128, NQT, 64]), I("Emat", [64, S])
        mdiag_d, medge_d, ident_d = I("mdiag", [128, 128]), I("medge", [128, 128]), I("ident", [128, 128])
        out_d = K.dram("out", [S, 1024], F32, "ExternalOutput")
        cfox_d = K.dram("cfox", [2, S], F32, "ExternalOutput")

        def cload(name, src, shape, dt=BF16):
            t = K.sb(name, shape, dt)
            K.dma(t[:], src, eng=("gpsimd" if dt == BF16 else "sync"))
            return t
        mdiag = cload("mdiag_s", mdiag_d, [128, 128])
        medge = cload("medge_s", medge_d, [128, 128])
        identf = cload("identf", ident_d, [128, 128], F32)
        cmpmask = cload("cmpmask_s", cmpmask_d, [128, 2, NQT, 128])
        Emat = cload("Emat_s", E_d, [64, S])
        keep = cload("keep_s", keep_d, [128, NQT, 64], F32)
        fill = cload("fill_s", fill_d, [128, NQT, 64], F32)
        cmpb = cload("cmpb_s", cmpb_d, [128, 4, 2, NQT], F32)
        nalb = cload("nalb_s", nsa_alb, [128, 4, NQT], F32)
        salb = cload("salb_s", swa_alb, [128, 4, 2], F32)
        ssink = cload("ssink_s", swa_sink, [128, 4], F32)

        C = ACtx()
        C.oc = 0
        C.sc = 0
        C.ps_s = [K.ps("pss%d" % i, [128, 512], F32) for i in range(4)]
        C.ps_o = [K.ps("pso%d" % i, [128, 512], F32) for i in range(2)]
        C.pT = [K.sb("pT%d" % i, [128, 128], BF16) for i in range(4)]
        psx = [K.ps("psx%d" % i, [128, 512], F32) for i in range(2)]
        qb = [K.sb("qb%d" % i, [128, S], BF16) for i in range(2)]
        kb = [K.sb("kb%d" % i, [128, S], BF16) for i in range(1)] * 2
        q2b = [K.sb("q2b%d" % i, [64, S], BF16) for i in range(1)] * 2
        k2b = K.sb("k2b", [64, S], BF16)
        vb = [K.sb("vb%d" % i, [128, NQT, 129], BF16) for i in range(2)]
        ob = [K.sb("ob%d" % i, [128, NQT, 128], F32) for i in range(1)] * 2
        onsa = [K.sb("onsa%d" % i, [128, NQT, 128], F32) for i in range(2)]
        flt = [onsa[i][0:2].rr("p n d -> p (n d)") for i in range(2)]
        rinv = [K.sb("rinv%d" % i, [128, 1], F32) for i in range(4)]
        cnt = {"r": 0, "h": 0}

        def nr():
            cnt["r"] += 1
            return rinv[cnt["r"] % 4]

        def load_v(vbt, src, dv):
            K.dma(vbt[:, :, 0:dv], src.rr("(n p) d -> p n d", p=128), eng="gpsimd")
            K.memset(vbt[:, :, dv:dv + 1], 1.0)

        def causal(qt):
            return [(kt, (mdiag[:] if kt == qt else None)) for kt in range(qt + 1)]

        def band(nprev):
            def f(qt):
                l = []
                for kt in range(max(0, qt - nprev), qt + 1):
                    l.append((kt, mdiag[:] if kt == qt else (medge[:] if kt == qt - nprev else None)))
                return l
            return f

        def plain_epi(obuf, dv):
            def f(qt, po):
                r = nr()
                K.recip(r[:], po[:, dv:dv + 1])
                K.ts(obuf[:, qt, 0:dv], po[:, 0:dv], r[:, 0:1], None, ALU.mult)
            return f

        def store(obuf, col0, dv):
            K.dma(out_d[:, col0:col0 + dv].rr("(n p) d -> p n d", p=128), obuf[:, :, 0:dv])

        K.dma(k2b[:], swa_k, eng="gpsimd")
        load_v(vb[0], swa_v, 64)
        sinkc = K.sb("sinkc", [128, 4], F32)
        for h in range(4):
            K.act(sinkc[:, h:h + 1], salb[:, h, 0:1], AF.Exp, bias=ssink[:, h:h + 1])
        for h in range(4):
            qt_ = q2b[h % 2]
            K.dma(qt_[:], swa_q[h], eng="gpsimd")
            obuf = ob[cnt["h"] % 2]
            cnt["h"] += 1

            def epi(qt, po, h=h, obuf=obuf):
                r = nr()
                K.tt(r[:], po[:, 64:65], sinkc[:, h:h + 1], ALU.add)
                K.recip(r[:], r[:])
                K.ts(obuf[:, qt, 0:64], po[:, 0:64], r[:, 0:1], None, ALU.mult)
            attn_head(K, C, [qt_], [k2b], vb[0], 65, band(1), 0.125,
                      lambda kt, qt, h=h: salb[:, h, (qt - kt):(qt - kt) + 1], None, epi)
            store(obuf, 768 + h * 64, 64)

        fl, fl2 = flt
        fbn = K.sb("fbn", [2, 1], F32)
        K.dma(fl[:], fox_fl)
        K.dma(fbn[:], fox_fb)
        K.ts(fbn[:], fbn[:], -1.0, None, ALU.mult)
        K.act(fl[:], fl[:], AF.Exp, bias=fbn[:, 0:1], scale=-1.0)
        K.act(fl[:], fl[:], AF.Ln, bias=1.0)
        a, b_ = fl, fl2
        s = 1
        while s < S:
            K.copy(b_[:, 0:s], a[:, 0:s], eng="gpsimd")
            K.tt(b_[:, s:S], a[:, s:S], a[:, 0:S - s], ALU.add)
            a, b_ = b_, a
            s *= 2
        K.dma(cfox_d, a[:])
        cK = K.sb("cK", [128, 2, NQT], F32)
        cR = K.sb("cR", [128, 2, NQT], F32)
        for h in range(2):
            K.dma(cK[:, h, :], cfox_d[h].rr("(n p) -> p n", p=128), allow_slow_non_contiguous=True)
            K.dma(cR[:, h, :], V(cfox_d.ap[h, 127:S:128].partition_broadcast(128), cfox_d.res), allow_slow_non_contiguous=True)
        fbias = [K.sb("fbias%d" % i, [128, NQT], F32) for i in range(2)]
        for h in range(2):
            K.dma(qb[h % 2][:], fox_q[h], eng="gpsimd")
            K.dma(kb[h % 2][:], fox_k[h], eng="gpsimd")
            load_v(vb[(h + 1) % 2], fox_v[h], 128)
            obuf = ob[cnt["h"] % 2]
            cnt["h"] += 1
            fbs = {}

            def fb(kt, qt, h=h, fbs=fbs):
                if qt not in fbs:
                    t = fbias[qt % 2]
                    K.ts(t[:, 0:qt + 1], cK[:, h, 0:qt + 1], cR[:, h, qt:qt + 1], None, ALU.subtract)
                    fbs[qt] = t
                return fbs[qt][:, kt:kt + 1]
            attn_head(K, C, [qb[h % 2]], [kb[h % 2]], vb[(h + 1) % 2], 129, causal, 128 ** -0.5, fb, None,
                      plain_epi(obuf, 128))
            store(obuf, 256 + h * 128, 128)

        K.dma(k2b[:], mla_kr, eng="gpsimd")
        for h in range(2):
            K.dma(qb[h % 2][:], mla_qn[h], eng="gpsimd")
            K.dma(kb[h % 2][:], mla_kn[h], eng="gpsimd")
            K.dma(q2b[h % 2][:], mla_qr[h], eng="gpsimd")
            load_v(vb[(h + 1) % 2], mla_v[h], 128)
            obuf = ob[cnt["h"] % 2]
            cnt["h"] += 1
            attn_head(K, C, [qb[h % 2], q2b[h % 2]], [kb[h % 2], k2b], vb[(h + 1) % 2], 129, causal, 192 ** -0.5,
                      None, None, plain_epi(obuf, 128))
            store(obuf, 512 + h * 128, 128)

        sg = K.sb("sg", [128, NQT, 6], F32)
        K.dma(sg[:], nsa_gl.rr("(n p) c -> p n c", p=128))
        K.act(sg[:], sg[:], AF.Sigmoid)
        kcT = K.sb("kcT", [128, 256], BF16)
        vca = K.sb("vca", [128, 2, 193], BF16)
        w1b = K.sb("w1b", [128, 32, 128], BF16)
        w2b = K.sb("w2b", [128, 128], BF16)
        posT = K.sb("posT", [128, 32], F32)
        win = [K.sb("win%d" % i, [128, 255], BF16) for i in range(3)]
        hsb = K.sb("hsb", [128, 256], F32)
        h2 = K.sb("h2", [128, 256], F32)
        gT = K.sb("gT", [128, 256], BF16)
        K.memset(gT[:], 0.0)
        K.memset(kcT[:], 0.0)
        K.memset(vca[:], 0.0)
        K.dma(vca[:, :, 0:64], ovl_d, eng="gpsimd")
        K.memset(vca[:, 0, 64:65], 1.0)
        K.memset(vca[0:127, 1, 64:65], 1.0)
        for which, srcT, pos_d, w1_d, w2_d in (("k", nsa_kc, kc_pos, kc_w1, kc_w2), ("v", nsa_vc, vc_pos, vc_w1, vc_w2)):
            xT = qb[0]
            K.dma(xT[:], srcT, eng="gpsimd")
            K.dma(posT[:], pos_d)
            K.dma(w1b[:], w1_d.rr("(w d) n -> d w n", d=128), eng="gpsimd")
            K.dma(w2b[:], w2_d, eng="gpsimd")
            ph = psx[0]
            for w in range(32):
                wt = win[w % 3]
                K.ts(wt[:], xT[:, w:w + 16 * 254 + 1:16], posT[:, w:w + 1], None, ALU.add)
                K.mm(ph[:, 0:255], w1b[:, w, :], wt[:], start=(w == 0), stop=(w == 31))
            K.copy(hsb[:, 0:255], ph[:, 0:255])
            K.act(h2[:, 0:255], hsb[:, 0:255], AF.Square)
            K.ts(h2[:, 0:255], h2[:, 0:255], 0.044715, 1.0, ALU.mult, ALU.add)
            K.tt(h2[:, 0:255], h2[:, 0:255], hsb[:, 0:255], ALU.mult)
            K.act(h2[:, 0:255], h2[:, 0:255], AF.Tanh, scale=0.7978845608028654)
            K.ts(h2[:, 0:255], h2[:, 0:255], 1.0, None, ALU.add)
            K.stt(gT[:, 0:255], hsb[:, 0:255], 0.5, h2[:, 0:255], ALU.mult, ALU.mult)
            if which == "k":
                p2 = psx[1]
                K.mm(p2[:, 0:256], w2b[:], gT[:], start=True, stop=True)
                K.copy(kcT[:], p2[:, 0:256])
            else:
                for ct in range(2):
                    p2 = psx[1]
                    K.mm(p2[:, 0:128], gT[:, ct * 128:(ct + 1) * 128], w2b[:], start=True, stop=True)
                    K.copy(vca[:, ct, 65:193], p2[:, 0:128])
        imp = K.sb("imp", [128, NQT, 64], F32)

        def cmp_klist(qt):
            l = []
            for ct in range(2):
                m = cmpmask_np[:, ct, qt, :]
                if not m.any():
                    continue
                l.append((ct, None if m.all() else cmpmask[:, ct, qt, :]))
            return l
        for h in range(4):
            K.dma(qb[(h + 1) % 2][:], nsa_q[h], eng="gpsimd")

            def epi(qt, po, h=h):
                r = nr()
                K.ts(r[:], po[:, 64:65], 1e-30, None, ALU.max)
                K.recip(r[:], r[:])
                if h == 0:
                    K.ts(imp[:, qt, :], po[:, 0:64], r[:, 0:1], None, ALU.mult)
                else:
                    K.stt(imp[:, qt, :], po[:, 0:64], r[:, 0:1], imp[:, qt, :], ALU.mult, ALU.add)
                if h < 2:
                    K.tt(r[:], r[:], sg[:, qt, h:h + 1], ALU.mult)
                    K.ts(onsa[h][:, qt, :], po[:, 65:193], r[:, 0:1], None, ALU.mult)
            attn_head(K, C, [qb[(h + 1) % 2]], [kcT], vca, 193, cmp_klist, 128 ** -0.5,
                      lambda kt, qt, h=h: cmpb[:, h, kt, qt:qt + 1], None, epi)
        K.tt(imp[:], imp[:], keep[:], ALU.mult)
        K.tt(imp[:], imp[:], fill[:], ALU.add)
        selbT = K.sb("selbT", [64, S], BF16)
        m8 = [K.sb("m8_%d" % i, [128, 16], F32) for i in range(2)]
        wk = [K.sb("wk%d" % i, [128, 64], F32) for i in range(2)]
        sel = [K.sb("sel%d" % i, [128, 64], F32) for i in range(2)]
        for qt in range(NQT):
            m = m8[qt % 2]
            w_ = wk[qt % 2]
            sl = sel[qt % 2]
            K.max8(m[:, 0:8], imp[:, qt, :])
            K.match_replace(w_[:], m[:, 0:8], imp[:, qt, :], -3.0e6)
            K.max8(m[:, 8:16], w_[:])
            K.ts(sl[:], imp[:, qt, :], m[:, 15:16], None, ALU.is_ge)
            K.ts(sl[:], sl[:], BIG, -BIG, ALU.mult, ALU.add)
            pt_ = psx[qt % 2]
            K.tr(pt_[0:64, 0:128], sl[:], identf[:])
            K.copy(selbT[:, qt * 128:(qt + 1) * 128], pt_[0:64, 0:128], eng="scalar")
        for h in range(2):
            K.dma(qb[h % 2][:], nsa_q[h], eng="gpsimd")
            for br, kT_d, v_d, klf in ((1, nsa_ks, nsa_vs, causal), (2, nsa_kw, nsa_vw, band(4))):
                kbt = kb[br % 2]
                vbt = vb[br % 2]
                K.dma(kbt[:], kT_d, eng="gpsimd")
                load_v(vbt, v_d, 128)

                def epi(qt, po, h=h, br=br):
                    r = nr()
                    K.recip(r[:], po[:, 128:129])
                    K.tt(r[:], r[:], sg[:, qt, br * 2 + h:br * 2 + h + 1], ALU.mult)
                    K.stt(onsa[h][:, qt, :], po[:, 0:128], r[:, 0:1], onsa[h][:, qt, :], ALU.mult, ALU.add)
                ex = None
                if br == 1:
                    ex = lambda kt, qt: (Emat[:, kt * 128:(kt + 1) * 128], selbT[:, qt * 128:(qt + 1) * 128])
                attn_head(K, C, [qb[h % 2]], [kbt], vbt, 129, klf, 128 ** -0.5,
                          lambda kt, qt, h=h: nalb[:, h, (qt - kt):(qt - kt) + 1], ex, epi)
            store(onsa[h], h * 128, 128)
        K.S.emit()
    return nc


def _c(a):
    return np.ascontiguousarray(a, dtype=np.float32)


def b_const_inputs():
    cmpmask, ovl, keep, fill, E = nsa_tables()
    i = np.arange(128)
    return dict(cmpmask=_c(cmpmask), ovl=_c(ovl), keep=_c(keep), fill=_c(fill), Emat=_c(E),
                mdiag=_c(i[:, None] <= i[None, :]), medge=_c(i[:, None] > i[None, :]), ident=np.eye(128, dtype=np.float32))


def b_core_inputs(proj, q_mla, kv_mla, kro, P, l, hq):
    T = lambda a: _c(np.asarray(a).T)
    d = {}
    i = np.arange(128, dtype=np.float64)
    g = hq // 2
    heads = [4 * hq + r for r in range(4)]
    d["swa_qT"] = _c(np.stack([proj[:, 7008 + h * 64:7008 + (h + 1) * 64].T for h in heads]))
    d["swa_kT"] = T(proj[:, 8032 + g * 64:8032 + (g + 1) * 64])
    d["swa_v"] = _c(proj[:, 8032 + (2 + g) * 64:8032 + (3 + g) * 64])
    sl = np.array([2.0 ** (-(h + 1) / 2.0) for h in heads])
    d["swa_alb"] = _c(sl[None, :, None] * (i[:, None, None] - 63.5 - 128.0 * np.arange(2)[None, None, :]))
    d["swa_sink"] = _c(np.tile(P["swa_sinks"][l][heads][None, :], (128, 1)))
    fh = [2 * hq, 2 * hq + 1]
    bq = lambda w, h: proj[:, 2584 + (w * 8 + h) * 128:2584 + (w * 8 + h + 1) * 128]
    d["fox_qT"] = _c(np.stack([bq(0, h).T for h in fh]))
    d["fox_kT"] = _c(np.stack([bq(1, h).T for h in fh]))
    d["fox_v"] = _c(np.stack([bq(2, h) for h in fh]))
    d["fox_fl"] = _c(np.stack([proj[:, 5656 + h] for h in fh]))
    d["fox_fb"] = _c(P["fox_f_bias"][l][fh][:, None])
    q3 = q_mla.reshape(S, 8, 192)
    kv3 = kv_mla.reshape(S, 8, 256)
    d["mla_qnT"] = _c(np.stack([q3[:, h, 0:128].T for h in fh]))
    d["mla_qrT"] = _c(np.stack([q3[:, h, 128:192].T for h in fh]))
    d["mla_knT"] = _c(np.stack([kv3[:, h, 0:128].T for h in fh]))
    d["mla_krT"] = T(kro)
    d["mla_v"] = _c(np.stack([kv3[:, h, 128:256] for h in fh]))
    mine = [2 * hq, 2 * hq + 1]
    nh = mine + [h for h in range(4 * g, 4 * g + 4) if h not in mine]
    d["nsa_qT"] = _c(np.stack([proj[:, h * 128:(h + 1) * 128].T for h in nh]))
    akv = lambda br, kvi: proj[:, 1024 + ((br * 2 + kvi) * 2 + g) * 128:1024 + ((br * 2 + kvi) * 2 + g + 1) * 128]
    d["nsa_kcT"], d["nsa_vcT"] = T(akv(0, 0)), T(akv(0, 1))
    d["nsa_ksT"], d["nsa_vs"] = T(akv(1, 0)), _c(akv(1, 1))
    d["nsa_kwT"], d["nsa_vw"] = T(akv(2, 0)), _c(akv(2, 1))
    d["nsa_gl"] = _c(np.stack([proj[:, 2560 + br * 8 + h] for br in range(3) for h in mine], axis=1))
    for nm in ("kc", "vc"):
        d[nm + "_posT"] = T(P["nsa_%s_pos" % nm][l])
        d[nm + "_w1"] = _c(P["nsa_%s_w1" % nm][l])
        d[nm + "_w2"] = _c(P["nsa_%s_w2" % nm][l])
    nsl = np.array([2.0 ** (-(h + 1)) for h in nh])
    d["nsa_alb"] = _c(nsl[None, :, None] * (i[:, None, None] - 63.5 - 128.0 * np.arange(NQT)[None, None, :]))
    cend = 16.0 * (np.arange(2)[None, :, None] * 128 + i[:, None, None]) + 31.0
    tref = np.arange(NQT)[None, None, :] * 128 + 63.5
    cb = nsl[None, :, None, None] * (cend - tref)[:, None, :, :]
    d["cmpb"] = _c(np.minimum(cb, 40.0))
    return d


def build_d():
    nc = bass.Bass("TRN2", target_bir_lowering=False)
    with contextlib.ExitStack() as st:
        K = KB(nc, st)
        I = lambda n, s, dt=F32: K.dram(n, s, dt, "ExternalInput")
        mg_d, rt_d, g8_d = I("mg", [NTOK, 8]), I("rtall", [NTOK, 4]), I("g8", [1])
        xn2_d = I("xn2", [NTOK, D], BF16)
        gffn_d = I("gffn", [D])
        wg_d, wu_d, wd_d = I("wg", [8, D, 384]), I("wu", [8, D, 384]), I("wd", [8, 384, D])
        tri_d, io384_d, pn_d = I("tri", [128, 128]), I("iota384", [128, 384]), I("pn", [128, NTT, 2])
        ident_d, io8_d = I("ident", [128, 128]), I("iota8", [128, 8])
        y_d = K.dram("y", [8 * CE, D], F32, "ExternalOutput")
        dl_d = K.dram("destl", [NTOK, 2], F32, "ExternalOutput")

        identf = K.sb("identf", [128, 128], F32)
        identb = K.sb("identb", [128, 128], BF16)
        trif = K.sb("trif", [128, 128], F32)
        trib = K.sb("trib", [128, 128], BF16)
        oneb = K.sb("oneb", [128, 128], BF16)
        io384 = K.sb("io384", [128, 384], F32)
        io8 = K.sb("io8", [128, 8], F32)
        pnf = K.sb("pnf", [128, NTT, 2], F32)
        pnb = K.sb("pnb", [128, NTT, 2], BF16)
        g2col = K.sb("g2col", [128, 32], F32)
        g8 = K.sb("g8s", [128, 1], F32)
        Mt = K.sb("Mt", [128, NTT, 8], F32)
        Mb = K.sb("Mb", [128, NTT, 8], BF16)
        Rf = K.sb("Rf", [128, NTT, 8], F32)
        Rb = K.sb("Rb", [128, NTT, 8], BF16)
        slot = K.sb("slot", [128, NTT, 8], F32)
        oh = K.sb("ohd", [128, NTT, 8], F32)
        rt = K.sb("rt", [128, NTT, 4], F32)
        rel = K.sb("rel", [128, NTT], F32)
        sl = K.sb("sl", [128, NTT], F32)
        dls = K.sb("dls", [128, NTT, 2], F32)
        OHb = [K.sb("OHb%d" % i, [128, 384], BF16) for i in range(4)]
        idxf = K.sb("idxf", [128, 3, 8, 2], F32)
        idxv = K.sb("idxv", [128, 3, 8], F32)
        idxi = K.sb("idxi", [128, 3, 8], I32)
        B = [K.ps("B%d" % i, [128, 512], F32) for i in range(4)]
        tpb = [K.ps("tpb%d" % i, [128, 8, 128], BF16) for i in range(2)]
        psy = [K.ps("psy%d" % i, [128, 512], F32) for i in range(2)]

        K.dma(identf[:], ident_d)
        K.copy(identb[:], identf[:])
        K.dma(trif[:], tri_d)
        K.copy(trib[:], trif[:])
        K.memset(oneb[:], 1.0)
        K.dma(io384[:], io384_d)
        K.dma(io8[:], io8_d)
        K.dma(pnf[:], pn_d)
        K.copy(pnb[:], pnf[:])
        K.dma(g2col[:], col_view(gffn_d), allow_slow_non_contiguous=True)
        K.dma(g8[:], V(g8_d.ap.partition_broadcast(128), g8_d.res))
        K.dma(Mt[:], mg_d.rr("(n p) e -> p n e", p=128))
        K.dma(rt[:], rt_d.rr("(n p) k -> p n k", p=128))
        K.copy(Mb[:], Mt[:])
        K.memset(Rf[:, 0, :], 0.0)
        for n in range(1, NTT):
            K.tt(Rf[:, n, :], Rf[:, n - 1, :], Mt[:, n - 1, :], ALU.add)
        K.copy(Rb[:], Rf[:])
        sv = B[0][:].rr("p (n e) -> p n e", e=8)
        for n in range(NTT):
            K.mm(sv[:, n, :], trib[:], Mb[:, n, :], start=True, stop=False)
            K.mm(sv[:, n, :], oneb[:], Rb[:, n, :], start=False, stop=True)
        K.copy(slot[:], sv)
        io8_3 = io8[:].un(1).bc([128, NTT, 8])
        for k in range(2):
            K.ts(rel[:], rt[:, :, k], g8[:, 0:1], None, ALU.subtract)
            K.tt(oh[:], io8_3, rel[:].un(2).bc([128, NTT, 8]), ALU.is_equal)
            K.tt(oh[:], oh[:], slot[:], ALU.mult)
            K.reduce(sl[:], oh[:], ALU.add)
            K.stt(dls[:, :, k], rel[:], float(CE), sl[:], ALU.mult, ALU.add)
        K.dma(dl_d.rr("(n p) k -> p n k", p=128), dls[:])
        psi = [B[1 + s_][:, 0:16].rr("p (e k) -> p e k", k=2) for s_ in range(3)]
        oc = 0
        for e in range(8):
            for n in range(NTT):
                o_ = OHb[oc % 4]
                oc += 1
                K.ts(o_[:], io384[:], slot[:, n, e:e + 1], Mt[:, n, e:e + 1], ALU.is_equal, ALU.mult)
                for s_ in range(3):
                    K.mm(psi[s_][:, e, :], o_[:, s_ * 128:(s_ + 1) * 128], pnb[:, n, :], start=(n == 0), stop=(n == NTT - 1))
        for s_ in range(3):
            K.copy(idxf[:, s_, :, :], psi[s_])
        K.stt(idxv[:], idxf[:, :, :, 1], 128.0, idxf[:, :, :, 0], ALU.mult, ALU.add)
        K.copy(idxi[:], idxv[:])
        XeT = K.sb("XeT", [128, 32, CE], BF16)
        wgb = [K.sb("wgb%d" % i, [128, 32, 384], BF16) for i in range(2)]
        wub = [K.sb("wub%d" % i, [128, 32, 384], BF16) for i in range(2)]
        wdb = K.sb("wdb", [128, 3, D], BF16)
        xg = [K.sb("xg%d" % i, [128, D], BF16) for i in range(2)]
        ysb = K.sb("ysb", [128, D], F32)
        actT = K.sb("actT", [128, 3, CE], BF16)
        sil = [K.sb("sil%d" % i, [128, CE], F32) for i in range(2)]
        gc = 0
        for e in range(8):
            K.dma(wgb[e % 2][:], wg_d[e].rr("(c p) f -> p c f", p=128), eng="gpsimd")
            K.dma(wub[e % 2][:], wu_d[e].rr("(c p) f -> p c f", p=128), eng="gpsimd")
            for s_ in range(3):
                x_ = xg[gc % 2]
                gc += 1
                K.gather(x_[:], xn2_d, idxi[:, s_, e:e + 1])
                for k in range(4):
                    tp = tpb[k % 2]
                    for c in range(8):
                        cc = k * 8 + c
                        K.tr(tp[:, c, :], x_[:, cc * 128:(cc + 1) * 128], identb[:])
                    K.tt(XeT.sub(k, (slice(None), slice(k * 8, k * 8 + 8), slice(s_ * 128, (s_ + 1) * 128))),
                         tp[:], g2col[:, k * 8:k * 8 + 8].un(2).bc([128, 8, 128]), ALU.mult)
            K.dma(wdb[:], wd_d[e].rr("(c p) n -> p c n", p=128), eng="gpsimd")
            for f in range(3):
                pg, pu = B[f % 2], B[2 + f % 2]
                for c in range(32):
                    K.mm(pg[:, 0:CE], wgb[e % 2][:, c, f * 128:(f + 1) * 128],
                         XeT.sub(c // 8, (slice(None), c, slice(None))), start=(c == 0), stop=(c == 31))
                for c in range(32):
                    K.mm(pu[:, 0:CE], wub[e % 2][:, c, f * 128:(f + 1) * 128],
                         XeT.sub(c // 8, (slice(None), c, slice(None))), start=(c == 0), stop=(c == 31))
                K.act(sil[f % 2][:], pg[:, 0:CE], AF.Silu)
                K.tt(actT[:, f, :], sil[f % 2][:], pu[:, 0:CE], ALU.mult)
            for s_ in range(3):
                for j in range(8):
                    py = psy[j % 2]
                    for f in range(3):
                        K.mm(py[:], actT[:, f, s_ * 128:(s_ + 1) * 128], wdb[:, f, j * 512:(j + 1) * 512],
                             start=(f == 0), stop=(f == 2))
                    K.copy(ysb[:, j * 512:(j + 1) * 512], py[:], eng=("scalar" if j % 2 else "vector"))
                r0 = e * CE + s_ * 128
                K.dma(y_d[r0:r0 + 128, :], ysb[:])
        K.S.emit()
    return nc


def build_e(final):
    nc = bass.Bass("TRN2", target_bir_lowering=False)
    with contextlib.ExitStack() as st:
        K = KB(nc, st)
        I = lambda n, s, dt=F32: K.dram(n, s, dt, "ExternalInput")
        xm_d, y_d = I("xmid", [NT, D]), I("yall", [64 * CE, D])
        dl8_d, rt_d = I("destl8", [8, NT, 2]), I("rt", [NT, 4])
        lo8_d, base8_d, gfin_d = I("lo8", [128, 8]), I("base8", [128, 8]), I("gfin", [D])
        out_d = K.dram("xout", [NT, D], F32, "ExternalOutput")
        lo8 = K.sb("lo8s", [128, 8], F32)
        base8 = K.sb("base8s", [128, 8], F32)
        K.dma(lo8[:], lo8_d)
        K.dma(base8[:], base8_d)
        gb = K.sb("gb", [128, D], F32)
        if final:
            K.dma(gb[:], V(gfin_d.ap.partition_broadcast(128), gfin_d.res))
        xm = K.sb("xm", [128, D], F32)
        ya = K.sb("ya", [128, D], F32)
        yb = K.sb("yb", [128, D], F32)
        acc = K.sb("acc", [128, D], F32)
        junk = K.sb("junk", [128, D], BF16)
        rts = K.sb("rts", [128, 4], F32)
        dl8 = K.sb("dl8", [128, 8, 2], F32)
        g1 = K.sb("g1", [128, 8], F32)
        g2 = K.sb("g2", [128, 8], F32)
        tmp8 = K.sb("tmp8", [128, 8], F32)
        df = K.sb("df", [128, 2], F32)
        di = K.sb("di", [128, 2], I32)
        ss = K.sb("ss", [128, 1], F32)
        tmp = K.sb("tmp", [128, 1], F32)
        rs = K.sb("rs", [128, 1], F32)
        for n in range(NT // 128):
            r0 = n * 128
            K.dma(xm[:], xm_d[r0:r0 + 128, :])
            K.dma(rts[:], rt_d[r0:r0 + 128, :])
            K.dma(dl8[:], dl8_d[:, r0:r0 + 128, :].rr("g t k -> t g k"))
            K.ts(g1[:], lo8[:], rts[:, 0:1], None, ALU.is_le)
            K.ts(g2[:], lo8[:], 8.0, rts[:, 0:1], ALU.add, ALU.is_gt)
            K.tt(g1[:], g1[:], g2[:], ALU.mult)
            for k in range(2):
                K.tt(tmp8[:], dl8[:, :, k], base8[:], ALU.add)
                K.tt(tmp8[:], tmp8[:], g1[:], ALU.mult)
                K.reduce(df[:, k:k + 1], tmp8[:], ALU.add)
            K.copy(di[:], df[:])
            K.gather(ya[:], y_d, di[:, 0:1])
            K.gather(yb[:], y_d, di[:, 1:2])
            K.stt(acc[:], ya[:], rts[:, 2:3], xm[:], ALU.mult, ALU.add)
            K.stt(acc[:], yb[:], rts[:, 3:4], acc[:], ALU.mult, ALU.add)
            if final:
                K.act(junk[:], acc[:], AF.Square, accum=ss[:])
                K.rstd(rs[:], ss[:], float(D), tmp[:])
                K.stt(acc[:], acc[:], rs[:, 0:1], gb[:], ALU.mult, ALU.mult)
            K.dma(out_d[r0:r0 + 128, :], acc[:])
        K.S.emit()
    return nc


_PROGS = {}
_DBG = None
_DBG_STOP = False


def _prog(name, fn):
    if name not in _PROGS:
        _PROGS[name] = fn()
    return _PROGS[name]


def _run(name, fn, in_maps):
    res = run_bass_kernel_spmd(_prog(name, fn), in_maps, core_ids=list(range(8)))
    return res.results


CE2 = 1024
CG = 4096


def build_d2():
    nc = bass.Bass("TRN2", target_bir_lowering=False)
    NS = CE2 // 128
    with contextlib.ExitStack() as st:
        K = KB(nc, st)
        I = lambda n, s, dt=F32: K.dram(n, s, dt, "ExternalInput")
        mg_d, rt_d, g8_d = I("mg", [NTOK, 8]), I("rtall", [NTOK, 4]), I("g8", [1])
        xn2_d = I("xn2", [NTOK, D], BF16)
        gffn_d = I("gffn", [D])
        wg_d, wu_d, wd_d = I("wg", [8, D, 384]), I("wu", [8, D, 384]), I("wd", [8, 384, D])
        tri_d, tok_d = I("tri", [128, 128]), I("tokid", [128, NTT])
        ident_d, io8_d, lo8_d = I("ident", [128, 128]), I("iota8", [128, 8]), I("lo8", [128, 8])
        tr1_d, tr2_d = I("trash1", [128, 1]), I("trash2", [128, 1])
        y_d = K.dram("y", [8 * CE2, D], F32, "ExternalOutput")
        z_d = K.dram("z", [CG, D], F32, "ExternalOutput")
        inv_d = K.dram("inv", [128, NTT], I32, "ExternalOutput")
        info_d = K.dram("info", [NTOK, 4], F32, "ExternalOutput")
        idx_d = K.dram("idxl", [8 * CE2 + 128, 2], I32, "ExternalOutput")
        tl_d = K.dram("tokl", [CG + 128, 2], I32, "ExternalOutput")

        identf = K.sb("identf", [128, 128], F32)
        identb = K.sb("identb", [128, 128], BF16)
        trif = K.sb("trif", [128, 128], F32)
        trib = K.sb("trib", [128, 128], BF16)
        oneb = K.sb("oneb", [128, 128], BF16)
        io8 = K.sb("io8", [128, 8], F32)
        lo8 = K.sb("lo8s", [128, 8], F32)
        tokf = K.sb("tokf", [128, NTT], F32)
        toki = K.sb("toki", [128, NTT, 2], I32)
        g2col = K.sb("g2col", [128, 32], F32)
        g8 = K.sb("g8s", [128, 1], F32)
        tr1 = K.sb("tr1", [128, 1], F32)
        tr2 = K.sb("tr2", [128, 1], F32)
        Mt = K.sb("Mt", [128, NTT, 8], F32)
        Mb = K.sb("Mb", [128, NTT, 8], BF16)
        Rf = K.sb("Rf", [128, NTT, 8], F32)
        Rb = K.sb("Rb", [128, NTT, 8], BF16)
        slot = K.sb("slot", [128, NTT, 8], F32)
        G8 = K.sb("G8", [128, NTT, 8], F32)
        posa = K.sb("posa", [128, NTT, 8], F32)
        oh = K.sb("ohd", [128, NTT, 8], F32)
        oh2 = K.sb("ohd2", [128, NTT, 8], F32)
        rt = K.sb("rt", [128, NTT, 4], F32)
        rel = K.sb("rel", [128, NTT], F32)
        sl = K.sb("sl", [128, NTT], F32)
        okk = K.sb("okk", [128, NTT], F32)
        ok2 = K.sb("ok2", [128, NTT], F32)
        ing = K.sb("ing", [128, NTT], F32)
        dtmp = K.sb("dtmp", [128, NTT], F32)
        info = K.sb("infos", [128, NTT, 4], F32)
        di = [K.sb("di%d" % k, [128, NTT], I32) for k in range(3)]
        invf = K.sb("invf", [128, NTT], F32)
        invi = K.sb("invi", [128, NTT], I32)
        zt = K.sb("zt", [128, (8 * CE2 + 128) // 128, 2], I32)
        pgp = K.ps("pgp", [128, CE2], F32)
        pup = K.ps("pup", [128, CE2], F32)
        tpb = [K.ps("tpb%d" % i, [128, 8, 128], BF16) for i in range(2)]
        psy = [K.ps("psy%d" % i, [128, 512], F32) for i in range(2)]

        K.dma(identf[:], ident_d)
        K.copy(identb[:], identf[:])
        K.dma(trif[:], tri_d)
        K.copy(trib[:], trif[:])
        K.memset(oneb[:], 1.0)
        K.dma(io8[:], io8_d)
        K.dma(lo8[:], lo8_d)
        K.dma(tokf[:], tok_d)
        K.copy(toki[:, :, 0], tokf[:])
        K.copy(toki[:, :, 1], tokf[:])
        K.dma(tr1[:], tr1_d)
        K.dma(tr2[:], tr2_d)
        K.dma(g2col[:], col_view(gffn_d), allow_slow_non_contiguous=True)
        K.dma(g8[:], V(g8_d.ap.partition_broadcast(128), g8_d.res))
        K.dma(Mt[:], mg_d.rr("(n p) e -> p n e", p=128))
        K.dma(rt[:], rt_d.rr("(n p) k -> p n k", p=128))
        K.memset(zt[:], 0)
        K.dma(idx_d.rr("(s p) o -> p s o", p=128), zt[:])
        K.dma(tl_d.rr("(s p) o -> p s o", p=128), zt[:, 0:(CG + 128) // 128, :])

        def excl_cumsum(dst, src_f, pv):
            K.copy(Mb[:], src_f)
            K.memset(Rf[:, 0, :], 0.0)
            for n in range(1, NTT):
                K.tt(Rf[:, n, :], Rf[:, n - 1, :], src_f[:, n - 1, :], ALU.add)
            K.copy(Rb[:], Rf[:])
            for n in range(NTT):
                K.mm(pv[:, n, :], trib[:], Mb[:, n, :], start=True, stop=False)
                K.mm(pv[:, n, :], oneb[:], Rb[:, n, :], start=False, stop=True)
            K.copy(dst, pv)
        sv = psy[0][:].rr("p (n e) -> p n e", e=8)
        excl_cumsum(slot[:], Mt[:], sv)
        io8_3 = io8[:].un(1).bc([128, NTT, 8])
        lo8_3 = lo8[:].un(1).bc([128, NTT, 8])
        ea3 = rt[:, :, 0].un(2).bc([128, NTT, 8])
        K.tt(G8[:], lo8_3, ea3, ALU.is_le)
        K.ts(oh[:], lo8_3, 8.0, None, ALU.add)
        K.tt(oh[:], oh[:], ea3, ALU.is_gt)
        K.tt(G8[:], G8[:], oh[:], ALU.mult)
        sv2 = psy[1][:].rr("p (n e) -> p n e", e=8)
        excl_cumsum(posa[:], G8[:], sv2)
        K.tt(oh[:], G8[:], posa[:], ALU.mult)
        K.reduce(sl[:], oh[:], ALU.add)
        K.tt(oh[:], G8[:], io8_3, ALU.mult)
        K.reduce(dtmp[:], oh[:], ALU.add)
        K.stt(invf[:], dtmp[:], float(CG), sl[:], ALU.mult, ALU.add)
        K.copy(invi[:], invf[:])
        K.dma(inv_d, invi[:])
        K.ts(rel[:], rt[:, :, 0], g8[:, 0:1], None, ALU.subtract)
        K.ts(ing[:], rel[:], 0.0, None, ALU.is_ge)
        K.ts(ok2[:], rel[:], 8.0, None, ALU.is_lt)
        K.tt(ing[:], ing[:], ok2[:], ALU.mult)
        K.ts(ok2[:], sl[:], float(CG), None, ALU.is_lt)
        K.tt(ok2[:], ok2[:], ing[:], ALU.mult)
        K.ts(dtmp[:], sl[:], tr2[:, 0:1], None, ALU.subtract)
        K.tt(dtmp[:], dtmp[:], ok2[:], ALU.mult)
        K.ts(dtmp[:], dtmp[:], tr2[:, 0:1], None, ALU.add)
        K.copy(di[2][:], dtmp[:])
        for k in range(2):
            K.ts(rel[:], rt[:, :, k], g8[:, 0:1], None, ALU.subtract)
            K.tt(oh2[:], io8_3, rel[:].un(2).bc([128, NTT, 8]), ALU.is_equal)
            K.tt(oh2[:], oh2[:], slot[:], ALU.mult)
            K.reduce(sl[:], oh2[:], ALU.add)
            K.stt(info[:, :, k], rel[:], float(CE2), sl[:], ALU.mult, ALU.add)
            K.copy(info[:, :, 2 + k], rt[:, :, 2 + k])
            K.ts(okk[:], sl[:], float(CE2), None, ALU.is_lt)
            K.tt(okk[:], okk[:], ing[:], ALU.mult)
            K.ts(dtmp[:], info[:, :, k], tr1[:, 0:1], None, ALU.subtract)
            K.tt(dtmp[:], dtmp[:], okk[:], ALU.mult)
            K.ts(dtmp[:], dtmp[:], tr1[:, 0:1], None, ALU.add)
            K.copy(di[k][:], dtmp[:])
        K.dma(info_d.rr("(n p) k -> p n k", p=128), info[:])
        for n in range(NTT):
            for k in range(2):
                K.scatter(idx_d, di[k][:, n:n + 1], toki[:, n, :])
            K.scatter(tl_d, di[2][:, n:n + 1], toki[:, n, :])
        XeT = K.sb("XeT", [128, 32, CE2], BF16)
        wgb = K.sb("wgb", [128, 32, 384], BF16)
        wub = K.sb("wub", [128, 32, 384], BF16)
        wdb = K.sb("wdb", [128, 3, D], BF16)
        xg = [K.sb("xg%d" % i, [128, D], BF16) for i in range(2)]
        ysb = K.sb("ysb", [128, D], F32)
        actT = K.sb("actT", [128, 3, CE2], BF16)
        sil = K.sb("sil", [128, CE2], F32)
        idxt = K.sb("idxt", [128, NS, 2], I32)
        gc = 0
        for e in range(8):
            K.dma(wgb[:], wg_d[e].rr("(c p) f -> p c f", p=128), eng="gpsimd")
            K.dma(wub[:], wu_d[e].rr("(c p) f -> p c f", p=128), eng="gpsimd")
            K.dma(idxt[:], idx_d[e * CE2:(e + 1) * CE2, :].rr("(s p) o -> p s o", p=128))
            for s_ in range(NS):
                x_ = xg[gc % 2]
                gc += 1
                K.gather(x_[:], xn2_d, idxt[:, s_, 0:1])
                for k in range(4):
                    tp = tpb[k % 2]
                    for c in range(8):
                        cc = k * 8 + c
                        K.tr(tp[:, c, :], x_[:, cc * 128:(cc + 1) * 128], identb[:])
                    K.tt(XeT.sub(k, (slice(None), slice(k * 8, k * 8 + 8), slice(s_ * 128, (s_ + 1) * 128))),
                         tp[:], g2col[:, k * 8:k * 8 + 8].un(2).bc([128, 8, 128]), ALU.mult)
            K.dma(wdb[:], wd_d[e].rr("(c p) n -> p c n", p=128), eng="gpsimd")
            for f in range(3):
                for (wb_, pp) in ((wgb, pgp), (wub, pup)):
                    for hh in range(CE2 // 512):
                        for c in range(32):
                            K.mm(pp.sub(hh, (slice(None), slice(hh * 512, (hh + 1) * 512))), wb_[:, c, f * 128:(f + 1) * 128],
                                 XeT.sub(c // 8, (slice(None), c, slice(hh * 512, (hh + 1) * 512))), start=(c == 0), stop=(c == 31))
                for hh in range(CE2 // 512):
                    hs = slice(hh * 512, (hh + 1) * 512)
                    K.act(sil[:, hs], pgp.sub(hh, (slice(None), hs)), AF.Silu)
                    K.tt(actT[:, f, hs], sil[:, hs], pup.sub(hh, (slice(None), hs)), ALU.mult)
            for s_ in range(NS):
                for j in range(8):
                    py = psy[j % 2]
                    for f in range(3):
                        K.mm(py[:], actT[:, f, s_ * 128:(s_ + 1) * 128], wdb[:, f, j * 512:(j + 1) * 512],
                             start=(f == 0), stop=(f == 2))
                    K.copy(ysb[:, j * 512:(j + 1) * 512], py[:], eng=("scalar" if j % 2 else "vector"))
                r0 = e * CE2 + s_ * 128
                K.dma(y_d[r0:r0 + 128, :], ysb[:])
        tlt = K.sb("tlt", [128, CG // 128, 2], I32)
        inf = [K.sb("inf%d" % i, [128, 4], F32) for i in range(2)]
        ii = [K.sb("ii%d" % i, [128, 2], I32) for i in range(2)]
        ya = V(XeT.h[:, 0:8, :].rearrange("p c t -> p (c t)").bitcast(F32), XeT.sub(0).res)
        yb = ysb
        K.dma(tlt[:], tl_d[0:CG, :].rr("(s p) o -> p s o", p=128))
        for j in range(CG // 128):
            f_ = inf[j % 2]
            i_ = ii[j % 2]
            K.gather(f_[:], info_d, tlt[:, j, 0:1])
            K.ts(f_[:, 0:2], f_[:, 0:2], 0.0, float(8 * CE2 - 1), ALU.max, ALU.min)
            K.copy(i_[:], f_[:, 0:2])
            K.gather(ya[:], y_d, i_[:, 0:1])
            K.gather(yb[:], y_d, i_[:, 1:2])
            K.ts(ya[:], ya[:], f_[:, 2:3], None, ALU.mult)
            K.stt(ya[:], yb[:], f_[:, 3:4], ya[:], ALU.mult, ALU.add)
            K.dma(z_d[j * 128:(j + 1) * 128, :], ya[:])
        K.S.emit()
    return nc


def build_f():
    nc = bass.Bass("TRN2", target_bir_lowering=False)
    with contextlib.ExitStack() as st:
        K = KB(nc, st)
        x_d = K.dram("x", [NT, D], F32, "ExternalInput")
        xa_d = K.dram("xadd", [NT, D], F32, "ExternalInput")
        g_d = K.dram("g", [D], F32, "ExternalInput")
        o_d = K.dram("xout", [NT, D], F32, "ExternalOutput")
        gb = K.sb("gb", [128, D], F32)
        K.dma(gb[:], V(g_d.ap.partition_broadcast(128), g_d.res))
        xa = [K.sb("xa%d" % i, [128, D], F32) for i in range(2)]
        xb = [K.sb("xb%d" % i, [128, D], F32) for i in range(2)]
        junk = K.sb("junk", [128, D], BF16)
        ss = [K.sb("ss%d" % i, [128, 1], F32) for i in range(2)]
        tmp = [K.sb("tmp%d" % i, [128, 1], F32) for i in range(2)]
        rs = [K.sb("rs%d" % i, [128, 1], F32) for i in range(2)]
        for n in range(NT // 128):
            a, b = xa[n % 2], xb[n % 2]
            K.dma(a[:], x_d[n * 128:(n + 1) * 128, :])
            K.dma(b[:], xa_d[n * 128:(n + 1) * 128, :])
            K.tt(a[:], a[:], b[:], ALU.add)
            K.act(junk[:], a[:], AF.Square, accum=ss[n % 2][:])
            K.rstd(rs[n % 2][:], ss[n % 2][:], float(D), tmp[n % 2][:])
            K.stt(b[:], a[:], rs[n % 2][:, 0:1], gb[:], ALU.mult, ALU.mult)
            K.dma(o_d[n * 128:(n + 1) * 128, :], b[:])
        K.S.emit()
    return nc


def kernel(**P):
    P = {k: np.asarray(v) for k, v in P.items()}
    xmid = _c(P["x"]).reshape(NTOK, D)
    moe = np.zeros((NTOK, D), np.float32)
    ident = np.eye(128, dtype=np.float32)
    i128 = np.arange(128)
    iota64 = _c(np.tile(np.arange(64)[None, :], (128, 1)))
    tri = _c(np.triu(np.ones((128, 128)), 1))
    iota8 = _c(np.tile(np.arange(8)[None, :], (128, 1)))
    lo8 = _c(iota8 * 8.0)
    tokid = _c(np.arange(NTT)[None, :] * 128 + i128[:, None])
    trash1 = _c((8 * CE2 + i128)[:, None])
    trash2 = _c((CG + i128)[:, None])
    pos = np.arange(S, dtype=np.float32)
    inv = (10000.0 ** (-np.arange(0, 64, 2, dtype=np.float32) / 64)).astype(np.float32)
    ang = pos[:, None] * inv[None, :]
    cosT, sinT = np.cos(ang).astype(np.float32), np.sin(ang).astype(np.float32)
    bconst = b_const_inputs()
    sh = lambda a, c: a[c * NT:(c + 1) * NT]
    for l in range(2):
        r = _run("a", build_a, [dict(x=_c(sh(xmid, c)), xadd=_c(sh(moe, c)), g=_c(P["norm_mix_g"][l]), w=_c(P["w_in"][l]),
                                     ident=ident) for c in range(8)])
        proj = np.concatenate([r[c]["proj"] for c in range(8)], axis=0)
        x = np.concatenate([r[c]["xsum"] for c in range(8)], axis=0)
        r = _run("a2", build_a2, [dict(cq=_c(sh(proj, c)[:, 5664:6432]), ckv=_c(sh(proj, c)[:, 6432:6944]),
                                       kr=_c(sh(proj, c)[:, 6944:7008]), gq=_c(P["mla_q_norm_g"][l]),
                                       gkv=_c(P["mla_kv_norm_g"][l]), wuq=_c(P["mla_w_uq"][l]), wukv=_c(P["mla_w_ukv"][l]),
                                       cos=_c(cosT[(c % 4) * NT:(c % 4 + 1) * NT]), sin=_c(sinT[(c % 4) * NT:(c % 4 + 1) * NT]),
                                       ident=ident) for c in range(8)])
        qm = np.concatenate([r[c]["q"] for c in range(8)], axis=0)
        kvm = np.concatenate([r[c]["kv"] for c in range(8)], axis=0)
        krm = np.concatenate([r[c]["kro"] for c in range(8)], axis=0)
        maps = []
        for c in range(8):
            b, hq = c // 4, c % 4
            bs = slice(b * S, (b + 1) * S)
            d = dict(bconst)
            d.update(b_core_inputs(proj[bs], qm[bs], kvm[bs], krm[bs], P, l, hq))
            maps.append(d)
        r = _run("b", build_b, maps)
        o_all = np.empty((NTOK, D), np.float32)
        for c in range(8):
            b, hq = c // 4, c % 4
            o = r[c]["out"]
            for gi in range(4):
                o_all[b * S:(b + 1) * S, gi * 1024 + hq * 256:gi * 1024 + (hq + 1) * 256] = o[:, gi * 256:(gi + 1) * 256]
        rw = _c(np.concatenate([P["router_group_w"][l], P["router_expert_w"][l]], axis=1))
        rb = _c(np.concatenate([P["router_group_b"][l], P["router_expert_b"][l]]))
        r = _run("c", build_c, [dict(o=_c(sh(o_all, c)), x=_c(sh(x, c)), gout=_c(P["out_norm_g"][l]), wout=_c(P["w_out"][l]),
                                     gffn=_c(P["norm_ffn_g"][l]), rw=rw, rb=rb, ident=ident, iota64=iota64) for c in range(8)])
        xmid = np.concatenate([r[c]["xmid"] for c in range(8)], axis=0)
        xn2 = np.concatenate([r[c]["xn2"] for c in range(8)], axis=0)
        mall = np.concatenate([r[c]["mroute"] for c in range(8)], axis=0)
        rtall = np.concatenate([r[c]["route"] for c in range(8)], axis=0)
        r = _run("d", build_d2, [dict(mg=_c(mall[:, 8 * g:8 * g + 8]), rtall=_c(rtall), g8=np.array([8.0 * g], np.float32),
                                      xn2=np.ascontiguousarray(xn2), gffn=_c(P["norm_ffn_g"][l]),
                                      wg=_c(P["exp_w_gate"][l][8 * g:8 * g + 8]), wu=_c(P["exp_w_up"][l][8 * g:8 * g + 8]),
                                      wd=_c(P["exp_w_down"][l][8 * g:8 * g + 8]), tri=tri, tokid=tokid, ident=ident,
                                      iota8=iota8, lo8=lo8, trash1=trash1, trash2=trash2) for g in range(8)])
        zall = np.concatenate([r[g]["z"] for g in range(8)], axis=0)
        moe = np.take(zall, r[0]["inv"].T.reshape(-1), axis=0)
        if _DBG is not None:
            _DBG.append(dict(proj=proj, o_all=o_all, xmid=xmid, moe=moe, rtall=rtall, mall=mall))
            if _DBG_STOP:
                return xmid + moe
    r = _run("f", build_f, [dict(x=_c(sh(xmid, c)), xadd=_c(sh(moe, c)), g=_c(P["final_norm_g"])) for c in range(8)])
    out = np.concatenate([r[c]["xout"] for c in range(8)], axis=0)
    return out.reshape(2, S, D).astype(np.float32)
```

```python
import numpy as np
import ml_dtypes
import concourse.bass as bass
import concourse.mybir as mybir
from concourse.bass_utils import run_bass_kernel_spmd

F32 = mybir.dt.float32
BF16 = mybir.dt.bfloat16
I32 = mybir.dt.int32
AF = mybir.ActivationFunctionType
ALU = mybir.AluOpType
AX = mybir.AxisListType

ENGS = ("tensor", "vector", "scalar", "gpsimd", "sync")
DMA_SLOTS = 8


class Res:
    __slots__ = ("name", "lw", "rd")

    def __init__(self, name):
        self.name = name
        self.lw = None
        self.rd = {}


class Op:
    __slots__ = ("eng", "fn", "dma", "deps", "sig", "sem", "val", "idx")

    def __init__(self, eng, fn, dma):
        self.eng = eng
        self.fn = fn
        self.dma = dma
        self.deps = []
        self.sig = False
        self.sem = None
        self.val = 0


class Sched:
    def __init__(self, nc):
        self.nc = nc
        self.ops = []
        self.dma_cnt = {e: 0 for e in ENGS}
        self.dma_last = {}
        self.out_dmas = []

    def op(self, eng, fn, reads=(), writes=(), dma=False):
        o = Op(eng, fn, dma)
        o.idx = len(self.ops)
        deps = {}
        for r in reads:
            if r.lw is not None:
                deps[r.lw.idx] = (r.lw, "raw")
        for w in writes:
            if w.lw is not None and w.lw.idx not in deps:
                deps[w.lw.idx] = (w.lw, "waw")
            for rr in w.rd.values():
                if rr.idx not in deps:
                    deps[rr.idx] = (rr, "war")
        if dma:
            n = self.dma_cnt[eng]
            self.dma_cnt[eng] = n + 1
            slot = n % DMA_SLOTS
            o.sem = ("dma", eng, slot)
            o.val = 16 * (n // DMA_SLOTS + 1)
            o.sig = True
            prev = self.dma_last.get((eng, slot))
            if prev is not None and prev.idx not in deps:
                deps[prev.idx] = (prev, "slot")
            self.dma_last[(eng, slot)] = o
        for d, kind in deps.values():
            if not d.dma and d.eng == eng:
                if kind != "raw" or eng == "tensor":
                    continue
            o.deps.append(d)
            d.sig = True
        for r in reads:
            r.rd[o.sem if dma else eng] = o
        for w in writes:
            w.lw = o
            w.rd = {}
        self.ops.append(o)
        return o

    def emit(self):
        nc = self.nc
        cnt = {e: 0 for e in ENGS}
        for o in self.ops:
            if not o.dma and o.sig:
                cnt[o.eng] += 1
                o.sem = ("eng", o.eng)
                o.val = cnt[o.eng]
        semkeys = [("eng", e) for e in ENGS]
        for e in ENGS:
            for s in range(min(DMA_SLOTS, self.dma_cnt[e])):
                semkeys.append(("dma", e, s))
        import contextlib
        with contextlib.ExitStack() as st:
            sems = {k: st.enter_context(nc.semaphore("s_" + "_".join(str(x) for x in k))) for k in semkeys}
            block = st.enter_context(nc.Block())
            per = {e: [o for o in self.ops if o.eng == e] for e in ENGS}
            final = {}
            for (eng, slot), o in self.dma_last.items():
                final.setdefault(eng, []).append((sems[o.sem], o.val))

            def run(e, engobj):
                seen = {}
                for o in per[e]:
                    for d in o.deps:
                        if seen.get(d.sem, 0) < d.val:
                            engobj.wait_ge(sems[d.sem], d.val)
                            seen[d.sem] = d.val
                    ins = o.fn(engobj)
                    if o.sig:
                        ins.then_inc(sems[o.sem], 16 if o.dma else 1)
                for s, v in final.get(e, []):
                    engobj.wait_ge(s, v)

            @block.tensor
            def _(eng):
                run("tensor", eng)

            @block.vector
            def _(eng):
                run("vector", eng)

            @block.scalar
            def _(eng):
                run("scalar", eng)

            @block.gpsimd
            def _(eng):
                run("gpsimd", eng)

            @block.sync
            def _(eng):
                run("sync", eng)


class V:
    __slots__ = ("ap", "res")

    def __init__(self, ap, res):
        self.ap = ap
        self.res = res

    def __getitem__(self, idx):
        return V(self.ap[idx], self.res)

    def rr(self, s, **kw):
        return V(self.ap.rearrange(s, **kw), self.res)

    def bc(self, shape):
        return V(self.ap.broadcast_to(shape), self.res)

    def un(self, axis):
        return V(self.ap.unsqueeze(axis), self.res)


class Tile:
    def __init__(self, handle, name):
        self.h = handle
        self.name = name
        self.res = Res(name)
        self.subs = {}

    def __getitem__(self, idx):
        return V(self.h[idx], self.res)

    def sub(self, key, idx=None):
        r = self.subs.get(key)
        if r is None:
            r = self.subs[key] = Res("%s/%s" % (self.name, key))
        return V(self.h[idx] if idx is not None else self.h[:], r)


def _aps(x):
    return x.ap if isinstance(x, V) else x


class KB:
    def __init__(self, nc, st):
        self.nc = nc
        self.st = st
        self.S = Sched(nc)
        self.n = 0

    def sb(self, name, shape, dt):
        return Tile(self.st.enter_context(self.nc.sbuf_tensor(name, list(shape), dt)), name)

    def ps(self, name, shape, dt):
        return Tile(self.st.enter_context(self.nc.psum_tensor(name, list(shape), dt)), name)

    def dram(self, name, shape, dt, kind):
        ap = self.nc.dram_tensor(name, list(shape), dt, kind=kind).ap()
        return V(ap, Res(name))

    def _op(self, eng, fn, reads, writes, dma=False):
        rs = [r.res if isinstance(r, V) else r for r in reads if isinstance(r, (V, Res))]
        ws = [w.res if isinstance(w, V) else w for w in writes if isinstance(w, (V, Res))]
        return self.S.op(eng, fn, rs, ws, dma)

    def dma(self, out, in_, eng="sync", xr=(), xw=(), **kw):
        return self._op(eng, lambda e: e.dma_start(out=out.ap, in_=in_.ap, **kw), [in_] + list(xr), [out] + list(xw),
                        dma=True)

    def gather(self, out, table, idx, **kw):
        return self._op("gpsimd", lambda e: e.indirect_dma_start(
            out=out.ap, out_offset=None, in_=table.ap,
            in_offset=bass.IndirectOffsetOnAxis(ap=idx.ap, axis=0), **kw), [table, idx], [out], dma=True)

    def scatter(self, table, idx, in_, **kw):
        return self._op("gpsimd", lambda e: e.indirect_dma_start(
            out=table.ap, out_offset=bass.IndirectOffsetOnAxis(ap=idx.ap, axis=0),
            in_=in_.ap, in_offset=None, **kw), [in_, idx], [table], dma=True)

    def mm(self, out, lhsT, rhs, start=True, stop=True):
        return self._op("tensor", lambda e: e.matmul(out.ap, lhsT=lhsT.ap, rhs=rhs.ap, start=start, stop=stop),
                        [lhsT, rhs], [out])

    def tr(self, out, in_, ident):
        return self._op("tensor", lambda e: e.transpose(out.ap, in_.ap, ident.ap), [in_, ident], [out])

    def act(self, out, in_, func, bias=0.0, scale=1.0, accum=None, eng="scalar"):
        kw = {}
        if accum is not None:
            kw["accum_out"] = accum.ap
        ws = [out] + ([accum] if accum is not None else [])
        return self._op(eng, lambda e: e.activation(out=out.ap, in_=in_.ap, func=func, bias=_aps(bias),
                                                    scale=_aps(scale), **kw), [in_, bias, scale], ws)

    def tt(self, out, in0, in1, op, eng="vector"):
        return self._op(eng, lambda e: e.tensor_tensor(out=out.ap, in0=in0.ap, in1=in1.ap, op=op), [in0, in1], [out])

    def ts(self, out, in0, s1, s2, op0, op1=None, accum=None, eng="vector"):
        kw = {}
        if op1 is not None:
            kw["op1"] = op1
        if accum is not None:
            kw["accum_out"] = accum.ap
        ws = [out] + ([accum] if accum is not None else [])
        return self._op(eng, lambda e: e.tensor_scalar(out=out.ap, in0=in0.ap, scalar1=_aps(s1), scalar2=_aps(s2),
                                                       op0=op0, **kw), [in0, s1, s2], ws)

    def stt(self, out, in0, scalar, in1, op0, op1, eng="vector"):
        return self._op(eng, lambda e: e.scalar_tensor_tensor(out=out.ap, in0=in0.ap, scalar=_aps(scalar), in1=in1.ap,
                                                              op0=op0, op1=op1), [in0, scalar, in1], [out])

    def copy(self, out, in_, eng="vector"):
        if eng == "scalar":
            return self._op(eng, lambda e: e.copy(out=out.ap, in_=in_.ap), [in_], [out])
        return self._op(eng, lambda e: e.tensor_copy(out=out.ap, in_=in_.ap), [in_], [out])

    def memset(self, out, val, eng="vector"):
        return self._op(eng, lambda e: e.memset(out.ap, val), [], [out])

    def recip(self, out, in_):
        return self._op("vector", lambda e: e.reciprocal(out=out.ap, in_=in_.ap), [in_], [out])

    def reduce(self, out, in_, op, axis=AX.X):
        return self._op("vector", lambda e: e.tensor_reduce(out=out.ap, in_=in_.ap, axis=axis, op=op), [in_], [out])

    def max8(self, out, in_):
        return self._op("vector", lambda e: e.max(out=out.ap, in_=in_.ap), [in_], [out])

    def match_replace(self, out, rep, vals, imm):
        return self._op("vector", lambda e: e.match_replace(out=out.ap, in_to_replace=rep.ap, in_values=vals.ap,
                                                            imm_value=imm), [rep, vals], [out])

    def rstd(self, out, ss, n, tmp):
        self.ts(tmp, ss, 1.0 / n, 1e-6, ALU.mult, ALU.add)
        self.act(tmp, tmp, AF.Sqrt)
        self.recip(out, tmp)


import contextlib

D = 4096
NT = 1024
EPS = 1e-6
BIG = 30000.0


def col_view(v, p=128):
    return v.rr("(c p) -> p c", p=p)


def build_c():
    nc = bass.Bass("TRN2", target_bir_lowering=False)
    with contextlib.ExitStack() as st:
        K = KB(nc, st)
        o_d = K.dram("o", [NT, D], F32, "ExternalInput")
        x_d = K.dram("x", [NT, D], F32, "ExternalInput")
        gout_d = K.dram("gout", [D], F32, "ExternalInput")
        wout_d = K.dram("wout", [D, D], F32, "ExternalInput")
        gffn_d = K.dram("gffn", [D], F32, "ExternalInput")
        rw_d = K.dram("rw", [D, 72], F32, "ExternalInput")
        rb_d = K.dram("rb", [72], F32, "ExternalInput")
        ident_d = K.dram("ident", [128, 128], F32, "ExternalInput")
        xmid_d = K.dram("xmid", [NT, D], F32, "ExternalOutput")
        xn2_d = K.dram("xn2", [NT, D], BF16, "ExternalOutput")
        m_d = K.dram("mroute", [NT, 64], F32, "ExternalOutput")
        gw_d = K.dram("gwroute", [NT, 64], F32, "ExternalOutput")
        rt_d = K.dram("route", [NT, 4], F32, "ExternalOutput")
        iota_d = K.dram("iota64", [128, 64], F32, "ExternalInput")
        xmid_res = [Res("xmid%d" % n) for n in range(NT // 128)]

        identf = K.sb("identf", [128, 128], F32)
        identb = K.sb("identb", [128, 128], BF16)
        gcol = K.sb("gcol", [128, 32], F32)
        g2col = K.sb("g2col", [128, 32], F32)
        wr = K.sb("wr", [128, 32, 72], F32)
        rbb = K.sb("rbb", [128, 72], F32)
        iota = K.sb("iota", [128, 64], F32)
        rto = K.sb("rto", [128, 4], F32)
        rtmp = K.sb("rtmp", [128, 64], F32)
        K.dma(iota[:], iota_d)
        K.dma(identf[:], ident_d)
        K.copy(identb[:], identf[:])
        K.dma(gcol[:], col_view(gout_d), allow_slow_non_contiguous=True)
        K.dma(g2col[:], col_view(gffn_d), allow_slow_non_contiguous=True)
        K.dma(wr[:], rw_d.rr("(c p) n -> p c n", p=128))
        K.dma(rbb[:], V(rb_d.ap.partition_broadcast(128), rb_d.res))
        K.tt(wr[:], wr[:], g2col[:].un(2).bc([128, 32, 72]), ALU.mult)

        mixedT = K.sb("mixedT", [128, 32, 512], BF16)
        wbf = [K.sb("wbf%d" % i, [128, 32, 512], BF16) for i in range(2)]
        ot = K.sb("ot", [128, D], F32)
        onb = K.sb("onb", [128, D], BF16)
        junk = K.sb("junk", [128, 1024], BF16)
        xnb = K.sb("xnb", [128, D], BF16)
        ss = K.sb("ss", [128, 4], F32)
        tmp4 = K.sb("tmp4", [128, 4], F32)
        rs4 = K.sb("rs4", [128, 4], F32)
        xt = [K.sb("xt%d" % i, [128, 4, 512], F32) for i in range(2)]
        ysb = [K.sb("ysb%d" % i, [128, 512], F32) for i in range(2)]
        xmT = K.sb("xmT", [128, 32, 128], F32)
        lg = K.sb("lg", [128, 72], F32)
        sm = K.sb("sm", [128, 16], F32)
        oh = K.sb("oh", [128, 8], F32)
        msk = K.sb("msk", [128, 64], F32)
        m8 = K.sb("m8", [128, 8], F32)
        mo = K.sb("mo", [128, 64], F32)
        gwo = K.sb("gwo", [128, 64], F32)
        tpb = [K.ps("tpb%d" % i, [128, 8, 128], BF16) for i in range(2)]
        yps = [K.ps("yps%d" % i, [128, 512], F32) for i in range(2)]
        tpf = [K.ps("tpf%d" % i, [128, 4, 128], F32) for i in range(2)]
        lgp = K.ps("lgp", [128, 72], F32)

        wv = wout_d.rr("(c p) n -> p c n", p=128)
        wcount = 0
        for hf in range(NT // 512):
            for n in range(4):
                r0 = hf * 512 + n * 128
                K.dma(ot[:], o_d[r0:r0 + 128, :])
                for gi in range(4):
                    K.act(junk[:], ot[:, gi * 1024:(gi + 1) * 1024], AF.Square, accum=ss[:, gi:gi + 1])
                K.rstd(rs4[:], ss[:], 1024.0, tmp4[:])
                for gi in range(4):
                    K.ts(onb.sub(gi, (slice(None), slice(gi * 1024, (gi + 1) * 1024))),
                         ot[:, gi * 1024:(gi + 1) * 1024], rs4[:, gi:gi + 1], None, ALU.mult)
                for k in range(4):
                    tp = tpb[k % 2]
                    for c in range(8):
                        cc = k * 8 + c
                        K.tr(tp[:, c, :], onb.sub(cc // 8, (slice(None), slice(cc * 128, (cc + 1) * 128))), identb[:])
                    K.tt(mixedT.sub((n, k), (slice(None), slice(k * 8, k * 8 + 8), slice(n * 128, (n + 1) * 128))),
                         tp[:], gcol[:, k * 8:k * 8 + 8].un(2).bc([128, 8, 128]), ALU.mult)
            for j in range(8):
                wb = wbf[wcount % 2]
                wcount += 1
                K.dma(wb[:], wv[:, :, j * 512:(j + 1) * 512], eng="gpsimd")
                xb = xt[j % 2]
                K.dma(xb[:], x_d[hf * 512:(hf + 1) * 512, j * 512:(j + 1) * 512].rr("(n p) c -> p n c", p=128))
                for n in range(4):
                    yp = yps[n % 2]
                    for c in range(32):
                        K.mm(yp[:], mixedT.sub((n, c // 8), (slice(None), c, slice(n * 128, (n + 1) * 128))),
                             wb[:, c, :], start=(c == 0), stop=(c == 31))
                    yb = ysb[n % 2]
                    K.tt(yb[:], yp[:], xb[:, n, :], ALU.add)
                    r0 = hf * 512 + n * 128
                    K.dma(V(xmid_d.ap[r0:r0 + 128, j * 512:(j + 1) * 512], xmid_res[hf * 4 + n]), yb[:])
            for n in range(4):
                r0 = hf * 512 + n * 128
                K.dma(ot[:], V(xmid_d.ap[r0:r0 + 128, :], xmid_res[hf * 4 + n]))
                K.act(xnb[:], ot[:], AF.Square, accum=ss[:, 0:1])
                K.rstd(rs4[:, 0:1], ss[:, 0:1], float(D), tmp4[:, 0:1])
                K.ts(xnb[:], ot[:], rs4[:, 0:1], None, ALU.mult)
                K.dma(xn2_d[r0:r0 + 128, :], xnb[:])
                for k in range(8):
                    tp = tpf[k % 2]
                    for c in range(4):
                        cc = k * 4 + c
                        K.tr(tp[:, c, :], ot[:, cc * 128:(cc + 1) * 128], identf[:])
                    K.copy(xmT.sub(k, (slice(None), slice(k * 4, k * 4 + 4), slice(None))), tp[:],
                           eng=("scalar" if k % 2 else "vector"))
                for c in range(32):
                    K.mm(lgp[:], xmT.sub(c // 4, (slice(None), c, slice(None))), wr[:, c, :],
                         start=(c == 0), stop=(c == 31))
                K.stt(lg[:], lgp[:], rs4[:, 0:1], rbb[:], ALU.mult, ALU.add)
                router_math(K, lg, sm, oh, msk, m8, mo, gwo, iota, rto, rtmp)
                K.dma(rt_d[r0:r0 + 128, :], rto[:])
                K.dma(m_d[r0:r0 + 128, :], mo[:])
                K.dma(gw_d[r0:r0 + 128, :], gwo[:])
        K.S.emit()
    return nc


def router_math(K, lg, sm, oh, msk, m8, mo, gwo, iota, rto, rtmp):
    K.reduce(sm[:, 0:1], lg[:, 0:8], ALU.max)
    K.ts(oh[:], lg[:, 0:8], sm[:, 0:1], None, ALU.is_ge)
    K.ts(sm[:, 1:2], sm[:, 0:1], -1.0, None, ALU.mult)
    K.act(msk[:, 0:8], lg[:, 0:8], AF.Exp, bias=sm[:, 1:2], accum=sm[:, 2:3])
    K.recip(sm[:, 3:4], sm[:, 2:3])
    K.ts(oh[:], oh[:], BIG, -BIG, ALU.mult, ALU.add)
    K.tt(msk[:].rr("p (g e) -> p g e", g=8), lg[:, 8:72].rr("p (g e) -> p g e", g=8),
         oh[:].un(2).bc([128, 8, 8]), ALU.add)
    K.max8(m8[:], msk[:])
    K.ts(mo[:], msk[:], m8[:, 1:2], None, ALU.is_ge)
    K.tt(sm[:, 4:5], m8[:, 0:1], m8[:, 1:2], ALU.add)
    K.ts(sm[:, 4:5], sm[:, 4:5], -1.0, None, ALU.mult)
    K.act(gwo[:], msk[:], AF.Sigmoid, bias=sm[:, 4:5], scale=2.0)
    K.stt(gwo[:], gwo[:], sm[:, 3:4], mo[:], ALU.mult, ALU.mult)
    K.stt(rtmp[:], iota[:], 1.0, mo[:], ALU.add, ALU.mult)
    K.reduce(rto[:, 1:2], rtmp[:], ALU.max)
    K.ts(rto[:, 1:2], rto[:, 1:2], -1.0, None, ALU.add)
    K.ts(rtmp[:], iota[:], -1.0, 64.0, ALU.mult, ALU.add)
    K.tt(rtmp[:], rtmp[:], mo[:], ALU.mult)
    K.reduce(rto[:, 0:1], rtmp[:], ALU.max)
    K.ts(rto[:, 0:1], rto[:, 0:1], -1.0, 64.0, ALU.mult, ALU.add)
    for k in range(2):
        K.ts(rtmp[:], iota[:], rto[:, k:k + 1], None, ALU.is_equal)
        K.tt(rtmp[:], rtmp[:], gwo[:], ALU.mult)
        K.reduce(rto[:, 2 + k:3 + k], rtmp[:], ALU.add)


CE = 384
NTOK = 8192
NTT = NTOK // 128
OOB = 1.0e6


def build_d1():
    nc = bass.Bass("TRN2", target_bir_lowering=False)
    with contextlib.ExitStack() as st:
        K = KB(nc, st)
        m_d = K.dram("mall", [NTOK, 64], F32, "ExternalInput")
        rt_d = K.dram("rtall", [NTOK, 4], F32, "ExternalInput")
        gb_d = K.dram("gbase", [1], F32, "ExternalInput")
        tri_d = K.dram("tri", [128, 128], F32, "ExternalInput")
        tok_d = K.dram("tokid", [128, NTT], F32, "ExternalInput")
        iota_d = K.dram("iota64", [128, 64], F32, "ExternalInput")
        idx_d = K.dram("idxlist", [8 * CE, 2], I32, "ExternalOutput")
        dg_d = K.dram("destg", [NTOK, 2], I32, "ExternalOutput")

        Mt = K.sb("Mt", [128, NTT, 64], F32)
        Mb = K.sb("Mb", [128, NTT, 64], BF16)
        Rf = K.sb("Rf", [128, NTT, 64], F32)
        Rb = K.sb("Rb", [128, NTT, 64], BF16)
        slot = K.sb("slot", [128, NTT, 64], F32)
        oh = K.sb("ohd", [128, NTT, 64], F32)
        trif = K.sb("trif", [128, 128], F32)
        trib = K.sb("trib", [128, 128], BF16)
        oneb = K.sb("oneb", [128, 128], BF16)
        iota = K.sb("iota", [128, 64], F32)
        tokf = K.sb("tokf", [128, NTT], F32)
        toki = K.sb("toki", [128, NTT, 2], I32)
        rt = K.sb("rt", [128, NTT, 4], F32)
        gb = K.sb("gb", [128, 1], F32)
        sl = K.sb("sl", [128, NTT], F32)
        dgl = K.sb("dgl", [128, NTT], F32)
        dl = K.sb("dl", [128, NTT], F32)
        ok = K.sb("ok", [128, NTT], F32)
        ok2 = K.sb("ok2", [128, NTT], F32)
        di = [K.sb("di%d" % k, [128, NTT], I32) for k in range(2)]
        dgi = K.sb("dgi", [128, NTT, 2], I32)
        zt = K.sb("zt", [128, 8 * CE // 128, 2], I32)
        pss = [K.ps("pss%d" % i, [128, 8, 64], F32) for i in range(8)]

        K.dma(Mt[:], m_d.rr("(n p) e -> p n e", p=128))
        K.dma(rt[:], rt_d.rr("(n p) k -> p n k", p=128))
        K.dma(trif[:], tri_d)
        K.dma(tokf[:], tok_d)
        K.dma(iota[:], iota_d)
        K.dma(gb[:], V(gb_d.ap.partition_broadcast(128), gb_d.res))
        K.copy(trib[:], trif[:])
        K.memset(oneb[:], 1.0)
        K.copy(toki[:, :, 0], tokf[:])
        K.copy(toki[:, :, 1], tokf[:])
        K.memset(zt[:], 0)
        K.dma(idx_d.rr("(s p) o -> p s o", p=128), zt[:])
        K.copy(Mb[:], Mt[:])
        K.memset(Rf[:, 0, :], 0.0)
        for n in range(1, NTT):
            K.tt(Rf[:, n, :], Rf[:, n - 1, :], Mt[:, n - 1, :], ALU.add)
        K.copy(Rb[:], Rf[:])
        for n in range(NTT):
            p = pss[n // 8]
            K.mm(p[:, n % 8, :], trib[:], Mb[:, n, :], start=True, stop=False)
            K.mm(p[:, n % 8, :], oneb[:], Rb[:, n, :], start=False, stop=True)
        for b in range(8):
            K.copy(slot[:, b * 8:(b + 1) * 8, :], pss[b][:], eng=("scalar" if b % 2 else "vector"))
        iota3 = iota[:].un(1).bc([128, NTT, 64])
        for k in range(2):
            K.tt(oh[:], iota3, rt[:, :, k].un(2).bc([128, NTT, 64]), ALU.is_equal)
            K.tt(oh[:], oh[:], slot[:], ALU.mult)
            K.reduce(sl[:], oh[:], ALU.add)
            K.stt(dgl[:], rt[:, :, k], float(CE), sl[:], ALU.mult, ALU.add)
            K.copy(dgi[:, :, k], dgl[:])
            K.ts(ok[:], sl[:], float(CE), None, ALU.is_lt)
            K.ts(dl[:], dgl[:], gb[:, 0:1], None, ALU.subtract)
            K.ts(ok2[:], dl[:], 0.0, None, ALU.is_ge)
            K.tt(ok[:], ok[:], ok2[:], ALU.mult)
            K.ts(ok2[:], dl[:], float(8 * CE), None, ALU.is_lt)
            K.tt(ok[:], ok[:], ok2[:], ALU.mult)
            K.ts(dl[:], dl[:], -OOB, None, ALU.add)
            K.tt(dl[:], dl[:], ok[:], ALU.mult)
            K.ts(dl[:], dl[:], OOB, None, ALU.add)
            K.copy(di[k][:], dl[:])
        K.dma(dg_d.rr("(n p) k -> p n k", p=128), dgi[:])
        for n in range(NTT):
            for k in range(2):
                K.scatter(idx_d, di[k][:, n:n + 1], toki[:, n, :], bounds_check=8 * CE - 1, oob_is_err=False)
        K.S.emit()
    return nc


NPROJ = 8288


def ot_pre(K):
    if not hasattr(K, "_otp"):
        K._otp = K.sb("otp", [128, D], F32)
    return K._otp


def norm_transpose(K, src_d, r0, width, gcols, ot, onb, ss, rs, tmp, tpb, identb, dstT, n, junk):
    nch = width // 128
    K.dma(ot[:, 0:width], src_d[r0:r0 + 128, :])
    K.act(junk[:, 0:width], ot[:, 0:width], AF.Square, accum=ss[:, 0:1])
    K.rstd(rs[:, 0:1], ss[:, 0:1], float(width), tmp[:, 0:1])
    K.ts(onb[:, 0:width], ot[:, 0:width], rs[:, 0:1], None, ALU.mult)
    for k in range((nch + 7) // 8):
        tp = tpb[k % 2]
        m = min(8, nch - k * 8)
        for c in range(m):
            cc = k * 8 + c
            K.tr(tp[:, c, :], onb[:, cc * 128:(cc + 1) * 128], identb[:])
        K.tt(dstT.sub((n, k), (slice(None), slice(k * 8, k * 8 + m), slice(n * 128, (n + 1) * 128))),
             tp[:, 0:m, :], gcols[:, k * 8:k * 8 + m].un(2).bc([128, m, 128]), ALU.mult)


def build_a():
    nc = bass.Bass("TRN2", target_bir_lowering=False)
    with contextlib.ExitStack() as st:
        K = KB(nc, st)
        x_d = K.dram("x", [NT, D], F32, "ExternalInput")
        xa_d = K.dram("xadd", [NT, D], F32, "ExternalInput")
        g_d = K.dram("g", [D], F32, "ExternalInput")
        w_d = K.dram("w", [D, NPROJ], F32, "ExternalInput")
        ident_d = K.dram("ident", [128, 128], F32, "ExternalInput")
        p_d = K.dram("proj", [NT, NPROJ], F32, "ExternalOutput")
        xs_d = K.dram("xsum", [NT, D], F32, "ExternalOutput")
        xs_res = [Res("xs%d" % n) for n in range(NT // 128)]
        xa = K.sb("xa", [128, D], F32)
        for n in range(NT // 128):
            K.dma(ot_pre(K)[:], x_d[n * 128:(n + 1) * 128, :])
            K.dma(xa[:], xa_d[n * 128:(n + 1) * 128, :])
            K.tt(xa[:], xa[:], ot_pre(K)[:], ALU.add)
            K.dma(V(xs_d.ap[n * 128:(n + 1) * 128, :], xs_res[n]), xa[:])
        identf = K.sb("identf", [128, 128], F32)
        identb = K.sb("identb", [128, 128], BF16)
        gcol = K.sb("gcol", [128, 32], F32)
        K.dma(identf[:], ident_d)
        K.copy(identb[:], identf[:])
        K.dma(gcol[:], col_view(g_d), allow_slow_non_contiguous=True)
        xT = K.sb("xT", [128, 32, 512], BF16)
        wbf = [K.sb("wbf%d" % i, [128, 32, 512], BF16) for i in range(2)]
        ot = K.sb("ot", [128, D], F32)
        onb = K.sb("onb", [128, D], BF16)
        junk = K.sb("junk", [128, D], BF16)
        ss = K.sb("ss", [128, 1], F32)
        tmp = K.sb("tmp", [128, 1], F32)
        rs = K.sb("rs", [128, 1], F32)
        ysb = [K.sb("ysb%d" % i, [128, 512], F32) for i in range(4)]
        tpb = [K.ps("tpb%d" % i, [128, 8, 128], BF16) for i in range(2)]
        yps = [K.ps("yps%d" % i, [128, 512], F32) for i in range(4)]
        wv = w_d.rr("(c p) n -> p c n", p=128)
        chunks = [(j * 512, 512) for j in range(16)] + [(8192, 96)]
        wcount = 0
        ycount = 0
        for hf in range(NT // 512):
            for n in range(4):
                norm_transpose(K, V(xs_d.ap, xs_res[hf * 4 + n]), hf * 512 + n * 128, D, gcol, ot, onb, ss, rs, tmp, tpb,
                               identb, xT, n, junk)
            for (c0, cw) in chunks:
                wb = wbf[wcount % 2]
                wcount += 1
                K.dma(wb[:, :, 0:cw], wv[:, :, c0:c0 + cw], eng="gpsimd")
                for n in range(4):
                    yp = yps[ycount % 4]
                    yb = ysb[ycount % 4]
                    for c in range(32):
                        K.mm(yp[:, 0:cw], xT.sub((n, c // 8), (slice(None), c, slice(n * 128, (n + 1) * 128))),
                             wb[:, c, 0:cw], start=(c == 0), stop=(c == 31))
                    K.copy(yb[:, 0:cw], yp[:, 0:cw], eng=("scalar" if ycount % 2 else "vector"))
                    ycount += 1
                    r0 = hf * 512 + n * 128
                    K.dma(p_d[r0:r0 + 128, c0:c0 + cw], yb[:, 0:cw])
        K.S.emit()
    return nc


def build_a2():
    nc = bass.Bass("TRN2", target_bir_lowering=False)
    with contextlib.ExitStack() as st:
        K = KB(nc, st)
        cq_d = K.dram("cq", [NT, 768], F32, "ExternalInput")
        ckv_d = K.dram("ckv", [NT, 512], F32, "ExternalInput")
        kr_d = K.dram("kr", [NT, 64], F32, "ExternalInput")
        gq_d = K.dram("gq", [768], F32, "ExternalInput")
        gkv_d = K.dram("gkv", [512], F32, "ExternalInput")
        wuq_d = K.dram("wuq", [768, 1536], F32, "ExternalInput")
        wukv_d = K.dram("wukv", [512, 2048], F32, "ExternalInput")
        cos_d = K.dram("cos", [NT, 32], F32, "ExternalInput")
        sin_d = K.dram("sin", [NT, 32], F32, "ExternalInput")
        ident_d = K.dram("ident", [128, 128], F32, "ExternalInput")
        q_d = K.dram("q", [NT, 1536], F32, "ExternalOutput")
        kv_d = K.dram("kv", [NT, 2048], F32, "ExternalOutput")
        kro_d = K.dram("kro", [NT, 64], F32, "ExternalOutput")
        identf = K.sb("identf", [128, 128], F32)
        identb = K.sb("identb", [128, 128], BF16)
        gqc = K.sb("gqc", [128, 6], F32)
        gkvc = K.sb("gkvc", [128, 4], F32)
        wuq = K.sb("wuqs", [128, 6, 1536], BF16)
        wukv = K.sb("wukvs", [128, 4, 2048], BF16)
        K.dma(identf[:], ident_d)
        K.copy(identb[:], identf[:])
        K.dma(gqc[:], col_view(gq_d), allow_slow_non_contiguous=True)
        K.dma(gkvc[:], col_view(gkv_d), allow_slow_non_contiguous=True)
        K.dma(wuq[:], wuq_d.rr("(c p) n -> p c n", p=128), eng="gpsimd")
        K.dma(wukv[:], wukv_d.rr("(c p) n -> p c n", p=128), eng="gpsimd")
        cqT = K.sb("cqT", [128, 6, 128], BF16)
        ckvT = K.sb("ckvT", [128, 4, 128], BF16)
        ot = K.sb("ot", [128, 768], F32)
        onb = K.sb("onb", [128, 768], BF16)
        junk = K.sb("junk", [128, 768], BF16)
        ss = K.sb("ss", [128, 1], F32)
        tmp = K.sb("tmp", [128, 1], F32)
        rs = K.sb("rs", [128, 1], F32)
        qsb = K.sb("qsb", [128, 1536], F32)
        kvsb = K.sb("kvsb", [128, 2048], F32)
        krs = K.sb("krs", [128, 64], F32)
        cs = K.sb("cs", [128, 32], F32)
        sn = K.sb("sn", [128, 32], F32)
        t1 = K.sb("t1", [128, 8, 32], F32)
        t2 = K.sb("t2", [128, 8, 32], F32)
        t3 = K.sb("t3", [128, 8, 32], F32)
        t4 = K.sb("t4", [128, 8, 32], F32)
        tpb = [K.ps("tpb%d" % i, [128, 8, 128], BF16) for i in range(2)]
        yps = [K.ps("yps%d" % i, [128, 512], F32) for i in range(4)]

        def rope(buf3, nh):
            x1 = buf3[:, :, 0:32]
            x2 = buf3[:, :, 32:64]
            cb = cs[:].un(1).bc([128, nh, 32])
            sb_ = sn[:].un(1).bc([128, nh, 32])
            K.tt(t1[:, 0:nh, :], x1, cb, ALU.mult)
            K.tt(t2[:, 0:nh, :], x2, sb_, ALU.mult)
            K.tt(t3[:, 0:nh, :], x2, cb, ALU.mult)
            K.tt(t4[:, 0:nh, :], x1, sb_, ALU.mult)
            K.tt(x1, t1[:, 0:nh, :], t2[:, 0:nh, :], ALU.subtract)
            K.tt(x2, t3[:, 0:nh, :], t4[:, 0:nh, :], ALU.add)

        yc = 0
        for n in range(NT // 128):
            r0 = n * 128
            norm_transpose(K, cq_d, r0, 768, gqc, ot, onb, ss, rs, tmp, tpb, identb, cqT, 0, junk)
            norm_transpose(K, ckv_d, r0, 512, gkvc, ot, onb, ss, rs, tmp, tpb, identb, ckvT, 0, junk)
            K.dma(krs[:], kr_d[r0:r0 + 128, :])
            K.dma(cs[:], cos_d[r0:r0 + 128, :])
            K.dma(sn[:], sin_d[r0:r0 + 128, :])
            for j in range(3):
                yp = yps[yc % 4]
                yc += 1
                for c in range(6):
                    K.mm(yp[:], cqT.sub((0, 0), (slice(None), c, slice(None))), wuq[:, c, j * 512:(j + 1) * 512],
                         start=(c == 0), stop=(c == 5))
                K.copy(qsb[:, j * 512:(j + 1) * 512], yp[:], eng=("scalar" if j % 2 else "vector"))
            for j in range(4):
                yp = yps[yc % 4]
                yc += 1
                for c in range(4):
                    K.mm(yp[:], ckvT.sub((0, 0), (slice(None), c, slice(None))), wukv[:, c, j * 512:(j + 1) * 512],
                         start=(c == 0), stop=(c == 3))
                K.copy(kvsb[:, j * 512:(j + 1) * 512], yp[:], eng=("scalar" if j % 2 else "vector"))
            rope(qsb[:].rr("p (h d) -> p h d", d=192)[:, :, 128:192], 8)
            rope(krs[:].rr("p (h d) -> p h d", d=64), 1)
            K.dma(q_d[r0:r0 + 128, :], qsb[:])
            K.dma(kv_d[r0:r0 + 128, :], kvsb[:])
            K.dma(kro_d[r0:r0 + 128, :], krs[:])
        K.S.emit()
    return nc


S = 4096
NQT = S // 128


def nsa_tables():
    c = np.arange(256)
    c_end = 16 * c + 31
    t = np.arange(S)
    cm = (t[None, :] >= c_end[:, None]) & (c[:, None] < 255)
    cmpmask = cm.reshape(2, 128, NQT, 128).transpose(1, 0, 2, 3).astype(np.float32)
    j = np.arange(64)
    c_start = 16 * c
    ovl = ((c_start[:, None] <= j[None, :] * 64 + 63) & (c_end[:, None] >= j[None, :] * 64) & (c[:, None] < 255))
    ovl = ovl.reshape(2, 128, 64).transpose(1, 0, 2).astype(np.float32)
    cur = (t // 64)[:, None]
    forced = (j[None, :] == 0) | (j[None, :] == cur) | (j[None, :] == cur - 1)
    future = j[None, :] > cur
    keep = (~(forced | future)).astype(np.float32)
    fill = np.where(forced, 1e6 + j[None, :] * 16.0, 0.0) + np.where(future, -1e6 - j[None, :] * 16.0, 0.0)
    keep = keep.reshape(NQT, 128, 64).transpose(1, 0, 2)
    fill = fill.reshape(NQT, 128, 64).transpose(1, 0, 2).astype(np.float32)
    E = (np.arange(S)[None, :] // 64 == j[:, None]).astype(np.float32)
    return cmpmask, ovl, np.ascontiguousarray(keep), np.ascontiguousarray(fill), E


class ACtx:
    pass


def attn_head(K, C, qparts, kparts, vaug, vw, klist_fn, scale, bias_fn, extra_fn, epilogue):
    LOOK = 2
    jobs = []
    for qt in range(NQT):
        kl = klist_fn(qt)
        for i, (kt, mk) in enumerate(kl):
            jobs.append((qt, kt, mk, i == 0, i == len(kl) - 1))
    st = {}
    po_of = {}

    def front(j):
        qt, kt, mk, first, last = jobs[j]
        pss = C.ps_s[C.sc % 4]
        pt = C.pT[C.sc % 4]
        C.sc += 1
        ex = extra_fn(kt, qt) if extra_fn else None
        npart = len(qparts)
        for p in range(npart):
            K.mm(pss[:, 0:128], kparts[p][:, kt * 128:(kt + 1) * 128], qparts[p][:, qt * 128:(qt + 1) * 128],
                 start=(p == 0), stop=(p == npart - 1 and ex is None))
        if ex is not None:
            K.mm(pss[:, 0:128], ex[0], ex[1], start=False, stop=True)
        b = bias_fn(kt, qt) if bias_fn else 0.0
        K.act(pt[:], pss[:, 0:128], AF.Exp, bias=b, scale=scale)
        if mk is not None:
            K.tt(pt[:], pt[:], mk, ALU.mult)
        st[j] = pt

    def back(j):
        qt, kt, mk, first, last = jobs[j]
        if first:
            po_of[qt] = C.ps_o[C.oc % 2]
            C.oc += 1
        po = po_of[qt]
        K.mm(po[:, 0:vw], st.pop(j)[:], vaug[:, kt, 0:vw], start=first, stop=last)
        if last:
            epilogue(qt, po)
            del po_of[qt]
    n = len(jobs)
    for j in range(n + LOOK):
        if j < n:
            front(j)
        if j - LOOK >= 0:
            back(j - LOOK)


def build_b():
    cmpmask_np = nsa_tables()[0]
    nc = bass.Bass("TRN2", target_bir_lowering=False)
    with contextlib.ExitStack() as st:
        K = KB(nc, st)
        I = lambda n, s: K.dram(n, s, F32, "ExternalInput")
        swa_q, swa_k, swa_v = I("swa_qT", [4, 64, S]), I("swa_kT", [64, S]), I("swa_v", [S, 64])
        swa_alb, swa_sink = I("swa_alb", [128, 4, 2]), I("swa_sink", [128, 4])
        fox_q, fox_k, fox_v = I("fox_qT", [2, 128, S]), I("fox_kT", [2, 128, S]), I("fox_v", [2, S, 128])
        fox_fl, fox_fb = I("fox_fl", [2, S]), I("fox_fb", [2, 1])
        mla_qn, mla_qr, mla_kn = I("mla_qnT", [2, 128, S]), I("mla_qrT", [2, 64, S]), I("mla_knT", [2, 128, S])
        mla_kr, mla_v = I("mla_krT", [64, S]), I("mla_v", [2, S, 128])
        nsa_q = I("nsa_qT", [4, 128, S])
        nsa_kc, nsa_vc = I("nsa_kcT", [128, S]), I("nsa_vcT", [128, S])
        nsa_ks, nsa_vs = I("nsa_ksT", [128, S]), I("nsa_vs", [S, 128])
        nsa_kw, nsa_vw = I("nsa_kwT", [128, S]), I("nsa_vw", [S, 128])
        nsa_gl = I("nsa_gl", [S, 6])
        kc_pos, kc_w1, kc_w2 = I("kc_posT", [128, 32]), I("kc_w1", [S, 128]), I("kc_w2", [128, 128])
        vc_pos, vc_w1, vc_w2 = I("vc_posT", [128, 32]), I("vc_w1", [S, 128]), I("vc_w2", [128, 128])
        nsa_alb = I("nsa_alb", [128, 4, NQT])
        cmpb_d = I("cmpb", [128, 4, 2, NQT])
        cmpmask_d = I("cmpmask", [128, 2, NQT, 128])
        ovl_d, keep_d, fill_d, E_d = I("ovl", [128, 2, 64]), I("keep", [128, NQT, 64]), I("fill", [128, NQT, 64]), I("Emat", [64, S])
        mdiag_d, medge_d, ident_d = I("mdiag", [128, 128]), I("medge", [128, 128]), I("ident", [128, 128])
        out_d = K.dram("out", [S, 1024], F32, "ExternalOutput")
        cfox_d = K.dram("cfox", [2, S], F32, "ExternalOutput")

        def cload(name, src, shape, dt=BF16):
            t = K.sb(name, shape, dt)
            K.dma(t[:], src, eng=("gpsimd" if dt == BF16 else "sync"))
            return t
        mdiag = cload("mdiag_s", mdiag_d, [128, 128])
        medge = cload("medge_s", medge_d, [128, 128])
        identf = cload("identf", ident_d, [128, 128], F32)
        cmpmask = cload("cmpmask_s", cmpmask_d, [128, 2, NQT, 128])
        Emat = cload("Emat_s", E_d, [64, S])
        keep = cload("keep_s", keep_d, [128, NQT, 64], F32)
        fill = cload("fill_s", fill_d, [128, NQT, 64], F32)
        cmpb = cload("cmpb_s", cmpb_d, [128, 4, 2, NQT], F32)
        nalb = cload("nalb_s", nsa_alb, [128, 4, NQT], F32)
        salb = cload("salb_s", swa_alb, [128, 4, 2], F32)
        ssink = cload("ssink_s", swa_sink, [128, 4], F32)

        C = ACtx()
        C.oc = 0
        C.sc = 0
        C.ps_s = [K.ps("pss%d" % i, [128, 512], F32) for i in range(4)]
        C.ps_o = [K.ps("pso%d" % i, [128, 512], F32) for i in range(2)]
        C.pT = [K.sb("pT%d" % i, [128, 128], BF16) for i in range(4)]
        psx = [K.ps("psx%d" % i, [128, 512], F32) for i in range(2)]
        qb = [K.sb("qb%d" % i, [128, S], BF16) for i in range(2)]
        kb = [K.sb("kb%d" % i, [128, S], BF16) for i in range(1)] * 2
        q2b = [K.sb("q2b%d" % i, [64, S], BF16) for i in range(1)] * 2
        k2b = K.sb("k2b", [64, S], BF16)
        vb = [K.sb("vb%d" % i, [128, NQT, 129], BF16) for i in range(2)]
        ob = [K.sb("ob%d" % i, [128, NQT, 128], F32) for i in range(1)] * 2
        onsa = [K.sb("onsa%d" % i, [128, NQT, 128], F32) for i in range(2)]
        flt = [onsa[i][0:2].rr("p n d -> p (n d)") for i in range(2)]
        rinv = [K.sb("rinv%d" % i, [128, 1], F32) for i in range(4)]
        cnt = {"r": 0, "h": 0}

        def nr():
            cnt["r"] += 1
            return rinv[cnt["r"] % 4]

        def load_v(vbt, src, dv):
            K.dma(vbt[:, :, 0:dv], src.rr("(n p) d -> p n d", p=128), eng="gpsimd")
            K.memset(vbt[:, :, dv:dv + 1], 1.0)

        def causal(qt):
            return [(kt, (mdiag[:] if kt == qt else None)) for kt in range(qt + 1)]

        def band(nprev):
            def f(qt):
                l = []
                for kt in range(max(0, qt - nprev), qt + 1):
                    l.append((kt, mdiag[:] if kt == qt else (medge[:] if kt == qt - nprev else None)))
                return l
            return f

        def plain_epi(obuf, dv):
            def f(qt, po):
                r = nr()
                K.recip(r[:], po[:, dv:dv + 1])
                K.ts(obuf[:, qt, 0:dv], po[:, 0:dv], r[:, 0:1], None, ALU.mult)
            return f

        def store(obuf, col0, dv):
            K.dma(out_d[:, col0:col0 + dv].rr("(n p) d -> p n d", p=128), obuf[:, :, 0:dv])

        K.dma(k2b[:], swa_k, eng="gpsimd")
        load_v(vb[0], swa_v, 64)
        sinkc = K.sb("sinkc", [128, 4], F32)
        for h in range(4):
            K.act(sinkc[:, h:h + 1], salb[:, h, 0:1], AF.Exp, bias=ssink[:, h:h + 1])
        for h in range(4):
            qt_ = q2b[h % 2]
            K.dma(qt_[:], swa_q[h], eng="gpsimd")
            obuf = ob[cnt["h"] % 2]
            cnt["h"] += 1

            def epi(qt, po, h=h, obuf=obuf):
                r = nr()
                K.tt(r[:], po[:, 64:65], sinkc[:, h:h + 1], ALU.add)
                K.recip(r[:], r[:])
                K.ts(obuf[:, qt, 0:64], po[:, 0:64], r[:, 0:1], None, ALU.mult)
            attn_head(K, C, [qt_], [k2b], vb[0], 65, band(1), 0.125,
                      lambda kt, qt, h=h: salb[:, h, (qt - kt):(qt - kt) + 1], None, epi)
            store(obuf, 768 + h * 64, 64)

        fl, fl2 = flt
        fbn = K.sb("fbn", [2, 1], F32)
        K.dma(fl[:], fox_fl)
        K.dma(fbn[:], fox_fb)
        K.ts(fbn[:], fbn[:], -1.0, None, ALU.mult)
        K.act(fl[:], fl[:], AF.Exp, bias=fbn[:, 0:1], scale=-1.0)
        K.act(fl[:], fl[:], AF.Ln, bias=1.0)
        a, b_ = fl, fl2
        s = 1
        while s < S:
            K.copy(b_[:, 0:s], a[:, 0:s], eng="gpsimd")
            K.tt(b_[:, s:S], a[:, s:S], a[:, 0:S - s], ALU.add)
            a, b_ = b_, a
            s *= 2
        K.dma(cfox_d, a[:])
        cK = K.sb("cK", [128, 2, NQT], F32)
        cR = K.sb("cR", [128, 2, NQT], F32)
        for h in range(2):
            K.dma(cK[:, h, :], cfox_d[h].rr("(n p) -> p n", p=128), allow_slow_non_contiguous=True)
            K.dma(cR[:, h, :], V(cfox_d.ap[h, 127:S:128].partition_broadcast(128), cfox_d.res), allow_slow_non_contiguous=True)
        fbias = [K.sb("fbias%d" % i, [128, NQT], F32) for i in range(2)]
        for h in range(2):
            K.dma(qb[h % 2][:], fox_q[h], eng="gpsimd")
            K.dma(kb[h % 2][:], fox_k[h], eng="gpsimd")
            load_v(vb[(h + 1) % 2], fox_v[h], 128)
            obuf = ob[cnt["h"] % 2]
            cnt["h"] += 1
            fbs = {}

            def fb(kt, qt, h=h, fbs=fbs):
                if qt not in fbs:
                    t = fbias[qt % 2]
                    K.ts(t[:, 0:qt + 1], cK[:, h, 0:qt + 1], cR[:, h, qt:qt + 1], None, ALU.subtract)
                    fbs[qt] = t
                return fbs[qt][:, kt:kt + 1]
            attn_head(K, C, [qb[h % 2]], [kb[h % 2]], vb[(h + 1) % 2], 129, causal, 128 ** -0.5, fb, None,
                      plain_epi(obuf, 128))
            store(obuf, 256 + h * 128, 128)

        K.dma(k2b[:], mla_kr, eng="gpsimd")
        for h in range(2):
            K.dma(qb[h % 2][:], mla_qn[h], eng="gpsimd")
            K.dma(kb[h % 2][:], mla_kn[h], eng="gpsimd")
            K.dma(q2b[h % 2][:], mla_qr[h], eng="gpsimd")
            load_v(vb[(h + 1) % 2], mla_v[h], 128)
            obuf = ob[cnt["h"] % 2]
            cnt["h"] += 1
            attn_head(K, C, [qb[h % 2], q2b[h % 2]], [kb[h % 2], k2b], vb[(h + 1) % 2], 129, causal, 192 ** -0.5,
                      None, None, plain_epi(obuf, 128))
            store(obuf, 512 + h * 128, 128)

        sg = K.sb("sg", [128, NQT, 6], F32)
        K.dma(sg[:], nsa_gl.rr("(n p) c -> p n c", p=128))
        K.act(sg[:], sg[:], AF.Sigmoid)
        kcT = K.sb("kcT", [128, 256], BF16)
        vca = K.sb("vca", [128, 2, 193], BF16)
        w1b = K.sb("w1b", [128, 32, 128], BF16)
        w2b = K.sb("w2b", [128, 128], BF16)
        posT = K.sb("posT", [128, 32], F32)
        win = [K.sb("win%d" % i, [128, 255], BF16) for i in range(3)]
        hsb = K.sb("hsb", [128, 256], F32)
        h2 = K.sb("h2", [128, 256], F32)
        gT = K.sb("gT", [128, 256], BF16)
        K.memset(gT[:], 0.0)
        K.memset(kcT[:], 0.0)
        K.memset(vca[:], 0.0)
        K.dma(vca[:, :, 0:64], ovl_d, eng="gpsimd")
        K.memset(vca[:, 0, 64:65], 1.0)
        K.memset(vca[0:127, 1, 64:65], 1.0)
        for which, srcT, pos_d, w1_d, w2_d in (("k", nsa_kc, kc_pos, kc_w1, kc_w2), ("v", nsa_vc, vc_pos, vc_w1, vc_w2)):
            xT = qb[0]
            K.dma(xT[:], srcT, eng="gpsimd")
            K.dma(posT[:], pos_d)
            K.dma(w1b[:], w1_d.rr("(w d) n -> d w n", d=128), eng="gpsimd")
            K.dma(w2b[:], w2_d, eng="gpsimd")
            ph = psx[0]
            for w in range(32):
                wt = win[w % 3]
                K.ts(wt[:], xT[:, w:w + 16 * 254 + 1:16], posT[:, w:w + 1], None, ALU.add)
                K.mm(ph[:, 0:255], w1b[:, w, :], wt[:], start=(w == 0), stop=(w == 31))
            K.copy(hsb[:, 0:255], ph[:, 0:255])
            K.act(h2[:, 0:255], hsb[:, 0:255], AF.Square)
            K.ts(h2[:, 0:255], h2[:, 0:255], 0.044715, 1.0, ALU.mult, ALU.add)
            K.tt(h2[:, 0:255], h2[:, 0:255], hsb[:, 0:255], ALU.mult)
            K.act(h2[:, 0:255], h2[:, 0:255], AF.Tanh, scale=0.7978845608028654)
            K.ts(h2[:, 0:255], h2[:, 0:255], 1.0, None, ALU.add)
            K.stt(gT[:, 0:255], hsb[:, 0:255], 0.5, h2[:, 0:255], ALU.mult, ALU.mult)
            if which == "k":
                p2 = psx[1]
                K.mm(p2[:, 0:256], w2b[:], gT[:], start=True, stop=True)
                K.copy(kcT[:], p2[:, 0:256])
            else:
                for ct in range(2):
                    p2 = psx[1]
                    K.mm(p2[:, 0:128], gT[:, ct * 128:(ct + 1) * 128], w2b[:], start=True, stop=True)
                    K.copy(vca[:, ct, 65:193], p2[:, 0:128])
        imp = K.sb("imp", [128, NQT, 64], F32)

        def cmp_klist(qt):
            l = []
            for ct in range(2):
                m = cmpmask_np[:, ct, qt, :]
                if not m.any():
                    continue
                l.append((ct, None if m.all() else cmpmask[:, ct, qt, :]))
            return l
        for h in range(4):
            K.dma(qb[(h + 1) % 2][:], nsa_q[h], eng="gpsimd")

            def epi(qt, po, h=h):
                r = nr()
                K.ts(r[:], po[:, 64:65], 1e-30, None, ALU.max)
                K.recip(r[:], r[:])
                if h == 0:
                    K.ts(imp[:, qt, :], po[:, 0:64], r[:, 0:1], None, ALU.mult)
                else:
                    K.stt(imp[:, qt, :], po[:, 0:64], r[:, 0:1], imp[:, qt, :], ALU.mult, ALU.add)
                if h < 2:
                    K.tt(r[:], r[:], sg[:, qt, h:h + 1], ALU.mult)
                    K.ts(onsa[h][:, qt, :], po[:, 65:193], r[:, 0:1], None, ALU.mult)
            attn_head(K, C, [qb[(h + 1) % 2]], [kcT], vca, 193, cmp_klist, 128 ** -0.5,
                      lambda kt, qt, h=h: cmpb[:, h, kt, qt:qt + 1], None, epi)
        K.tt(imp[:], imp[:], keep[:], ALU.mult)
        K.tt(imp[:], imp[:], fill[:], ALU.add)
        selbT = K.sb("selbT", [64, S], BF16)
        m8 = [K.sb("m8_%d" % i, [128, 16], F32) for i in range(2)]
        wk = [K.sb("wk%d" % i, [128, 64], F32) for i in range(2)]
        sel = [K.sb("sel%d" % i, [128, 64], F32) for i in range(2)]
        for qt in range(NQT):
            m = m8[qt % 2]
            w_ = wk[qt % 2]
            sl = sel[qt % 2]
            K.max8(m[:, 0:8], imp[:, qt, :])
            K.match_replace(w_[:], m[:, 0:8], imp[:, qt, :], -3.0e6)
            K.max8(m[:, 8:16], w_[:])
            K.ts(sl[:], imp[:, qt, :], m[:, 15:16], None, ALU.is_ge)
            K.ts(sl[:], sl[:], BIG, -BIG, ALU.mult, ALU.add)
            pt_ = psx[qt % 2]
            K.tr(pt_[0:64, 0:128], sl[:], identf[:])
            K.copy(selbT[:, qt * 128:(qt + 1) * 128], pt_[0:64, 0:128], eng="scalar")
        for h in range(2):
            K.dma(qb[h % 2][:], nsa_q[h], eng="gpsimd")
            for br, kT_d, v_d, klf in ((1, nsa_ks, nsa_vs, causal), (2, nsa_kw, nsa_vw, band(4))):
                kbt = kb[br % 2]
                vbt = vb[br % 2]
                K.dma(kbt[:], kT_d, eng="gpsimd")
                load_v(vbt, v_d, 128)

                def epi(qt, po, h=h, br=br):
                    r = nr()
                    K.recip(r[:], po[:, 128:129])
                    K.tt(r[:], r[:], sg[:, qt, br * 2 + h:br * 2 + h + 1], ALU.mult)
                    K.stt(onsa[h][:, qt, :], po[:, 0:128], r[:, 0:1], onsa[h][:, qt, :], ALU.mult, ALU.add)
                ex = None
                if br == 1:
                    ex = lambda kt, qt: (Emat[:, kt * 128:(kt + 1) * 128], selbT[:, qt * 128:(qt + 1) * 128])
                attn_head(K, C, [qb[h % 2]], [kbt], vbt, 129, klf, 128 ** -0.5,
                          lambda kt, qt, h=h: nalb[:, h, (qt - kt):(qt - kt) + 1], ex, epi)
            store(onsa[h], h * 128, 128)
        K.S.emit()
    return nc


def _c(a):
    return np.ascontiguousarray(a, dtype=np.float32)


def b_const_inputs():
    cmpmask, ovl, keep, fill, E = nsa_tables()
    i = np.arange(128)
    return dict(cmpmask=_c(cmpmask), ovl=_c(ovl), keep=_c(keep), fill=_c(fill), Emat=_c(E),
                mdiag=_c(i[:, None] <= i[None, :]), medge=_c(i[:, None] > i[None, :]), ident=np.eye(128, dtype=np.float32))


def b_core_inputs(proj, q_mla, kv_mla, kro, P, l, hq):
    T = lambda a: _c(np.asarray(a).T)
    d = {}
    i = np.arange(128, dtype=np.float64)
    g = hq // 2
    heads = [4 * hq + r for r in range(4)]
    d["swa_qT"] = _c(np.stack([proj[:, 7008 + h * 64:7008 + (h + 1) * 64].T for h in heads]))
    d["swa_kT"] = T(proj[:, 8032 + g * 64:8032 + (g + 1) * 64])
    d["swa_v"] = _c(proj[:, 8032 + (2 + g) * 64:8032 + (3 + g) * 64])
    sl = np.array([2.0 ** (-(h + 1) / 2.0) for h in heads])
    d["swa_alb"] = _c(sl[None, :, None] * (i[:, None, None] - 63.5 - 128.0 * np.arange(2)[None, None, :]))
    d["swa_sink"] = _c(np.tile(P["swa_sinks"][l][heads][None, :], (128, 1)))
    fh = [2 * hq, 2 * hq + 1]
    bq = lambda w, h: proj[:, 2584 + (w * 8 + h) * 128:2584 + (w * 8 + h + 1) * 128]
    d["fox_qT"] = _c(np.stack([bq(0, h).T for h in fh]))
    d["fox_kT"] = _c(np.stack([bq(1, h).T for h in fh]))
    d["fox_v"] = _c(np.stack([bq(2, h) for h in fh]))
    d["fox_fl"] = _c(np.stack([proj[:, 5656 + h] for h in fh]))
    d["fox_fb"] = _c(P["fox_f_bias"][l][fh][:, None])
    q3 = q_mla.reshape(S, 8, 192)
    kv3 = kv_mla.reshape(S, 8, 256)
    d["mla_qnT"] = _c(np.stack([q3[:, h, 0:128].T for h in fh]))
    d["mla_qrT"] = _c(np.stack([q3[:, h, 128:192].T for h in fh]))
    d["mla_knT"] = _c(np.stack([kv3[:, h, 0:128].T for h in fh]))
    d["mla_krT"] = T(kro)
    d["mla_v"] = _c(np.stack([kv3[:, h, 128:256] for h in fh]))
    mine = [2 * hq, 2 * hq + 1]
    nh = mine + [h for h in range(4 * g, 4 * g + 4) if h not in mine]
    d["nsa_qT"] = _c(np.stack([proj[:, h * 128:(h + 1) * 128].T for h in nh]))
    akv = lambda br, kvi: proj[:, 1024 + ((br * 2 + kvi) * 2 + g) * 128:1024 + ((br * 2 + kvi) * 2 + g + 1) * 128]
    d["nsa_kcT"], d["nsa_vcT"] = T(akv(0, 0)), T(akv(0, 1))
    d["nsa_ksT"], d["nsa_vs"] = T(akv(1, 0)), _c(akv(1, 1))
    d["nsa_kwT"], d["nsa_vw"] = T(akv(2, 0)), _c(akv(2, 1))
    d["nsa_gl"] = _c(np.stack([proj[:, 2560 + br * 8 + h] for br in range(3) for h in mine], axis=1))
    d["kc_posT"], d["kc_w1"], d["kc_w2"] = T(P["nsa_kc_pos"][l]), _c(P["nsa_kc_w1"][l]), _c(P["nsa_kc_w2"][l])
    d["vc_posT"], d["vc_w1"], d["vc_w2"] = T(P["nsa_vc_pos"][l]), _c(P["nsa_vc_w1"][l]), _c(P["nsa_vc_w2"][l])
    nsl = np.array([2.0 ** (-(h + 1)) for h in nh])
    d["nsa_alb"] = _c(nsl[None, :, None] * (i[:, None, None] - 63.5 - 128.0 * np.arange(NQT)[None, None, :]))
    cend = 16.0 * (np.arange(2)[None, :, None] * 128 + i[:, None, None]) + 31.0
    tref = np.arange(NQT)[None, None, :] * 128 + 63.5
    cb = nsl[None, :, None, None] * (cend - tref)[:, None, :, :]
    d["cmpb"] = _c(np.minimum(cb, 40.0))
    return d


def build_d():
    nc = bass.Bass("TRN2", target_bir_lowering=False)
    with contextlib.ExitStack() as st:
        K = KB(nc, st)
        I = lambda n, s, dt=F32: K.dram(n, s, dt, "ExternalInput")
        mg_d, rt_d, g8_d = I("mg", [NTOK, 8]), I("rtall", [NTOK, 4]), I("g8", [1])
        xn2_d = I("xn2", [NTOK, D], BF16)
        gffn_d = I("gffn", [D])
        wg_d, wu_d, wd_d = I("wg", [8, D, 384]), I("wu", [8, D, 384]), I("wd", [8, 384, D])
        tri_d, io384_d, pn_d = I("tri", [128, 128]), I("iota384", [128, 384]), I("pn", [128, NTT, 2])
        ident_d, io8_d = I("ident", [128, 128]), I("iota8", [128, 8])
        y_d = K.dram("y", [8 * CE, D], F32, "ExternalOutput")
        dl_d = K.dram("destl", [NTOK, 2], F32, "ExternalOutput")

        identf = K.sb("identf", [128, 128], F32)
        identb = K.sb("identb", [128, 128], BF16)
        trif = K.sb("trif", [128, 128], F32)
        trib = K.sb("trib", [128, 128], BF16)
        oneb = K.sb("oneb", [128, 128], BF16)
        io384 = K.sb("io384", [128, 384], F32)
        io8 = K.sb("io8", [128, 8], F32)
        pnf = K.sb("pnf", [128, NTT, 2], F32)
        pnb = K.sb("pnb", [128, NTT, 2], BF16)
        g2col = K.sb("g2col", [128, 32], F32)
        g8 = K.sb("g8s", [128, 1], F32)
        Mt = K.sb("Mt", [128, NTT, 8], F32)
        Mb = K.sb("Mb", [128, NTT, 8], BF16)
        Rf = K.sb("Rf", [128, NTT, 8], F32)
        Rb = K.sb("Rb", [128, NTT, 8], BF16)
        slot = K.sb("slot", [128, NTT, 8], F32)
        oh = K.sb("ohd", [128, NTT, 8], F32)
        rt = K.sb("rt", [128, NTT, 4], F32)
        rel = K.sb("rel", [128, NTT], F32)
        sl = K.sb("sl", [128, NTT], F32)
        dls = K.sb("dls", [128, NTT, 2], F32)
        OHb = [K.sb("OHb%d" % i, [128, 384], BF16) for i in range(4)]
        idxf = K.sb("idxf", [128, 3, 8, 2], F32)
        idxv = K.sb("idxv", [128, 3, 8], F32)
        idxi = K.sb("idxi", [128, 3, 8], I32)
        B = [K.ps("B%d" % i, [128, 512], F32) for i in range(4)]
        tpb = [K.ps("tpb%d" % i, [128, 8, 128], BF16) for i in range(2)]
        psy = [K.ps("psy%d" % i, [128, 512], F32) for i in range(2)]

        K.dma(identf[:], ident_d)
        K.copy(identb[:], identf[:])
        K.dma(trif[:], tri_d)
        K.copy(trib[:], trif[:])
        K.memset(oneb[:], 1.0)
        K.dma(io384[:], io384_d)
        K.dma(io8[:], io8_d)
        K.dma(pnf[:], pn_d)
        K.copy(pnb[:], pnf[:])
        K.dma(g2col[:], col_view(gffn_d), allow_slow_non_contiguous=True)
        K.dma(g8[:], V(g8_d.ap.partition_broadcast(128), g8_d.res))
        K.dma(Mt[:], mg_d.rr("(n p) e -> p n e", p=128))
        K.dma(rt[:], rt_d.rr("(n p) k -> p n k", p=128))
        K.copy(Mb[:], Mt[:])
        K.memset(Rf[:, 0, :], 0.0)
        for n in range(1, NTT):
            K.tt(Rf[:, n, :], Rf[:, n - 1, :], Mt[:, n - 1, :], ALU.add)
        K.copy(Rb[:], Rf[:])
        sv = B[0][:].rr("p (n e) -> p n e", e=8)
        for n in range(NTT):
            K.mm(sv[:, n, :], trib[:], Mb[:, n, :], start=True, stop=False)
            K.mm(sv[:, n, :], oneb[:], Rb[:, n, :], start=False, stop=True)
        K.copy(slot[:], sv)
        io8_3 = io8[:].un(1).bc([128, NTT, 8])
        for k in range(2):
            K.ts(rel[:], rt[:, :, k], g8[:, 0:1], None, ALU.subtract)
            K.tt(oh[:], io8_3, rel[:].un(2).bc([128, NTT, 8]), ALU.is_equal)
            K.tt(oh[:], oh[:], slot[:], ALU.mult)
            K.reduce(sl[:], oh[:], ALU.add)
            K.stt(dls[:, :, k], rel[:], float(CE), sl[:], ALU.mult, ALU.add)
        K.dma(dl_d.rr("(n p) k -> p n k", p=128), dls[:])
        psi = [B[1 + s_][:, 0:16].rr("p (e k) -> p e k", k=2) for s_ in range(3)]
        oc = 0
        for e in range(8):
            for n in range(NTT):
                o_ = OHb[oc % 4]
                oc += 1
                K.ts(o_[:], io384[:], slot[:, n, e:e + 1], Mt[:, n, e:e + 1], ALU.is_equal, ALU.mult)
                for s_ in range(3):
                    K.mm(psi[s_][:, e, :], o_[:, s_ * 128:(s_ + 1) * 128], pnb[:, n, :], start=(n == 0), stop=(n == NTT - 1))
        for s_ in range(3):
            K.copy(idxf[:, s_, :, :], psi[s_])
        K.stt(idxv[:], idxf[:, :, :, 1], 128.0, idxf[:, :, :, 0], ALU.mult, ALU.add)
        K.copy(idxi[:], idxv[:])
        XeT = K.sb("XeT", [128, 32, CE], BF16)
        wgb = [K.sb("wgb%d" % i, [128, 32, 384], BF16) for i in range(2)]
        wub = [K.sb("wub%d" % i, [128, 32, 384], BF16) for i in range(2)]
        wdb = K.sb("wdb", [128, 3, D], BF16)
        xg = [K.sb("xg%d" % i, [128, D], BF16) for i in range(2)]
        ysb = K.sb("ysb", [128, D], F32)
        actT = K.sb("actT", [128, 3, CE], BF16)
        sil = [K.sb("sil%d" % i, [128, CE], F32) for i in range(2)]
        gc = 0
        for e in range(8):
            K.dma(wgb[e % 2][:], wg_d[e].rr("(c p) f -> p c f", p=128), eng="gpsimd")
            K.dma(wub[e % 2][:], wu_d[e].rr("(c p) f -> p c f", p=128), eng="gpsimd")
            for s_ in range(3):
                x_ = xg[gc % 2]
                gc += 1
                K.gather(x_[:], xn2_d, idxi[:, s_, e:e + 1])
                for k in range(4):
                    tp = tpb[k % 2]
                    for c in range(8):
                        cc = k * 8 + c
                        K.tr(tp[:, c, :], x_[:, cc * 128:(cc + 1) * 128], identb[:])
                    K.tt(XeT.sub(k, (slice(None), slice(k * 8, k * 8 + 8), slice(s_ * 128, (s_ + 1) * 128))),
                         tp[:], g2col[:, k * 8:k * 8 + 8].un(2).bc([128, 8, 128]), ALU.mult)
            K.dma(wdb[:], wd_d[e].rr("(c p) n -> p c n", p=128), eng="gpsimd")
            for f in range(3):
                pg, pu = B[f % 2], B[2 + f % 2]
                for c in range(32):
                    K.mm(pg[:, 0:CE], wgb[e % 2][:, c, f * 128:(f + 1) * 128],
                         XeT.sub(c // 8, (slice(None), c, slice(None))), start=(c == 0), stop=(c == 31))
                for c in range(32):
                    K.mm(pu[:, 0:CE], wub[e % 2][:, c, f * 128:(f + 1) * 128],
                         XeT.sub(c // 8, (slice(None), c, slice(None))), start=(c == 0), stop=(c == 31))
                K.act(sil[f % 2][:], pg[:, 0:CE], AF.Silu)
                K.tt(actT[:, f, :], sil[f % 2][:], pu[:, 0:CE], ALU.mult)
            for s_ in range(3):
                for j in range(8):
                    py = psy[j % 2]
                    for f in range(3):
                        K.mm(py[:], actT[:, f, s_ * 128:(s_ + 1) * 128], wdb[:, f, j * 512:(j + 1) * 512],
                             start=(f == 0), stop=(f == 2))
                    K.copy(ysb[:, j * 512:(j + 1) * 512], py[:], eng=("scalar" if j % 2 else "vector"))
                r0 = e * CE + s_ * 128
                K.dma(y_d[r0:r0 + 128, :], ysb[:])
        K.S.emit()
    return nc


def build_e(final):
    nc = bass.Bass("TRN2", target_bir_lowering=False)
    with contextlib.ExitStack() as st:
        K = KB(nc, st)
        I = lambda n, s, dt=F32: K.dram(n, s, dt, "ExternalInput")
        xm_d, y_d = I("xmid", [NT, D]), I("yall", [64 * CE, D])
        dl8_d, rt_d = I("destl8", [8, NT, 2]), I("rt", [NT, 4])
        lo8_d, base8_d, gfin_d = I("lo8", [128, 8]), I("base8", [128, 8]), I("gfin", [D])
        out_d = K.dram("xout", [NT, D], F32, "ExternalOutput")
        lo8 = K.sb("lo8s", [128, 8], F32)
        base8 = K.sb("base8s", [128, 8], F32)
        K.dma(lo8[:], lo8_d)
        K.dma(base8[:], base8_d)
        gb = K.sb("gb", [128, D], F32)
        if final:
            K.dma(gb[:], V(gfin_d.ap.partition_broadcast(128), gfin_d.res))
        xm = K.sb("xm", [128, D], F32)
        ya = K.sb("ya", [128, D], F32)
        yb = K.sb("yb", [128, D], F32)
        acc = K.sb("acc", [128, D], F32)
        junk = K.sb("junk", [128, D], BF16)
        rts = K.sb("rts", [128, 4], F32)
        dl8 = K.sb("dl8", [128, 8, 2], F32)
        g1 = K.sb("g1", [128, 8], F32)
        g2 = K.sb("g2", [128, 8], F32)
        tmp8 = K.sb("tmp8", [128, 8], F32)
        df = K.sb("df", [128, 2], F32)
        di = K.sb("di", [128, 2], I32)
        ss = K.sb("ss", [128, 1], F32)
        tmp = K.sb("tmp", [128, 1], F32)
        rs = K.sb("rs", [128, 1], F32)
        for n in range(NT // 128):
            r0 = n * 128
            K.dma(xm[:], xm_d[r0:r0 + 128, :])
            K.dma(rts[:], rt_d[r0:r0 + 128, :])
            K.dma(dl8[:], dl8_d[:, r0:r0 + 128, :].rr("g t k -> t g k"))
            K.ts(g1[:], lo8[:], rts[:, 0:1], None, ALU.is_le)
            K.ts(g2[:], lo8[:], 8.0, rts[:, 0:1], ALU.add, ALU.is_gt)
            K.tt(g1[:], g1[:], g2[:], ALU.mult)
            for k in range(2):
                K.tt(tmp8[:], dl8[:, :, k], base8[:], ALU.add)
                K.tt(tmp8[:], tmp8[:], g1[:], ALU.mult)
                K.reduce(df[:, k:k + 1], tmp8[:], ALU.add)
            K.copy(di[:], df[:])
            K.gather(ya[:], y_d, di[:, 0:1])
            K.gather(yb[:], y_d, di[:, 1:2])
            K.stt(acc[:], ya[:], rts[:, 2:3], xm[:], ALU.mult, ALU.add)
            K.stt(acc[:], yb[:], rts[:, 3:4], acc[:], ALU.mult, ALU.add)
            if final:
                K.act(junk[:], acc[:], AF.Square, accum=ss[:])
                K.rstd(rs[:], ss[:], float(D), tmp[:])
                K.stt(acc[:], acc[:], rs[:, 0:1], gb[:], ALU.mult, ALU.mult)
            K.dma(out_d[r0:r0 + 128, :], acc[:])
        K.S.emit()
    return nc


_PROGS = {}
_DBG = None
_DBG_STOP = False


def _prog(name, fn):
    if name not in _PROGS:
        _PROGS[name] = fn()
    return _PROGS[name]


def _run(name, fn, in_maps):
    res = run_bass_kernel_spmd(_prog(name, fn), in_maps, core_ids=list(range(8)))
    return res.results


CE2 = 1024
CG = 4096


def build_d2():
    nc = bass.Bass("TRN2", target_bir_lowering=False)
    NS = CE2 // 128
    with contextlib.ExitStack() as st:
        K = KB(nc, st)
        I = lambda n, s, dt=F32: K.dram(n, s, dt, "ExternalInput")
        mg_d, rt_d, g8_d = I("mg", [NTOK, 8]), I("rtall", [NTOK, 4]), I("g8", [1])
        xn2_d = I("xn2", [NTOK, D], BF16)
        gffn_d = I("gffn", [D])
        wg_d, wu_d, wd_d = I("wg", [8, D, 384]), I("wu", [8, D, 384]), I("wd", [8, 384, D])
        tri_d, tok_d = I("tri", [128, 128]), I("tokid", [128, NTT])
        ident_d, io8_d, lo8_d = I("ident", [128, 128]), I("iota8", [128, 8]), I("lo8", [128, 8])
        tr1_d, tr2_d = I("trash1", [128, 1]), I("trash2", [128, 1])
        y_d = K.dram("y", [8 * CE2, D], F32, "ExternalOutput")
        z_d = K.dram("z", [CG, D], F32, "ExternalOutput")
        inv_d = K.dram("inv", [128, NTT], I32, "ExternalOutput")
        info_d = K.dram("info", [NTOK, 4], F32, "ExternalOutput")
        idx_d = K.dram("idxl", [8 * CE2 + 128, 2], I32, "ExternalOutput")
        tl_d = K.dram("tokl", [CG + 128, 2], I32, "ExternalOutput")

        identf = K.sb("identf", [128, 128], F32)
        identb = K.sb("identb", [128, 128], BF16)
        trif = K.sb("trif", [128, 128], F32)
        trib = K.sb("trib", [128, 128], BF16)
        oneb = K.sb("oneb", [128, 128], BF16)
        io8 = K.sb("io8", [128, 8], F32)
        lo8 = K.sb("lo8s", [128, 8], F32)
        tokf = K.sb("tokf", [128, NTT], F32)
        toki = K.sb("toki", [128, NTT, 2], I32)
        g2col = K.sb("g2col", [128, 32], F32)
        g8 = K.sb("g8s", [128, 1], F32)
        tr1 = K.sb("tr1", [128, 1], F32)
        tr2 = K.sb("tr2", [128, 1], F32)
        Mt = K.sb("Mt", [128, NTT, 8], F32)
        Mb = K.sb("Mb", [128, NTT, 8], BF16)
        Rf = K.sb("Rf", [128, NTT, 8], F32)
        Rb = K.sb("Rb", [128, NTT, 8], BF16)
        slot = K.sb("slot", [128, NTT, 8], F32)
        G8 = K.sb("G8", [128, NTT, 8], F32)
        posa = K.sb("posa", [128, NTT, 8], F32)
        oh = K.sb("ohd", [128, NTT, 8], F32)
        oh2 = K.sb("ohd2", [128, NTT, 8], F32)
        rt = K.sb("rt", [128, NTT, 4], F32)
        rel = K.sb("rel", [128, NTT], F32)
        sl = K.sb("sl", [128, NTT], F32)
        okk = K.sb("okk", [128, NTT], F32)
        ok2 = K.sb("ok2", [128, NTT], F32)
        ing = K.sb("ing", [128, NTT], F32)
        dtmp = K.sb("dtmp", [128, NTT], F32)
        info = K.sb("infos", [128, NTT, 4], F32)
        di = [K.sb("di%d" % k, [128, NTT], I32) for k in range(3)]
        invf = K.sb("invf", [128, NTT], F32)
        invi = K.sb("invi", [128, NTT], I32)
        zt = K.sb("zt", [128, (8 * CE2 + 128) // 128, 2], I32)
        pgp = K.ps("pgp", [128, CE2], F32)
        pup = K.ps("pup", [128, CE2], F32)
        tpb = [K.ps("tpb%d" % i, [128, 8, 128], BF16) for i in range(2)]
        psy = [K.ps("psy%d" % i, [128, 512], F32) for i in range(2)]

        K.dma(identf[:], ident_d)
        K.copy(identb[:], identf[:])
        K.dma(trif[:], tri_d)
        K.copy(trib[:], trif[:])
        K.memset(oneb[:], 1.0)
        K.dma(io8[:], io8_d)
        K.dma(lo8[:], lo8_d)
        K.dma(tokf[:], tok_d)
        K.copy(toki[:, :, 0], tokf[:])
        K.copy(toki[:, :, 1], tokf[:])
        K.dma(tr1[:], tr1_d)
        K.dma(tr2[:], tr2_d)
        K.dma(g2col[:], col_view(gffn_d), allow_slow_non_contiguous=True)
        K.dma(g8[:], V(g8_d.ap.partition_broadcast(128), g8_d.res))
        K.dma(Mt[:], mg_d.rr("(n p) e -> p n e", p=128))
        K.dma(rt[:], rt_d.rr("(n p) k -> p n k", p=128))
        K.memset(zt[:], 0)
        idx_rs = [Res("idxr%d" % i) for i in range(6)]
        tl_rs = [Res("tlr%d" % i) for i in range(3)]
        K.dma(idx_d.rr("(s p) o -> p s o", p=128), zt[:], xw=idx_rs)
        K.dma(tl_d.rr("(s p) o -> p s o", p=128), zt[:, 0:(CG + 128) // 128, :], xw=tl_rs)

        def excl_cumsum(dst, src_f, pv):
            K.copy(Mb[:], src_f)
            K.memset(Rf[:, 0, :], 0.0)
            for n in range(1, NTT):
                K.tt(Rf[:, n, :], Rf[:, n - 1, :], src_f[:, n - 1, :], ALU.add)
            K.copy(Rb[:], Rf[:])
            for n in range(NTT):
                K.mm(pv[:, n, :], trib[:], Mb[:, n, :], start=True, stop=False)
                K.mm(pv[:, n, :], oneb[:], Rb[:, n, :], start=False, stop=True)
            K.copy(dst, pv)
        sv = psy[0][:].rr("p (n e) -> p n e", e=8)
        excl_cumsum(slot[:], Mt[:], sv)
        io8_3 = io8[:].un(1).bc([128, NTT, 8])
        lo8_3 = lo8[:].un(1).bc([128, NTT, 8])
        ea3 = rt[:, :, 0].un(2).bc([128, NTT, 8])
        K.tt(G8[:], lo8_3, ea3, ALU.is_le)
        K.ts(oh[:], lo8_3, 8.0, None, ALU.add)
        K.tt(oh[:], oh[:], ea3, ALU.is_gt)
        K.tt(G8[:], G8[:], oh[:], ALU.mult)
        sv2 = psy[1][:].rr("p (n e) -> p n e", e=8)
        excl_cumsum(posa[:], G8[:], sv2)
        K.tt(oh[:], G8[:], posa[:], ALU.mult)
        K.reduce(sl[:], oh[:], ALU.add)
        K.tt(oh[:], G8[:], io8_3, ALU.mult)
        K.reduce(dtmp[:], oh[:], ALU.add)
        K.stt(invf[:], dtmp[:], float(CG), sl[:], ALU.mult, ALU.add)
        K.copy(invi[:], invf[:])
        K.dma(inv_d, invi[:])
        K.ts(rel[:], rt[:, :, 0], g8[:, 0:1], None, ALU.subtract)
        K.ts(ing[:], rel[:], 0.0, None, ALU.is_ge)
        K.ts(ok2[:], rel[:], 8.0, None, ALU.is_lt)
        K.tt(ing[:], ing[:], ok2[:], ALU.mult)
        K.ts(ok2[:], sl[:], float(CG), None, ALU.is_lt)
        K.tt(ok2[:], ok2[:], ing[:], ALU.mult)
        K.ts(dtmp[:], sl[:], tr2[:, 0:1], None, ALU.subtract)
        K.tt(dtmp[:], dtmp[:], ok2[:], ALU.mult)
        K.ts(dtmp[:], dtmp[:], tr2[:, 0:1], None, ALU.add)
        K.copy(di[2][:], dtmp[:])
        for k in range(2):
            K.ts(rel[:], rt[:, :, k], g8[:, 0:1], None, ALU.subtract)
            K.tt(oh2[:], io8_3, rel[:].un(2).bc([128, NTT, 8]), ALU.is_equal)
            K.tt(oh2[:], oh2[:], slot[:], ALU.mult)
            K.reduce(sl[:], oh2[:], ALU.add)
            K.stt(info[:, :, k], rel[:], float(CE2), sl[:], ALU.mult, ALU.add)
            K.copy(info[:, :, 2 + k], rt[:, :, 2 + k])
            K.ts(okk[:], sl[:], float(CE2), None, ALU.is_lt)
            K.tt(okk[:], okk[:], ing[:], ALU.mult)
            K.ts(dtmp[:], info[:, :, k], tr1[:, 0:1], None, ALU.subtract)
            K.tt(dtmp[:], dtmp[:], okk[:], ALU.mult)
            K.ts(dtmp[:], dtmp[:], tr1[:, 0:1], None, ALU.add)
            K.copy(di[k][:], dtmp[:])
        K.dma(info_d.rr("(n p) k -> p n k", p=128), info[:])
        for n in range(NTT):
            for k in range(2):
                K.scatter(V(idx_d.ap, idx_rs[(2 * n + k) % 6]), di[k][:, n:n + 1], toki[:, n, :])
            K.scatter(V(tl_d.ap, tl_rs[n % 3]), di[2][:, n:n + 1], toki[:, n, :])
        XeT = K.sb("XeT", [128, 32, CE2], BF16)
        wgb = K.sb("wgb", [128, 32, 384], BF16)
        wub = K.sb("wub", [128, 32, 384], BF16)
        wdb = K.sb("wdb", [128, 3, D], BF16)
        xg = [K.sb("xg%d" % i, [128, D], BF16) for i in range(2)]
        ysb = K.sb("ysb", [128, D], F32)
        actT = K.sb("actT", [128, 3, CE2], BF16)
        sil = K.sb("sil", [128, CE2], F32)
        idxt = K.sb("idxt", [128, NS, 2], I32)
        gc = 0
        for e in range(8):
            K.dma(wgb[:], wg_d[e].rr("(c p) f -> p c f", p=128), eng="gpsimd")
            K.dma(wub[:], wu_d[e].rr("(c p) f -> p c f", p=128), eng="gpsimd")
            K.dma(idxt[:], idx_d[e * CE2:(e + 1) * CE2, :].rr("(s p) o -> p s o", p=128), xr=idx_rs)
            for s_ in range(NS):
                x_ = xg[gc % 2]
                gc += 1
                K.gather(x_[:], xn2_d, idxt[:, s_, 0:1])
                for k in range(4):
                    tp = tpb[k % 2]
                    for c in range(8):
                        cc = k * 8 + c
                        K.tr(tp[:, c, :], x_[:, cc * 128:(cc + 1) * 128], identb[:])
                    K.tt(XeT.sub(k, (slice(None), slice(k * 8, k * 8 + 8), slice(s_ * 128, (s_ + 1) * 128))),
                         tp[:], g2col[:, k * 8:k * 8 + 8].un(2).bc([128, 8, 128]), ALU.mult)
            K.dma(wdb[:], wd_d[e].rr("(c p) n -> p c n", p=128), eng="gpsimd")
            for f in range(3):
                for (wb_, pp) in ((wgb, pgp), (wub, pup)):
                    for hh in range(CE2 // 512):
                        for c in range(32):
                            K.mm(pp.sub(hh, (slice(None), slice(hh * 512, (hh + 1) * 512))), wb_[:, c, f * 128:(f + 1) * 128],
                                 XeT.sub(c // 8, (slice(None), c, slice(hh * 512, (hh + 1) * 512))), start=(c == 0), stop=(c == 31))
                for hh in range(CE2 // 512):
                    hs = slice(hh * 512, (hh + 1) * 512)
                    K.act(sil[:, hs], pgp.sub(hh, (slice(None), hs)), AF.Silu)
                    K.tt(actT[:, f, hs], sil[:, hs], pup.sub(hh, (slice(None), hs)), ALU.mult)
            for s_ in range(NS):
                for j in range(8):
                    py = psy[j % 2]
                    for f in range(3):
                        K.mm(py[:], actT[:, f, s_ * 128:(s_ + 1) * 128], wdb[:, f, j * 512:(j + 1) * 512],
                             start=(f == 0), stop=(f == 2))
                    K.copy(ysb[:, j * 512:(j + 1) * 512], py[:], eng=("scalar" if j % 2 else "vector"))
                r0 = e * CE2 + s_ * 128
                K.dma(y_d[r0:r0 + 128, :], ysb[:])
        tlt = K.sb("tlt", [128, CG // 128, 2], I32)
        inf = [K.sb("inf%d" % i, [128, 4], F32) for i in range(2)]
        ii = [K.sb("ii%d" % i, [128, 2], I32) for i in range(2)]
        cb = [V(XeT.h[:, 8 * k:8 * k + 8, :].rearrange("p c t -> p (c t)").bitcast(F32), XeT.sub(k).res) for k in range(4)]
        K.dma(tlt[:], tl_d[0:CG, :].rr("(s p) o -> p s o", p=128), xr=tl_rs)
        for j in range(CG // 128):
            f_ = inf[j % 2]
            i_ = ii[j % 2]
            ya, yb = cb[(2 * j) % 4], cb[(2 * j + 1) % 4]
            K.gather(f_[:], info_d, tlt[:, j, 0:1])
            K.ts(f_[:, 0:2], f_[:, 0:2], 0.0, float(8 * CE2 - 1), ALU.max, ALU.min)
            K.copy(i_[:], f_[:, 0:2])
            K.gather(ya[:], y_d, i_[:, 0:1])
            K.gather(yb[:], y_d, i_[:, 1:2])
            K.ts(ya[:], ya[:], f_[:, 2:3], None, ALU.mult)
            K.stt(ya[:], yb[:], f_[:, 3:4], ya[:], ALU.mult, ALU.add)
            K.dma(z_d[j * 128:(j + 1) * 128, :], ya[:])
        K.S.emit()
    return nc


def build_f():
    nc = bass.Bass("TRN2", target_bir_lowering=False)
    with contextlib.ExitStack() as st:
        K = KB(nc, st)
        x_d = K.dram("x", [NT, D], F32, "ExternalInput")
        xa_d = K.dram("xadd", [NT, D], F32, "ExternalInput")
        g_d = K.dram("g", [D], F32, "ExternalInput")
        o_d = K.dram("xout", [NT, D], F32, "ExternalOutput")
        gb = K.sb("gb", [128, D], F32)
        K.dma(gb[:], V(g_d.ap.partition_broadcast(128), g_d.res))
        xa = [K.sb("xa%d" % i, [128, D], F32) for i in range(2)]
        xb = [K.sb("xb%d" % i, [128, D], F32) for i in range(2)]
        junk = K.sb("junk", [128, D], BF16)
        ss = [K.sb("ss%d" % i, [128, 1], F32) for i in range(2)]
        tmp = [K.sb("tmp%d" % i, [128, 1], F32) for i in range(2)]
        rs = [K.sb("rs%d" % i, [128, 1], F32) for i in range(2)]
        for n in range(NT // 128):
            a, b = xa[n % 2], xb[n % 2]
            K.dma(a[:], x_d[n * 128:(n + 1) * 128, :])
            K.dma(b[:], xa_d[n * 128:(n + 1) * 128, :])
            K.tt(a[:], a[:], b[:], ALU.add)
            K.act(junk[:], a[:], AF.Square, accum=ss[n % 2][:])
            K.rstd(rs[n % 2][:], ss[n % 2][:], float(D), tmp[n % 2][:])
            K.stt(b[:], a[:], rs[n % 2][:, 0:1], gb[:], ALU.mult, ALU.mult)
            K.dma(o_d[n * 128:(n + 1) * 128, :], b[:])
        K.S.emit()
    return nc


def kernel(**P):
    P = {k: np.asarray(v) for k, v in P.items()}
    xmid = _c(P["x"]).reshape(NTOK, D)
    moe = np.zeros((NTOK, D), np.float32)
    ident = np.eye(128, dtype=np.float32)
    i128 = np.arange(128)
    iota64 = _c(np.tile(np.arange(64)[None, :], (128, 1)))
    tri = _c(np.triu(np.ones((128, 128)), 1))
    iota8 = _c(np.tile(np.arange(8)[None, :], (128, 1)))
    lo8 = _c(iota8 * 8.0)
    tokid = _c(np.arange(NTT)[None, :] * 128 + i128[:, None])
    trash1 = _c((8 * CE2 + i128)[:, None])
    trash2 = _c((CG + i128)[:, None])
    pos = np.arange(S, dtype=np.float32)
    inv = (10000.0 ** (-np.arange(0, 64, 2, dtype=np.float32) / 64)).astype(np.float32)
    ang = pos[:, None] * inv[None, :]
    cosT, sinT = np.cos(ang).astype(np.float32), np.sin(ang).astype(np.float32)
    bconst = b_const_inputs()
    sh = lambda a, c: a[c * NT:(c + 1) * NT]
    for l in range(2):
        r = _run("a", build_a, [dict(x=_c(sh(xmid, c)), xadd=_c(sh(moe, c)), g=_c(P["norm_mix_g"][l]), w=_c(P["w_in"][l]),
                                     ident=ident) for c in range(8)])
        proj = np.concatenate([r[c]["proj"] for c in range(8)], axis=0)
        x = np.concatenate([r[c]["xsum"] for c in range(8)], axis=0)
        r = _run("a2", build_a2, [dict(cq=_c(sh(proj, c)[:, 5664:6432]), ckv=_c(sh(proj, c)[:, 6432:6944]),
                                       kr=_c(sh(proj, c)[:, 6944:7008]), gq=_c(P["mla_q_norm_g"][l]),
                                       gkv=_c(P["mla_kv_norm_g"][l]), wuq=_c(P["mla_w_uq"][l]), wukv=_c(P["mla_w_ukv"][l]),
                                       cos=_c(cosT[(c % 4) * NT:(c % 4 + 1) * NT]), sin=_c(sinT[(c % 4) * NT:(c % 4 + 1) * NT]),
                                       ident=ident) for c in range(8)])
        qm = np.concatenate([r[c]["q"] for c in range(8)], axis=0)
        kvm = np.concatenate([r[c]["kv"] for c in range(8)], axis=0)
        krm = np.concatenate([r[c]["kro"] for c in range(8)], axis=0)
        maps = []
        for c in range(8):
            b, hq = c // 4, c % 4
            bs = slice(b * S, (b + 1) * S)
            d = dict(bconst)
            d.update(b_core_inputs(proj[bs], qm[bs], kvm[bs], krm[bs], P, l, hq))
            maps.append(d)
        r = _run("b", build_b, maps)
        o_all = np.empty((NTOK, D), np.float32)
        for c in range(8):
            b, hq = c // 4, c % 4
            o = r[c]["out"]
            for gi in range(4):
                o_all[b * S:(b + 1) * S, gi * 1024 + hq * 256:gi * 1024 + (hq + 1) * 256] = o[:, gi * 256:(gi + 1) * 256]
        rw = _c(np.concatenate([P["router_group_w"][l], P["router_expert_w"][l]], axis=1))
        rb = _c(np.concatenate([P["router_group_b"][l], P["router_expert_b"][l]]))
        r = _run("c", build_c, [dict(o=_c(sh(o_all, c)), x=_c(sh(x, c)), gout=_c(P["out_norm_g"][l]), wout=_c(P["w_out"][l]),
                                     gffn=_c(P["norm_ffn_g"][l]), rw=rw, rb=rb, ident=ident, iota64=iota64) for c in range(8)])
        xmid = np.concatenate([r[c]["xmid"] for c in range(8)], axis=0)
        xn2 = np.concatenate([r[c]["xn2"] for c in range(8)], axis=0)
        mall = np.concatenate([r[c]["mroute"] for c in range(8)], axis=0)
        rtall = np.concatenate([r[c]["route"] for c in range(8)], axis=0)
        r = _run("d", build_d2, [dict(mg=_c(mall[:, 8 * g:8 * g + 8]), rtall=_c(rtall), g8=np.array([8.0 * g], np.float32),
                                      xn2=np.ascontiguousarray(xn2), gffn=_c(P["norm_ffn_g"][l]),
                                      wg=_c(P["exp_w_gate"][l][8 * g:8 * g + 8]), wu=_c(P["exp_w_up"][l][8 * g:8 * g + 8]),
                                      wd=_c(P["exp_w_down"][l][8 * g:8 * g + 8]), tri=tri, tokid=tokid, ident=ident,
                                      iota8=iota8, lo8=lo8, trash1=trash1, trash2=trash2) for g in range(8)])
        zall = np.concatenate([r[g]["z"] for g in range(8)], axis=0)
        moe = np.take(zall, r[0]["inv"].T.reshape(-1), axis=0)
        if _DBG is not None:
            _DBG.append(dict(proj=proj, o_all=o_all, xmid=xmid, moe=moe, rtall=rtall, mall=mall))
            if _DBG_STOP:
                return xmid + moe
    r = _run("f", build_f, [dict(x=_c(sh(xmid, c)), xadd=_c(sh(moe, c)), g=_c(P["final_norm_g"])) for c in range(8)])
    out = np.concatenate([r[c]["xout"] for c in range(8)], axis=0)
    return out.reshape(2, S, D).astype(np.float32)
```

```python
import numpy as np
import ml_dtypes
import concourse.bass as bass
import concourse.mybir as mybir
from concourse.bass_utils import run_bass_kernel_spmd

F32 = mybir.dt.float32
BF16 = mybir.dt.bfloat16
I32 = mybir.dt.int32
AF = mybir.ActivationFunctionType
ALU = mybir.AluOpType
AX = mybir.AxisListType

ENGS = ("tensor", "vector", "scalar", "gpsimd", "sync")
DMA_SLOTS = 8


class Res:
    __slots__ = ("name", "lw", "rd")

    def __init__(self, name):
        self.name = name
        self.lw = None
        self.rd = {}


class Op:
    __slots__ = ("eng", "fn", "dma", "deps", "sig", "sem", "val", "idx")

    def __init__(self, eng, fn, dma):
        self.eng = eng
        self.fn = fn
        self.dma = dma
        self.deps = []
        self.sig = False
        self.sem = None
        self.val = 0


class Sched:
    def __init__(self, nc):
        self.nc = nc
        self.ops = []
        self.dma_cnt = {e: 0 for e in ENGS}
        self.dma_last = {}
        self.out_dmas = []

    def op(self, eng, fn, reads=(), writes=(), dma=False):
        o = Op(eng, fn, dma)
        o.idx = len(self.ops)
        deps = {}
        for r in reads:
            if r.lw is not None:
                deps[r.lw.idx] = (r.lw, "raw")
        for w in writes:
            if w.lw is not None and w.lw.idx not in deps:
                deps[w.lw.idx] = (w.lw, "waw")
            for rr in w.rd.values():
                if rr.idx not in deps:
                    deps[rr.idx] = (rr, "war")
        if dma:
            n = self.dma_cnt[eng]
            self.dma_cnt[eng] = n + 1
            slot = n % DMA_SLOTS
            o.sem = ("dma", eng, slot)
            o.val = 16 * (n // DMA_SLOTS + 1)
            o.sig = True
            prev = self.dma_last.get((eng, slot))
            if prev is not None and prev.idx not in deps:
                deps[prev.idx] = (prev, "slot")
            self.dma_last[(eng, slot)] = o
        for d, kind in deps.values():
            if not d.dma and d.eng == eng:
                if kind != "raw" or eng == "tensor":
                    continue
            o.deps.append(d)
            d.sig = True
        for r in reads:
            r.rd[o.sem if dma else eng] = o
        for w in writes:
            w.lw = o
            w.rd = {}
        self.ops.append(o)
        return o

    def emit(self):
        nc = self.nc
        cnt = {e: 0 for e in ENGS}
        for o in self.ops:
            if not o.dma and o.sig:
                cnt[o.eng] += 1
                o.sem = ("eng", o.eng)
                o.val = cnt[o.eng]
        semkeys = [("eng", e) for e in ENGS]
        for e in ENGS:
            for s in range(min(DMA_SLOTS, self.dma_cnt[e])):
                semkeys.append(("dma", e, s))
        import contextlib
        with contextlib.ExitStack() as st:
            sems = {k: st.enter_context(nc.semaphore("s_" + "_".join(str(x) for x in k))) for k in semkeys}
            block = st.enter_context(nc.Block())
            per = {e: [o for o in self.ops if o.eng == e] for e in ENGS}
            final = {}
            for (eng, slot), o in self.dma_last.items():
                final.setdefault(eng, []).append((sems[o.sem], o.val))

            def run(e, engobj):
                seen = {}
                for o in per[e]:
                    for d in o.deps:
                        if seen.get(d.sem, 0) < d.val:
                            engobj.wait_ge(sems[d.sem], d.val)
                            seen[d.sem] = d.val
                    ins = o.fn(engobj)
                    if o.sig:
                        ins.then_inc(sems[o.sem], 16 if o.dma else 1)
                for s, v in final.get(e, []):
                    engobj.wait_ge(s, v)

            @block.tensor
            def _(eng):
                run("tensor", eng)

            @block.vector
            def _(eng):
                run("vector", eng)

            @block.scalar
            def _(eng):
                run("scalar", eng)

            @block.gpsimd
            def _(eng):
                run("gpsimd", eng)

            @block.sync
            def _(eng):
                run("sync", eng)


class V:
    __slots__ = ("ap", "res")

    def __init__(self, ap, res):
        self.ap = ap
        self.res = res

    def __getitem__(self, idx):
        return V(self.ap[idx], self.res)

    def rr(self, s, **kw):
        return V(self.ap.rearrange(s, **kw), self.res)

    def bc(self, shape):
        return V(self.ap.broadcast_to(shape), self.res)

    def un(self, axis):
        return V(self.ap.unsqueeze(axis), self.res)


class Tile:
    def __init__(self, handle, name):
        self.h = handle
        self.name = name
        self.res = Res(name)
        self.subs = {}

    def __getitem__(self, idx):
        return V(self.h[idx], self.res)

    def sub(self, key, idx=None):
        r = self.subs.get(key)
        if r is None:
            r = self.subs[key] = Res("%s/%s" % (self.name, key))
        return V(self.h[idx] if idx is not None else self.h[:], r)


def _aps(x):
    return x.ap if isinstance(x, V) else x


class KB:
    def __init__(self, nc, st):
        self.nc = nc
        self.st = st
        self.S = Sched(nc)
        self.n = 0

    def sb(self, name, shape, dt):
        return Tile(self.st.enter_context(self.nc.sbuf_tensor(name, list(shape), dt)), name)

    def ps(self, name, shape, dt):
        return Tile(self.st.enter_context(self.nc.psum_tensor(name, list(shape), dt)), name)

    def dram(self, name, shape, dt, kind):
        ap = self.nc.dram_tensor(name, list(shape), dt, kind=kind).ap()
        return V(ap, Res(name))

    def _op(self, eng, fn, reads, writes, dma=False):
        rs = [r.res if isinstance(r, V) else r for r in reads if isinstance(r, (V, Res))]
        ws = [w.res if isinstance(w, V) else w for w in writes if isinstance(w, (V, Res))]
        return self.S.op(eng, fn, rs, ws, dma)

    def dma(self, out, in_, eng="sync", xr=(), xw=(), **kw):
        return self._op(eng, lambda e: e.dma_start(out=out.ap, in_=in_.ap, **kw), [in_] + list(xr), [out] + list(xw),
                        dma=True)

    def gather(self, out, table, idx, **kw):
        return self._op("gpsimd", lambda e: e.indirect_dma_start(
            out=out.ap, out_offset=None, in_=table.ap,
            in_offset=bass.IndirectOffsetOnAxis(ap=idx.ap, axis=0), **kw), [table, idx], [out], dma=True)

    def scatter(self, table, idx, in_, **kw):
        return self._op("gpsimd", lambda e: e.indirect_dma_start(
            out=table.ap, out_offset=bass.IndirectOffsetOnAxis(ap=idx.ap, axis=0),
            in_=in_.ap, in_offset=None, **kw), [in_, idx], [table], dma=True)

    def mm(self, out, lhsT, rhs, start=True, stop=True):
        return self._op("tensor", lambda e: e.matmul(out.ap, lhsT=lhsT.ap, rhs=rhs.ap, start=start, stop=stop),
                        [lhsT, rhs], [out])

    def tr(self, out, in_, ident):
        return self._op("tensor", lambda e: e.transpose(out.ap, in_.ap, ident.ap), [in_, ident], [out])

    def act(self, out, in_, func, bias=0.0, scale=1.0, accum=None, eng="scalar"):
        kw = {}
        if accum is not None:
            kw["accum_out"] = accum.ap
        ws = [out] + ([accum] if accum is not None else [])
        return self._op(eng, lambda e: e.activation(out=out.ap, in_=in_.ap, func=func, bias=_aps(bias),
                                                    scale=_aps(scale), **kw), [in_, bias, scale], ws)

    def tt(self, out, in0, in1, op, eng="vector"):
        return self._op(eng, lambda e: e.tensor_tensor(out=out.ap, in0=in0.ap, in1=in1.ap, op=op), [in0, in1], [out])

    def ts(self, out, in0, s1, s2, op0, op1=None, accum=None, eng="vector"):
        kw = {}
        if op1 is not None:
            kw["op1"] = op1
        if accum is not None:
            kw["accum_out"] = accum.ap
        ws = [out] + ([accum] if accum is not None else [])
        return self._op(eng, lambda e: e.tensor_scalar(out=out.ap, in0=in0.ap, scalar1=_aps(s1), scalar2=_aps(s2),
                                                       op0=op0, **kw), [in0, s1, s2], ws)

    def stt(self, out, in0, scalar, in1, op0, op1, eng="vector"):
        return self._op(eng, lambda e: e.scalar_tensor_tensor(out=out.ap, in0=in0.ap, scalar=_aps(scalar), in1=in1.ap,
                                                              op0=op0, op1=op1), [in0, scalar, in1], [out])

    def copy(self, out, in_, eng="vector"):
        if eng == "scalar":
            return self._op(eng, lambda e: e.copy(out=out.ap, in_=in_.ap), [in_], [out])
        return self._op(eng, lambda e: e.tensor_copy(out=out.ap, in_=in_.ap), [in_], [out])

    def memset(self, out, val, eng="vector"):
        return self._op(eng, lambda e: e.memset(out.ap, val), [], [out])

    def recip(self, out, in_):
        return self._op("vector", lambda e: e.reciprocal(out=out.ap, in_=in_.ap), [in_], [out])

    def reduce(self, out, in_, op, axis=AX.X):
        return self._op("vector", lambda e: e.tensor_reduce(out=out.ap, in_=in_.ap, axis=axis, op=op), [in_], [out])

    def max8(self, out, in_):
        return self._op("vector", lambda e: e.max(out=out.ap, in_=in_.ap), [in_], [out])

    def match_replace(self, out, rep, vals, imm):
        return self._op("vector", lambda e: e.match_replace(out=out.ap, in_to_replace=rep.ap, in_values=vals.ap,
                                                            imm_value=imm), [rep, vals], [out])

    def rstd(self, out, ss, n, tmp):
        self.ts(tmp, ss, 1.0 / n, 1e-6, ALU.mult, ALU.add)
        self.act(tmp, tmp, AF.Sqrt)
        self.recip(out, tmp)


import contextlib

D = 4096
NT = 1024
EPS = 1e-6
BIG = 30000.0


def col_view(v, p=128):
    return v.rr("(c p) -> p c", p=p)


def build_c():
    nc = bass.Bass("TRN2", target_bir_lowering=False)
    with contextlib.ExitStack() as st:
        K = KB(nc, st)
        o_d = K.dram("o", [NT, D], F32, "ExternalInput")
        x_d = K.dram("x", [NT, D], F32, "ExternalInput")
        gout_d = K.dram("gout", [D], F32, "ExternalInput")
        wout_d = K.dram("wout", [D, D], F32, "ExternalInput")
        gffn_d = K.dram("gffn", [D], F32, "ExternalInput")
        rw_d = K.dram("rw", [D, 72], F32, "ExternalInput")
        rb_d = K.dram("rb", [72], F32, "ExternalInput")
        ident_d = K.dram("ident", [128, 128], F32, "ExternalInput")
        xmid_d = K.dram("xmid", [NT, D], F32, "ExternalOutput")
        xn2_d = K.dram("xn2", [NT, D], BF16, "ExternalOutput")
        m_d = K.dram("mroute", [NT, 64], F32, "ExternalOutput")
        gw_d = K.dram("gwroute", [NT, 64], F32, "ExternalOutput")
        rt_d = K.dram("route", [NT, 4], F32, "ExternalOutput")
        iota_d = K.dram("iota64", [128, 64], F32, "ExternalInput")
        xmid_res = [Res("xmid%d" % n) for n in range(NT // 128)]

        identf = K.sb("identf", [128, 128], F32)
        identb = K.sb("identb", [128, 128], BF16)
        gcol = K.sb("gcol", [128, 32], F32)
        g2col = K.sb("g2col", [128, 32], F32)
        wr = K.sb("wr", [128, 32, 72], F32)
        rbb = K.sb("rbb", [128, 72], F32)
        iota = K.sb("iota", [128, 64], F32)
        rto = K.sb("rto", [128, 4], F32)
        rtmp = K.sb("rtmp", [128, 64], F32)
        K.dma(iota[:], iota_d)
        K.dma(identf[:], ident_d)
        K.copy(identb[:], identf[:])
        K.dma(gcol[:], col_view(gout_d), allow_slow_non_contiguous=True)
        K.dma(g2col[:], col_view(gffn_d), allow_slow_non_contiguous=True)
        K.dma(wr[:], rw_d.rr("(c p) n -> p c n", p=128))
        K.dma(rbb[:], V(rb_d.ap.partition_broadcast(128), rb_d.res))
        K.tt(wr[:], wr[:], g2col[:].un(2).bc([128, 32, 72]), ALU.mult)

        mixedT = K.sb("mixedT", [128, 32, 512], BF16)
        wbf = [K.sb("wbf%d" % i, [128, 32, 512], BF16) for i in range(2)]
        ot = K.sb("ot", [128, D], F32)
        onb = K.sb("onb", [128, D], BF16)
        junk = K.sb("junk", [128, 1024], BF16)
        xnb = K.sb("xnb", [128, D], BF16)
        ss = K.sb("ss", [128, 4], F32)
        tmp4 = K.sb("tmp4", [128, 4], F32)
        rs4 = K.sb("rs4", [128, 4], F32)
        xt = [K.sb("xt%d" % i, [128, 4, 512], F32) for i in range(2)]
        ysb = [K.sb("ysb%d" % i, [128, 512], F32) for i in range(2)]
        xmT = K.sb("xmT", [128, 32, 128], F32)
        lg = K.sb("lg", [128, 72], F32)
        sm = K.sb("sm", [128, 16], F32)
        oh = K.sb("oh", [128, 8], F32)
        msk = K.sb("msk", [128, 64], F32)
        m8 = K.sb("m8", [128, 8], F32)
        mo = K.sb("mo", [128, 64], F32)
        gwo = K.sb("gwo", [128, 64], F32)
        tpb = [K.ps("tpb%d" % i, [128, 8, 128], BF16) for i in range(2)]
        yps = [K.ps("yps%d" % i, [128, 512], F32) for i in range(2)]
        tpf = [K.ps("tpf%d" % i, [128, 4, 128], F32) for i in range(2)]
        lgp = K.ps("lgp", [128, 72], F32)

        wv = wout_d.rr("(c p) n -> p c n", p=128)
        wcount = 0
        for hf in range(NT // 512):
            for n in range(4):
                r0 = hf * 512 + n * 128
                K.dma(ot[:], o_d[r0:r0 + 128, :])
                for gi in range(4):
                    K.act(junk[:], ot[:, gi * 1024:(gi + 1) * 1024], AF.Square, accum=ss[:, gi:gi + 1])
                K.rstd(rs4[:], ss[:], 1024.0, tmp4[:])
                for gi in range(4):
                    K.ts(onb.sub(gi, (slice(None), slice(gi * 1024, (gi + 1) * 1024))),
                         ot[:, gi * 1024:(gi + 1) * 1024], rs4[:, gi:gi + 1], None, ALU.mult)
                for k in range(4):
                    tp = tpb[k % 2]
                    for c in range(8):
                        cc = k * 8 + c
                        K.tr(tp[:, c, :], onb.sub(cc // 8, (slice(None), slice(cc * 128, (cc + 1) * 128))), identb[:])
                    K.tt(mixedT.sub((n, k), (slice(None), slice(k * 8, k * 8 + 8), slice(n * 128, (n + 1) * 128))),
                         tp[:], gcol[:, k * 8:k * 8 + 8].un(2).bc([128, 8, 128]), ALU.mult)
            for j in range(8):
                wb = wbf[wcount % 2]
                wcount += 1
                K.dma(wb[:], wv[:, :, j * 512:(j + 1) * 512], eng="gpsimd")
                xb = xt[j % 2]
                K.dma(xb[:], x_d[hf * 512:(hf + 1) * 512, j * 512:(j + 1) * 512].rr("(n p) c -> p n c", p=128))
                for n in range(4):
                    yp = yps[n % 2]
                    for c in range(32):
                        K.mm(yp[:], mixedT.sub((n, c // 8), (slice(None), c, slice(n * 128, (n + 1) * 128))),
                             wb[:, c, :], start=(c == 0), stop=(c == 31))
                    yb = ysb[n % 2]
                    K.tt(yb[:], yp[:], xb[:, n, :], ALU.add)
                    r0 = hf * 512 + n * 128
                    K.dma(V(xmid_d.ap[r0:r0 + 128, j * 512:(j + 1) * 512], xmid_res[hf * 4 + n]), yb[:])
            for n in range(4):
                r0 = hf * 512 + n * 128
                K.dma(ot[:], V(xmid_d.ap[r0:r0 + 128, :], xmid_res[hf * 4 + n]))
                K.act(xnb[:], ot[:], AF.Square, accum=ss[:, 0:1])
                K.rstd(rs4[:, 0:1], ss[:, 0:1], float(D), tmp4[:, 0:1])
                K.ts(xnb[:], ot[:], rs4[:, 0:1], None, ALU.mult)
                K.dma(xn2_d[r0:r0 + 128, :], xnb[:])
                for k in range(8):
                    tp = tpf[k % 2]
                    for c in range(4):
                        cc = k * 4 + c
                        K.tr(tp[:, c, :], ot[:, cc * 128:(cc + 1) * 128], identf[:])
                    K.copy(xmT.sub(k, (slice(None), slice(k * 4, k * 4 + 4), slice(None))), tp[:],
                           eng=("scalar" if k % 2 else "vector"))
                for c in range(32):
                    K.mm(lgp[:], xmT.sub(c // 4, (slice(None), c, slice(None))), wr[:, c, :],
                         start=(c == 0), stop=(c == 31))
                K.stt(lg[:], lgp[:], rs4[:, 0:1], rbb[:], ALU.mult, ALU.add)
                router_math(K, lg, sm, oh, msk, m8, mo, gwo, iota, rto, rtmp)
                K.dma(rt_d[r0:r0 + 128, :], rto[:])
                K.dma(m_d[r0:r0 + 128, :], mo[:])
                K.dma(gw_d[r0:r0 + 128, :], gwo[:])
        K.S.emit()
    return nc


def router_math(K, lg, sm, oh, msk, m8, mo, gwo, iota, rto, rtmp):
    K.reduce(sm[:, 0:1], lg[:, 0:8], ALU.max)
    K.ts(oh[:], lg[:, 0:8], sm[:, 0:1], None, ALU.is_ge)
    K.ts(sm[:, 1:2], sm[:, 0:1], -1.0, None, ALU.mult)
    K.act(msk[:, 0:8], lg[:, 0:8], AF.Exp, bias=sm[:, 1:2], accum=sm[:, 2:3])
    K.recip(sm[:, 3:4], sm[:, 2:3])
    K.ts(oh[:], oh[:], BIG, -BIG, ALU.mult, ALU.add)
    K.tt(msk[:].rr("p (g e) -> p g e", g=8), lg[:, 8:72].rr("p (g e) -> p g e", g=8),
         oh[:].un(2).bc([128, 8, 8]), ALU.add)
    K.max8(m8[:], msk[:])
    K.ts(mo[:], msk[:], m8[:, 1:2], None, ALU.is_ge)
    K.tt(sm[:, 4:5], m8[:, 0:1], m8[:, 1:2], ALU.add)
    K.ts(sm[:, 4:5], sm[:, 4:5], -1.0, None, ALU.mult)
    K.act(gwo[:], msk[:], AF.Sigmoid, bias=sm[:, 4:5], scale=2.0)
    K.stt(gwo[:], gwo[:], sm[:, 3:4], mo[:], ALU.mult, ALU.mult)
    K.stt(rtmp[:], iota[:], 1.0, mo[:], ALU.add, ALU.mult)
    K.reduce(rto[:, 1:2], rtmp[:], ALU.max)
    K.ts(rto[:, 1:2], rto[:, 1:2], -1.0, None, ALU.add)
    K.ts(rtmp[:], iota[:], -1.0, 64.0, ALU.mult, ALU.add)
    K.tt(rtmp[:], rtmp[:], mo[:], ALU.mult)
    K.reduce(rto[:, 0:1], rtmp[:], ALU.max)
    K.ts(rto[:, 0:1], rto[:, 0:1], -1.0, 64.0, ALU.mult, ALU.add)
    for k in range(2):
        K.ts(rtmp[:], iota[:], rto[:, k:k + 1], None, ALU.is_equal)
        K.tt(rtmp[:], rtmp[:], gwo[:], ALU.mult)
        K.reduce(rto[:, 2 + k:3 + k], rtmp[:], ALU.add)


CE = 384
NTOK = 8192
NTT = NTOK // 128
OOB = 1.0e6


def build_d1():
    nc = bass.Bass("TRN2", target_bir_lowering=False)
    with contextlib.ExitStack() as st:
        K = KB(nc, st)
        m_d = K.dram("mall", [NTOK, 64], F32, "ExternalInput")
        rt_d = K.dram("rtall", [NTOK, 4], F32, "ExternalInput")
        gb_d = K.dram("gbase", [1], F32, "ExternalInput")
        tri_d = K.dram("tri", [128, 128], F32, "ExternalInput")
        tok_d = K.dram("tokid", [128, NTT], F32, "ExternalInput")
        iota_d = K.dram("iota64", [128, 64], F32, "ExternalInput")
        idx_d = K.dram("idxlist", [8 * CE, 2], I32, "ExternalOutput")
        dg_d = K.dram("destg", [NTOK, 2], I32, "ExternalOutput")

        Mt = K.sb("Mt", [128, NTT, 64], F32)
        Mb = K.sb("Mb", [128, NTT, 64], BF16)
        Rf = K.sb("Rf", [128, NTT, 64], F32)
        Rb = K.sb("Rb", [128, NTT, 64], BF16)
        slot = K.sb("slot", [128, NTT, 64], F32)
        oh = K.sb("ohd", [128, NTT, 64], F32)
        trif = K.sb("trif", [128, 128], F32)
        trib = K.sb("trib", [128, 128], BF16)
        oneb = K.sb("oneb", [128, 128], BF16)
        iota = K.sb("iota", [128, 64], F32)
        tokf = K.sb("tokf", [128, NTT], F32)
        toki = K.sb("toki", [128, NTT, 2], I32)
        rt = K.sb("rt", [128, NTT, 4], F32)
        gb = K.sb("gb", [128, 1], F32)
        sl = K.sb("sl", [128, NTT], F32)
        dgl = K.sb("dgl", [128, NTT], F32)
        dl = K.sb("dl", [128, NTT], F32)
        ok = K.sb("ok", [128, NTT], F32)
        ok2 = K.sb("ok2", [128, NTT], F32)
        di = [K.sb("di%d" % k, [128, NTT], I32) for k in range(2)]
        dgi = K.sb("dgi", [128, NTT, 2], I32)
        zt = K.sb("zt", [128, 8 * CE // 128, 2], I32)
        pss = [K.ps("pss%d" % i, [128, 8, 64], F32) for i in range(8)]

        K.dma(Mt[:], m_d.rr("(n p) e -> p n e", p=128))
        K.dma(rt[:], rt_d.rr("(n p) k -> p n k", p=128))
        K.dma(trif[:], tri_d)
        K.dma(tokf[:], tok_d)
        K.dma(iota[:], iota_d)
        K.dma(gb[:], V(gb_d.ap.partition_broadcast(128), gb_d.res))
        K.copy(trib[:], trif[:])
        K.memset(oneb[:], 1.0)
        K.copy(toki[:, :, 0], tokf[:])
        K.copy(toki[:, :, 1], tokf[:])
        K.memset(zt[:], 0)
        K.dma(idx_d.rr("(s p) o -> p s o", p=128), zt[:])
        K.copy(Mb[:], Mt[:])
        K.memset(Rf[:, 0, :], 0.0)
        for n in range(1, NTT):
            K.tt(Rf[:, n, :], Rf[:, n - 1, :], Mt[:, n - 1, :], ALU.add)
        K.copy(Rb[:], Rf[:])
        for n in range(NTT):
            p = pss[n // 8]
            K.mm(p[:, n % 8, :], trib[:], Mb[:, n, :], start=True, stop=False)
            K.mm(p[:, n % 8, :], oneb[:], Rb[:, n, :], start=False, stop=True)
        for b in range(8):
            K.copy(slot[:, b * 8:(b + 1) * 8, :], pss[b][:], eng=("scalar" if b % 2 else "vector"))
        iota3 = iota[:].un(1).bc([128, NTT, 64])
        for k in range(2):
            K.tt(oh[:], iota3, rt[:, :, k].un(2).bc([128, NTT, 64]), ALU.is_equal)
            K.tt(oh[:], oh[:], slot[:], ALU.mult)
            K.reduce(sl[:], oh[:], ALU.add)
            K.stt(dgl[:], rt[:, :, k], float(CE), sl[:], ALU.mult, ALU.add)
            K.copy(dgi[:, :, k], dgl[:])
            K.ts(ok[:], sl[:], float(CE), None, ALU.is_lt)
            K.ts(dl[:], dgl[:], gb[:, 0:1], None, ALU.subtract)
            K.ts(ok2[:], dl[:], 0.0, None, ALU.is_ge)
            K.tt(ok[:], ok[:], ok2[:], ALU.mult)
            K.ts(ok2[:], dl[:], float(8 * CE), None, ALU.is_lt)
            K.tt(ok[:], ok[:], ok2[:], ALU.mult)
            K.ts(dl[:], dl[:], -OOB, None, ALU.add)
            K.tt(dl[:], dl[:], ok[:], ALU.mult)
            K.ts(dl[:], dl[:], OOB, None, ALU.add)
            K.copy(di[k][:], dl[:])
        K.dma(dg_d.rr("(n p) k -> p n k", p=128), dgi[:])
        for n in range(NTT):
            for k in range(2):
                K.scatter(idx_d, di[k][:, n:n + 1], toki[:, n, :], bounds_check=8 * CE - 1, oob_is_err=False)
        K.S.emit()
    return nc


NPROJ = 8288


def ot_pre(K):
    if not hasattr(K, "_otp"):
        K._otp = K.sb("otp", [128, D], F32)
    return K._otp


def norm_transpose(K, src_d, r0, width, gcols, ot, onb, ss, rs, tmp, tpb, identb, dstT, n, junk):
    nch = width // 128
    K.dma(ot[:, 0:width], src_d[r0:r0 + 128, :])
    K.act(junk[:, 0:width], ot[:, 0:width], AF.Square, accum=ss[:, 0:1])
    K.rstd(rs[:, 0:1], ss[:, 0:1], float(width), tmp[:, 0:1])
    K.ts(onb[:, 0:width], ot[:, 0:width], rs[:, 0:1], None, ALU.mult)
    for k in range((nch + 7) // 8):
        tp = tpb[k % 2]
        m = min(8, nch - k * 8)
        for c in range(m):
            cc = k * 8 + c
            K.tr(tp[:, c, :], onb[:, cc * 128:(cc + 1) * 128], identb[:])
        K.tt(dstT.sub((n, k), (slice(None), slice(k * 8, k * 8 + m), slice(n * 128, (n + 1) * 128))),
             tp[:, 0:m, :], gcols[:, k * 8:k * 8 + m].un(2).bc([128, m, 128]), ALU.mult)


def build_a():
    nc = bass.Bass("TRN2", target_bir_lowering=False)
    with contextlib.ExitStack() as st:
        K = KB(nc, st)
        x_d = K.dram("x", [NT, D], F32, "ExternalInput")
        xa_d = K.dram("xadd", [NT, D], F32, "ExternalInput")
        g_d = K.dram("g", [D], F32, "ExternalInput")
        w_d = K.dram("w", [D, NPROJ], F32, "ExternalInput")
        ident_d = K.dram("ident", [128, 128], F32, "ExternalInput")
        p_d = K.dram("proj", [NT, NPROJ], F32, "ExternalOutput")
        xs_d = K.dram("xsum", [NT, D], F32, "ExternalOutput")
        xs_res = [Res("xs%d" % n) for n in range(NT // 128)]
        xa = K.sb("xa", [128, D], F32)
        for n in range(NT // 128):
            K.dma(ot_pre(K)[:], x_d[n * 128:(n + 1) * 128, :])
            K.dma(xa[:], xa_d[n * 128:(n + 1) * 128, :])
            K.tt(xa[:], xa[:], ot_pre(K)[:], ALU.add)
            K.dma(V(xs_d.ap[n * 128:(n + 1) * 128, :], xs_res[n]), xa[:])
        identf = K.sb("identf", [128, 128], F32)
        identb = K.sb("identb", [128, 128], BF16)
        gcol = K.sb("gcol", [128, 32], F32)
        K.dma(identf[:], ident_d)
        K.copy(identb[:], identf[:])
        K.dma(gcol[:], col_view(g_d), allow_slow_non_contiguous=True)
        xT = K.sb("xT", [128, 32, 512], BF16)
        wbf = [K.sb("wbf%d" % i, [128, 32, 512], BF16) for i in range(2)]
        ot = K.sb("ot", [128, D], F32)
        onb = K.sb("onb", [128, D], BF16)
        junk = K.sb("junk", [128, D], BF16)
        ss = K.sb("ss", [128, 1], F32)
        tmp = K.sb("tmp", [128, 1], F32)
        rs = K.sb("rs", [128, 1], F32)
        ysb = [K.sb("ysb%d" % i, [128, 512], F32) for i in range(4)]
        tpb = [K.ps("tpb%d" % i, [128, 8, 128], BF16) for i in range(2)]
        yps = [K.ps("yps%d" % i, [128, 512], F32) for i in range(4)]
        wv = w_d.rr("(c p) n -> p c n", p=128)
        chunks = [(j * 512, 512) for j in range(16)] + [(8192, 96)]
        wcount = 0
        ycount = 0
        for hf in range(NT // 512):
            for n in range(4):
                norm_transpose(K, V(xs_d.ap, xs_res[hf * 4 + n]), hf * 512 + n * 128, D, gcol, ot, onb, ss, rs, tmp, tpb,
                               identb, xT, n, junk)
            for (c0, cw) in chunks:
                wb = wbf[wcount % 2]
                wcount += 1
                K.dma(wb[:, :, 0:cw], wv[:, :, c0:c0 + cw], eng="gpsimd")
                for n in range(4):
                    yp = yps[ycount % 4]
                    yb = ysb[ycount % 4]
                    for c in range(32):
                        K.mm(yp[:, 0:cw], xT.sub((n, c // 8), (slice(None), c, slice(n * 128, (n + 1) * 128))),
                             wb[:, c, 0:cw], start=(c == 0), stop=(c == 31))
                    K.copy(yb[:, 0:cw], yp[:, 0:cw], eng=("scalar" if ycount % 2 else "vector"))
                    ycount += 1
                    r0 = hf * 512 + n * 128
                    K.dma(p_d[r0:r0 + 128, c0:c0 + cw], yb[:, 0:cw])
        K.S.emit()
    return nc


def build_a2():
    nc = bass.Bass("TRN2", target_bir_lowering=False)
    with contextlib.ExitStack() as st:
        K = KB(nc, st)
        cq_d = K.dram("cq", [NT, 768], F32, "ExternalInput")
        ckv_d = K.dram("ckv", [NT, 512], F32, "ExternalInput")
        kr_d = K.dram("kr", [NT, 64], F32, "ExternalInput")
        gq_d = K.dram("gq", [768], F32, "ExternalInput")
        gkv_d = K.dram("gkv", [512], F32, "ExternalInput")
        wuq_d = K.dram("wuq", [768, 1536], F32, "ExternalInput")
        wukv_d = K.dram("wukv", [512, 2048], F32, "ExternalInput")
        cos_d = K.dram("cos", [NT, 32], F32, "ExternalInput")
        sin_d = K.dram("sin", [NT, 32], F32, "ExternalInput")
        ident_d = K.dram("ident", [128, 128], F32, "ExternalInput")
        q_d = K.dram("q", [NT, 1536], F32, "ExternalOutput")
        kv_d = K.dram("kv", [NT, 2048], F32, "ExternalOutput")
        kro_d = K.dram("kro", [NT, 64], F32, "ExternalOutput")
        identf = K.sb("identf", [128, 128], F32)
        identb = K.sb("identb", [128, 128], BF16)
        gqc = K.sb("gqc", [128, 6], F32)
        gkvc = K.sb("gkvc", [128, 4], F32)
        wuq = K.sb("wuqs", [128, 6, 1536], BF16)
        wukv = K.sb("wukvs", [128, 4, 2048], BF16)
        K.dma(identf[:], ident_d)
        K.copy(identb[:], identf[:])
        K.dma(gqc[:], col_view(gq_d), allow_slow_non_contiguous=True)
        K.dma(gkvc[:], col_view(gkv_d), allow_slow_non_contiguous=True)
        K.dma(wuq[:], wuq_d.rr("(c p) n -> p c n", p=128), eng="gpsimd")
        K.dma(wukv[:], wukv_d.rr("(c p) n -> p c n", p=128), eng="gpsimd")
        cqT = K.sb("cqT", [128, 6, 128], BF16)
        ckvT = K.sb("ckvT", [128, 4, 128], BF16)
        ot = K.sb("ot", [128, 768], F32)
        onb = K.sb("onb", [128, 768], BF16)
        junk = K.sb("junk", [128, 768], BF16)
        ss = K.sb("ss", [128, 1], F32)
        tmp = K.sb("tmp", [128, 1], F32)
        rs = K.sb("rs", [128, 1], F32)
        qsb = K.sb("qsb", [128, 1536], F32)
        kvsb = K.sb("kvsb", [128, 2048], F32)
        krs = K.sb("krs", [128, 64], F32)
        cs = K.sb("cs", [128, 32], F32)
        sn = K.sb("sn", [128, 32], F32)
        t1 = K.sb("t1", [128, 8, 32], F32)
        t2 = K.sb("t2", [128, 8, 32], F32)
        t3 = K.sb("t3", [128, 8, 32], F32)
        t4 = K.sb("t4", [128, 8, 32], F32)
        tpb = [K.ps("tpb%d" % i, [128, 8, 128], BF16) for i in range(2)]
        yps = [K.ps("yps%d" % i, [128, 512], F32) for i in range(4)]

        def rope(buf3, nh):
            x1 = buf3[:, :, 0:32]
            x2 = buf3[:, :, 32:64]
            cb = cs[:].un(1).bc([128, nh, 32])
            sb_ = sn[:].un(1).bc([128, nh, 32])
            K.tt(t1[:, 0:nh, :], x1, cb, ALU.mult)
            K.tt(t2[:, 0:nh, :], x2, sb_, ALU.mult)
            K.tt(t3[:, 0:nh, :], x2, cb, ALU.mult)
            K.tt(t4[:, 0:nh, :], x1, sb_, ALU.mult)
            K.tt(x1, t1[:, 0:nh, :], t2[:, 0:nh, :], ALU.subtract)
            K.tt(x2, t3[:, 0:nh, :], t4[:, 0:nh, :], ALU.add)

        yc = 0
        for n in range(NT // 128):
            r0 = n * 128
            norm_transpose(K, cq_d, r0, 768, gqc, ot, onb, ss, rs, tmp, tpb, identb, cqT, 0, junk)
            norm_transpose(K, ckv_d, r0, 512, gkvc, ot, onb, ss, rs, tmp, tpb, identb, ckvT, 0, junk)
            K.dma(krs[:], kr_d[r0:r0 + 128, :])
            K.dma(cs[:], cos_d[r0:r0 + 128, :])
            K.dma(sn[:], sin_d[r0:r0 + 128, :])
            for j in range(3):
                yp = yps[yc % 4]
                yc += 1
                for c in range(6):
                    K.mm(yp[:], cqT.sub((0, 0), (slice(None), c, slice(None))), wuq[:, c, j * 512:(j + 1) * 512],
                         start=(c == 0), stop=(c == 5))
                K.copy(qsb[:, j * 512:(j + 1) * 512], yp[:], eng=("scalar" if j % 2 else "vector"))
            for j in range(4):
                yp = yps[yc % 4]
                yc += 1
                for c in range(4):
                    K.mm(yp[:], ckvT.sub((0, 0), (slice(None), c, slice(None))), wukv[:, c, j * 512:(j + 1) * 512],
                         start=(c == 0), stop=(c == 3))
                K.copy(kvsb[:, j * 512:(j + 1) * 512], yp[:], eng=("scalar" if j % 2 else "vector"))
            rope(qsb[:].rr("p (h d) -> p h d", d=192)[:, :, 128:192], 8)
            rope(krs[:].rr("p (h d) -> p h d", d=64), 1)
            K.dma(q_d[r0:r0 + 128, :], qsb[:])
            K.dma(kv_d[r0:r0 + 128, :], kvsb[:])
            K.dma(kro_d[r0:r0 + 128, :], krs[:])
        K.S.emit()
    return nc


S = 4096
NQT = S // 128


def nsa_tables():
    c = np.arange(256)
    c_end = 16 * c + 31
    t = np.arange(S)
    cm = (t[None, :] >= c_end[:, None]) & (c[:, None] < 255)
    cmpmask = cm.reshape(2, 128, NQT, 128).transpose(1, 0, 2, 3).astype(np.float32)
    j = np.arange(64)
    c_start = 16 * c
    ovl = ((c_start[:, None] <= j[None, :] * 64 + 63) & (c_end[:, None] >= j[None, :] * 64) & (c[:, None] < 255))
    ovl = ovl.reshape(2, 128, 64).transpose(1, 0, 2).astype(np.float32)
    cur = (t // 64)[:, None]
    forced = (j[None, :] == 0) | (j[None, :] == cur) | (j[None, :] == cur - 1)
    future = j[None, :] > cur
    keep = (~(forced | future)).astype(np.float32)
    fill = np.where(forced, 1e6 + j[None, :] * 16.0, 0.0) + np.where(future, -1e6 - j[None, :] * 16.0, 0.0)
    keep = keep.reshape(NQT, 128, 64).transpose(1, 0, 2)
    fill = fill.reshape(NQT, 128, 64).transpose(1, 0, 2).astype(np.float32)
    E = (np.arange(S)[None, :] // 64 == j[:, None]).astype(np.float32)
    return cmpmask, ovl, np.ascontiguousarray(keep), np.ascontiguousarray(fill), E


class ACtx:
    pass


def attn_head(K, C, qparts, kparts, vaug, vw, klist_fn, scale, bias_fn, extra_fn, epilogue):
    LOOK = 3
    jobs = []
    for qt in range(NQT):
        kl = klist_fn(qt)
        for i, (kt, mk) in enumerate(kl):
            jobs.append((qt, kt, mk, i == 0, i == len(kl) - 1))
    st = {}
    po_of = {}

    def front(j):
        qt, kt, mk, first, last = jobs[j]
        pss = C.ps_s[C.sc % 4]
        pt = C.pT[C.sc % 4]
        C.sc += 1
        ex = extra_fn(kt, qt) if extra_fn else None
        npart = len(qparts)
        for p in range(npart):
            K.mm(pss[:, 0:128], kparts[p][:, kt * 128:(kt + 1) * 128], qparts[p][:, qt * 128:(qt + 1) * 128],
                 start=(p == 0), stop=(p == npart - 1 and ex is None))
        if ex is not None:
            K.mm(pss[:, 0:128], ex[0], ex[1], start=False, stop=True)
        b = bias_fn(kt, qt) if bias_fn else 0.0
        K.act(pt[:], pss[:, 0:128], AF.Exp, bias=b, scale=scale)
        if mk is not None:
            K.tt(pt[:], pt[:], mk, ALU.mult)
        st[j] = pt

    def back(j):
        qt, kt, mk, first, last = jobs[j]
        if first:
            po_of[qt] = C.ps_o[C.oc % 2]
            C.oc += 1
        po = po_of[qt]
        K.mm(po[:, 0:vw], st.pop(j)[:], vaug[:, kt, 0:vw], start=first, stop=last)
        if last:
            epilogue(qt, po)
            del po_of[qt]
    n = len(jobs)
    for j in range(n + LOOK):
        if j < n:
            front(j)
        if j - LOOK >= 0:
            back(j - LOOK)


def build_b():
    cmpmask_np = nsa_tables()[0]
    nc = bass.Bass("TRN2", target_bir_lowering=False)
    with contextlib.ExitStack() as st:
        K = KB(nc, st)
        I = lambda n, s: K.dram(n, s, F32, "ExternalInput")
        swa_q, swa_k, swa_v = I("swa_qT", [4, 64, S]), I("swa_kT", [64, S]), I("swa_v", [S, 64])
        swa_alb, swa_sink = I("swa_alb", [128, 4, 2]), I("swa_sink", [128, 4])
        fox_q, fox_k, fox_v = I("fox_qT", [2, 128, S]), I("fox_kT", [2, 128, S]), I("fox_v", [2, S, 128])
        fox_fl, fox_fb = I("fox_fl", [2, S]), I("fox_fb", [2, 1])
        mla_qn, mla_qr, mla_kn = I("mla_qnT", [2, 128, S]), I("mla_qrT", [2, 64, S]), I("mla_knT", [2, 128, S])
        mla_kr, mla_v = I("mla_krT", [64, S]), I("mla_v", [2, S, 128])
        nsa_q = I("nsa_qT", [4, 128, S])
        nsa_kc, nsa_vc = I("nsa_kcT", [128, S]), I("nsa_vcT", [128, S])
        nsa_ks, nsa_vs = I("nsa_ksT", [128, S]), I("nsa_vs", [S, 128])
        nsa_kw, nsa_vw = I("nsa_kwT", [128, S]), I("nsa_vw", [S, 128])
        nsa_gl = I("nsa_gl", [S, 6])
        kc_pos, kc_w1, kc_w2 = I("kc_posT", [128, 32]), I("kc_w1", [S, 128]), I("kc_w2", [128, 128])
        vc_pos, vc_w1, vc_w2 = I("vc_posT", [128, 32]), I("vc_w1", [S, 128]), I("vc_w2", [128, 128])
        nsa_alb = I("nsa_alb", [128, 4, NQT])
        cmpb_d = I("cmpb", [128, 4, 2, NQT])
        cmpmask_d = I("cmpmask", [128, 2, NQT, 128])
        ovl_d, keep_d, fill_d, E_d = I("ovl", [128, 2, 64]), I("keep", [128, NQT, 64]), I("fill", [128, NQT, 64]), I("Emat", [64, S])
        mdiag_d, medge_d, ident_d = I("mdiag", [128, 128]), I("medge", [128, 128]), I("ident", [128, 128])
        out_d = K.dram("out", [S, 1024], F32, "ExternalOutput")
        cfox_d = K.dram("cfox", [2, S], F32, "ExternalOutput")

        def cload(name, src, shape, dt=BF16):
            t = K.sb(name, shape, dt)
            K.dma(t[:], src, eng=("gpsimd" if dt == BF16 else "sync"))
            return t
        mdiag = cload("mdiag_s", mdiag_d, [128, 128])
        medge = cload("medge_s", medge_d, [128, 128])
        identf = cload("identf", ident_d, [128, 128], F32)
        cmpmask = cload("cmpmask_s", cmpmask_d, [128, 2, NQT, 128])
        Emat = cload("Emat_s", E_d, [64, S])
        keep = cload("keep_s", keep_d, [128, NQT, 64], F32)
        fill = cload("fill_s", fill_d, [128, NQT, 64], F32)
        cmpb = cload("cmpb_s", cmpb_d, [128, 4, 2, NQT], F32)
        nalb = cload("nalb_s", nsa_alb, [128, 4, NQT], F32)
        salb = cload("salb_s", swa_alb, [128, 4, 2], F32)
        ssink = cload("ssink_s", swa_sink, [128, 4], F32)

        C = ACtx()
        C.oc = 0
        C.sc = 0
        C.ps_s = [K.ps("pss%d" % i, [128, 512], F32) for i in range(4)]
        C.ps_o = [K.ps("pso%d" % i, [128, 512], F32) for i in range(2)]
        C.pT = [K.sb("pT%d" % i, [128, 128], BF16) for i in range(4)]
        psx = [K.ps("psx%d" % i, [128, 512], F32) for i in range(2)]
        qb = [K.sb("qb%d" % i, [128, S], BF16) for i in range(2)]
        kb = [K.sb("kb%d" % i, [128, S], BF16) for i in range(1)] * 2
        q2b = [K.sb("q2b%d" % i, [64, S], BF16) for i in range(1)] * 2
        k2b = K.sb("k2b", [64, S], BF16)
        vb = [K.sb("vb%d" % i, [128, NQT, 129], BF16) for i in range(2)]
        ob = [K.sb("ob%d" % i, [128, NQT, 128], F32) for i in range(1)] * 2
        onsa = [K.sb("onsa%d" % i, [128, NQT, 128], F32) for i in range(2)]
        flt = [onsa[i][0:2].rr("p n d -> p (n d)") for i in range(2)]
        rinv = [K.sb("rinv%d" % i, [128, 1], F32) for i in range(4)]
        cnt = {"r": 0, "h": 0}

        def nr():
            cnt["r"] += 1
            return rinv[cnt["r"] % 4]

        def load_v(vbt, src, dv):
            K.dma(vbt[:, :, 0:dv], src.rr("(n p) d -> p n d", p=128), eng="gpsimd")
            K.memset(vbt[:, :, dv:dv + 1], 1.0)

        def causal(qt):
            return [(kt, (mdiag[:] if kt == qt else None)) for kt in range(qt + 1)]

        def band(nprev):
            def f(qt):
                l = []
                for kt in range(max(0, qt - nprev), qt + 1):
                    l.append((kt, mdiag[:] if kt == qt else (medge[:] if kt == qt - nprev else None)))
                return l
            return f

        def plain_epi(obuf, dv):
            def f(qt, po):
                r = nr()
                K.recip(r[:], po[:, dv:dv + 1])
                K.ts(obuf[:, qt, 0:dv], po[:, 0:dv], r[:, 0:1], None, ALU.mult)
            return f

        def store(obuf, col0, dv):
            K.dma(out_d[:, col0:col0 + dv].rr("(n p) d -> p n d", p=128), obuf[:, :, 0:dv])

        K.dma(k2b[:], swa_k, eng="gpsimd")
        load_v(vb[0], swa_v, 64)
        sinkc = K.sb("sinkc", [128, 4], F32)
        for h in range(4):
            K.act(sinkc[:, h:h + 1], salb[:, h, 0:1], AF.Exp, bias=ssink[:, h:h + 1])
        for h in range(4):
            qt_ = q2b[h % 2]
            K.dma(qt_[:], swa_q[h], eng="gpsimd")
            obuf = ob[cnt["h"] % 2]
            cnt["h"] += 1

            def epi(qt, po, h=h, obuf=obuf):
                r = nr()
                K.tt(r[:], po[:, 64:65], sinkc[:, h:h + 1], ALU.add)
                K.recip(r[:], r[:])
                K.ts(obuf[:, qt, 0:64], po[:, 0:64], r[:, 0:1], None, ALU.mult)
            attn_head(K, C, [qt_], [k2b], vb[0], 65, band(1), 0.125,
                      lambda kt, qt, h=h: salb[:, h, (qt - kt):(qt - kt) + 1], None, epi)
            store(obuf, 768 + h * 64, 64)

        fl, fl2 = flt
        fbn = K.sb("fbn", [2, 1], F32)
        K.dma(fl[:], fox_fl)
        K.dma(fbn[:], fox_fb)
        K.ts(fbn[:], fbn[:], -1.0, None, ALU.mult)
        K.act(fl[:], fl[:], AF.Exp, bias=fbn[:, 0:1], scale=-1.0)
        K.act(fl[:], fl[:], AF.Ln, bias=1.0)
        a, b_ = fl, fl2
        s = 1
        while s < S:
            K.copy(b_[:, 0:s], a[:, 0:s], eng="gpsimd")
            K.tt(b_[:, s:S], a[:, s:S], a[:, 0:S - s], ALU.add)
            a, b_ = b_, a
            s *= 2
        K.dma(cfox_d, a[:])
        cK = K.sb("cK", [128, 2, NQT], F32)
        cR = K.sb("cR", [128, 2, NQT], F32)
        for h in range(2):
            K.dma(cK[:, h, :], cfox_d[h].rr("(n p) -> p n", p=128), allow_slow_non_contiguous=True)
            K.dma(cR[:, h, :], V(cfox_d.ap[h, 127:S:128].partition_broadcast(128), cfox_d.res), allow_slow_non_contiguous=True)
        fbias = [K.sb("fbias%d" % i, [128, NQT], F32) for i in range(2)]
        for h in range(2):
            K.dma(qb[h % 2][:], fox_q[h], eng="gpsimd")
            K.dma(kb[h % 2][:], fox_k[h], eng="gpsimd")
            load_v(vb[(h + 1) % 2], fox_v[h], 128)
            obuf = ob[cnt["h"] % 2]
            cnt["h"] += 1
            fbs = {}

            def fb(kt, qt, h=h, fbs=fbs):
                if qt not in fbs:
                    t = fbias[qt % 2]
                    K.ts(t[:, 0:qt + 1], cK[:, h, 0:qt + 1], cR[:, h, qt:qt + 1], None, ALU.subtract)
                    fbs[qt] = t
                return fbs[qt][:, kt:kt + 1]
            attn_head(K, C, [qb[h % 2]], [kb[h % 2]], vb[(h + 1) % 2], 129, causal, 128 ** -0.5, fb, None,
                      plain_epi(obuf, 128))
            store(obuf, 256 + h * 128, 128)

        K.dma(k2b[:], mla_kr, eng="gpsimd")
        for h in range(2):
            K.dma(qb[h % 2][:], mla_qn[h], eng="gpsimd")
            K.dma(kb[h % 2][:], mla_kn[h], eng="gpsimd")
            K.dma(q2b[h % 2][:], mla_qr[h], eng="gpsimd")
            load_v(vb[(h + 1) % 2], mla_v[h], 128)
            obuf = ob[cnt["h"] % 2]
            cnt["h"] += 1
            attn_head(K, C, [qb[h % 2], q2b[h % 2]], [kb[h % 2], k2b], vb[(h + 1) % 2], 129, causal, 192 ** -0.5,
                      None, None, plain_epi(obuf, 128))
            store(obuf, 512 + h * 128, 128)

        sg = K.sb("sg", [128, NQT, 6], F32)
        K.dma(sg[:], nsa_gl.rr("(n p) c -> p n c", p=128))
        K.act(sg[:], sg[:], AF.Sigmoid)
        kcT = K.sb("kcT", [128, 256], BF16)
        vca = K.sb("vca", [128, 2, 193], BF16)
        w1b = K.sb("w1b", [128, 32, 128], BF16)
        w2b = K.sb("w2b", [128, 128], BF16)
        posT = K.sb("posT", [128, 32], F32)
        win = [K.sb("win%d" % i, [128, 255], BF16) for i in range(3)]
        hsb = K.sb("hsb", [128, 256], F32)
        h2 = K.sb("h2", [128, 256], F32)
        gT = K.sb("gT", [128, 256], BF16)
        K.memset(gT[:], 0.0)
        K.memset(kcT[:], 0.0)
        K.memset(vca[:], 0.0)
        K.dma(vca[:, :, 0:64], ovl_d, eng="gpsimd")
        K.memset(vca[:, 0, 64:65], 1.0)
        K.memset(vca[0:127, 1, 64:65], 1.0)
        for which, srcT, pos_d, w1_d, w2_d in (("k", nsa_kc, kc_pos, kc_w1, kc_w2), ("v", nsa_vc, vc_pos, vc_w1, vc_w2)):
            xT = qb[0]
            K.dma(xT[:], srcT, eng="gpsimd")
            K.dma(posT[:], pos_d)
            K.dma(w1b[:], w1_d.rr("(w d) n -> d w n", d=128), eng="gpsimd")
            K.dma(w2b[:], w2_d, eng="gpsimd")
            ph = psx[0]
            for w in range(32):
                wt = win[w % 3]
                K.ts(wt[:], xT[:, w:w + 16 * 254 + 1:16], posT[:, w:w + 1], None, ALU.add)
                K.mm(ph[:, 0:255], w1b[:, w, :], wt[:], start=(w == 0), stop=(w == 31))
            K.copy(hsb[:, 0:255], ph[:, 0:255])
            K.act(h2[:, 0:255], hsb[:, 0:255], AF.Square)
            K.ts(h2[:, 0:255], h2[:, 0:255], 0.044715, 1.0, ALU.mult, ALU.add)
            K.tt(h2[:, 0:255], h2[:, 0:255], hsb[:, 0:255], ALU.mult)
            K.act(h2[:, 0:255], h2[:, 0:255], AF.Tanh, scale=0.7978845608028654)
            K.ts(h2[:, 0:255], h2[:, 0:255], 1.0, None, ALU.add)
            K.stt(gT[:, 0:255], hsb[:, 0:255], 0.5, h2[:, 0:255], ALU.mult, ALU.mult)
            if which == "k":
                p2 = psx[1]
                K.mm(p2[:, 0:256], w2b[:], gT[:], start=True, stop=True)
                K.copy(kcT[:], p2[:, 0:256])
            else:
                for ct in range(2):
                    p2 = psx[1]
                    K.mm(p2[:, 0:128], gT[:, ct * 128:(ct + 1) * 128], w2b[:], start=True, stop=True)
                    K.copy(vca[:, ct, 65:193], p2[:, 0:128])
        imp = K.sb("imp", [128, NQT, 64], F32)

        def cmp_klist(qt):
            l = []
            for ct in range(2):
                m = cmpmask_np[:, ct, qt, :]
                if not m.any():
                    continue
                l.append((ct, None if m.all() else cmpmask[:, ct, qt, :]))
            return l
        for h in range(4):
            K.dma(qb[(h + 1) % 2][:], nsa_q[h], eng="gpsimd")

            def epi(qt, po, h=h):
                r = nr()
                K.ts(r[:], po[:, 64:65], 1e-30, None, ALU.max)
                K.recip(r[:], r[:])
                if h == 0:
                    K.ts(imp[:, qt, :], po[:, 0:64], r[:, 0:1], None, ALU.mult)
                else:
                    K.stt(imp[:, qt, :], po[:, 0:64], r[:, 0:1], imp[:, qt, :], ALU.mult, ALU.add)
                if h < 2:
                    K.tt(r[:], r[:], sg[:, qt, h:h + 1], ALU.mult)
                    K.ts(onsa[h][:, qt, :], po[:, 65:193], r[:, 0:1], None, ALU.mult)
            attn_head(K, C, [qb[(h + 1) % 2]], [kcT], vca, 193, cmp_klist, 128 ** -0.5,
                      lambda kt, qt, h=h: cmpb[:, h, kt, qt:qt + 1], None, epi)
        K.tt(imp[:], imp[:], keep[:], ALU.mult)
        K.tt(imp[:], imp[:], fill[:], ALU.add)
        selbT = K.sb("selbT", [64, S], BF16)
        m8 = [K.sb("m8_%d" % i, [128, 16], F32) for i in range(2)]
        wk = [K.sb("wk%d" % i, [128, 64], F32) for i in range(2)]
        sel = [K.sb("sel%d" % i, [128, 64], F32) for i in range(2)]
        for qt in range(NQT):
            m = m8[qt % 2]
            w_ = wk[qt % 2]
            sl = sel[qt % 2]
            K.max8(m[:, 0:8], imp[:, qt, :])
            K.match_replace(w_[:], m[:, 0:8], imp[:, qt, :], -3.0e6)
            K.max8(m[:, 8:16], w_[:])
            K.ts(sl[:], imp[:, qt, :], m[:, 15:16], None, ALU.is_ge)
            K.ts(sl[:], sl[:], BIG, -BIG, ALU.mult, ALU.add)
            pt_ = psx[qt % 2]
            K.tr(pt_[0:64, 0:128], sl[:], identf[:])
            K.copy(selbT[:, qt * 128:(qt + 1) * 128], pt_[0:64, 0:128], eng="scalar")
        for h in range(2):
            K.dma(qb[h % 2][:], nsa_q[h], eng="gpsimd")
            for br, kT_d, v_d, klf in ((1, nsa_ks, nsa_vs, causal), (2, nsa_kw, nsa_vw, band(4))):
                kbt = kb[br % 2]
                vbt = vb[br % 2]
                K.dma(kbt[:], kT_d, eng="gpsimd")
                load_v(vbt, v_d, 128)

                def epi(qt, po, h=h, br=br):
                    r = nr()
                    K.recip(r[:], po[:, 128:129])
                    K.tt(r[:], r[:], sg[:, qt, br * 2 + h:br * 2 + h + 1], ALU.mult)
                    K.stt(onsa[h][:, qt, :], po[:, 0:128], r[:, 0:1], onsa[h][:, qt, :], ALU.mult, ALU.add)
                ex = None
                if br == 1:
                    ex = lambda kt, qt: (Emat[:, kt * 128:(kt + 1) * 128], selbT[:, qt * 128:(qt + 1) * 128])
                attn_head(K, C, [qb[h % 2]], [kbt], vbt, 129, klf, 128 ** -0.5,
                          lambda kt, qt, h=h: nalb[:, h, (qt - kt):(qt - kt) + 1], ex, epi)
            store(onsa[h], h * 128, 128)
        K.S.emit()
    return nc


def _c(a):
    return np.ascontiguousarray(a, dtype=np.float32)


def b_const_inputs():
    cmpmask, ovl, keep, fill, E = nsa_tables()
    i = np.arange(128)
    return dict(cmpmask=_c(cmpmask), ovl=_c(ovl), keep=_c(keep), fill=_c(fill), Emat=_c(E),
                mdiag=_c(i[:, None] <= i[None, :]), medge=_c(i[:, None] > i[None, :]), ident=np.eye(128, dtype=np.float32))


def b_core_inputs(proj, q_mla, kv_mla, kro, P, l, hq):
    T = lambda a: _c(np.asarray(a).T)
    d = {}
    i = np.arange(128, dtype=np.float64)
    g = hq // 2
    heads = [4 * hq + r for r in range(4)]
    d["swa_qT"] = _c(np.stack([proj[:, 7008 + h * 64:7008 + (h + 1) * 64].T for h in heads]))
    d["swa_kT"] = T(proj[:, 8032 + g * 64:8032 + (g + 1) * 64])
    d["swa_v"] = _c(proj[:, 8032 + (2 + g) * 64:8032 + (3 + g) * 64])
    sl = np.array([2.0 ** (-(h + 1) / 2.0) for h in heads])
    d["swa_alb"] = _c(sl[None, :, None] * (i[:, None, None] - 63.5 - 128.0 * np.arange(2)[None, None, :]))
    d["swa_sink"] = _c(np.tile(P["swa_sinks"][l][heads][None, :], (128, 1)))
    fh = [2 * hq, 2 * hq + 1]
    bq = lambda w, h: proj[:, 2584 + (w * 8 + h) * 128:2584 + (w * 8 + h + 1) * 128]
    d["fox_qT"] = _c(np.stack([bq(0, h).T for h in fh]))
    d["fox_kT"] = _c(np.stack([bq(1, h).T for h in fh]))
    d["fox_v"] = _c(np.stack([bq(2, h) for h in fh]))
    d["fox_fl"] = _c(np.stack([proj[:, 5656 + h] for h in fh]))
    d["fox_fb"] = _c(P["fox_f_bias"][l][fh][:, None])
    q3 = q_mla.reshape(S, 8, 192)
    kv3 = kv_mla.reshape(S, 8, 256)
    d["mla_qnT"] = _c(np.stack([q3[:, h, 0:128].T for h in fh]))
    d["mla_qrT"] = _c(np.stack([q3[:, h, 128:192].T for h in fh]))
    d["mla_knT"] = _c(np.stack([kv3[:, h, 0:128].T for h in fh]))
    d["mla_krT"] = T(kro)
    d["mla_v"] = _c(np.stack([kv3[:, h, 128:256] for h in fh]))
    mine = [2 * hq, 2 * hq + 1]
    nh = mine + [h for h in range(4 * g, 4 * g + 4) if h not in mine]
    d["nsa_qT"] = _c(np.stack([proj[:, h * 128:(h + 1) * 128].T for h in nh]))
    akv = lambda br, kvi: proj[:, 1024 + ((br * 2 + kvi) * 2 + g) * 128:1024 + ((br * 2 + kvi) * 2 + g + 1) * 128]
    d["nsa_kcT"], d["nsa_vcT"] = T(akv(0, 0)), T(akv(0, 1))
    d["nsa_ksT"], d["nsa_vs"] = T(akv(1, 0)), _c(akv(1, 1))
    d["nsa_kwT"], d["nsa_vw"] = T(akv(2, 0)), _c(akv(2, 1))
    d["nsa_gl"] = _c(np.stack([proj[:, 2560 + br * 8 + h] for br in range(3) for h in mine], axis=1))
    d["kc_posT"], d["kc_w1"], d["kc_w2"] = T(P["nsa_kc_pos"][l]), _c(P["nsa_kc_w1"][l]), _c(P["nsa_kc_w2"][l])
    d["vc_posT"], d["vc_w1"], d["vc_w2"] = T(P["nsa_vc_pos"][l]), _c(P["nsa_vc_w1"][l]), _c(P["nsa_vc_w2"][l])
    nsl = np.array([2.0 ** (-(h + 1)) for h in nh])
    d["nsa_alb"] = _c(nsl[None, :, None] * (i[:, None, None] - 63.5 - 128.0 * np.arange(NQT)[None, None, :]))
    cend = 16.0 * (np.arange(2)[None, :, None] * 128 + i[:, None, None]) + 31.0
    tref = np.arange(NQT)[None, None, :] * 128 + 63.5
    cb = nsl[None, :, None, None] * (cend - tref)[:, None, :, :]
    d["cmpb"] = _c(np.minimum(cb, 40.0))
    return d


def build_d():
    nc = bass.Bass("TRN2", target_bir_lowering=False)
    with contextlib.ExitStack() as st:
        K = KB(nc, st)
        I = lambda n, s, dt=F32: K.dram(n, s, dt, "ExternalInput")
        mg_d, rt_d, g8_d = I("mg", [NTOK, 8]), I("rtall", [NTOK, 4]), I("g8", [1])
        xn2_d = I("xn2", [NTOK, D], BF16)
        gffn_d = I("gffn", [D])
        wg_d, wu_d, wd_d = I("wg", [8, D, 384]), I("wu", [8, D, 384]), I("wd", [8, 384, D])
        tri_d, io384_d, pn_d = I("tri", [128, 128]), I("iota384", [128, 384]), I("pn", [128, NTT, 2])
        ident_d, io8_d = I("ident", [128, 128]), I("iota8", [128, 8])
        y_d = K.dram("y", [8 * CE, D], F32, "ExternalOutput")
        dl_d = K.dram("destl", [NTOK, 2], F32, "ExternalOutput")

        identf = K.sb("identf", [128, 128], F32)
        identb = K.sb("identb", [128, 128], BF16)
        trif = K.sb("trif", [128, 128], F32)
        trib = K.sb("trib", [128, 128], BF16)
        oneb = K.sb("oneb", [128, 128], BF16)
        io384 = K.sb("io384", [128, 384], F32)
        io8 = K.sb("io8", [128, 8], F32)
        pnf = K.sb("pnf", [128, NTT, 2], F32)
        pnb = K.sb("pnb", [128, NTT, 2], BF16)
        g2col = K.sb("g2col", [128, 32], F32)
        g8 = K.sb("g8s", [128, 1], F32)
        Mt = K.sb("Mt", [128, NTT, 8], F32)
        Mb = K.sb("Mb", [128, NTT, 8], BF16)
        Rf = K.sb("Rf", [128, NTT, 8], F32)
        Rb = K.sb("Rb", [128, NTT, 8], BF16)
        slot = K.sb("slot", [128, NTT, 8], F32)
        oh = K.sb("ohd", [128, NTT, 8], F32)
        rt = K.sb("rt", [128, NTT, 4], F32)
        rel = K.sb("rel", [128, NTT], F32)
        sl = K.sb("sl", [128, NTT], F32)
        dls = K.sb("dls", [128, NTT, 2], F32)
        OHb = [K.sb("OHb%d" % i, [128, 384], BF16) for i in range(4)]
        idxf = K.sb("idxf", [128, 3, 8, 2], F32)
        idxv = K.sb("idxv", [128, 3, 8], F32)
        idxi = K.sb("idxi", [128, 3, 8], I32)
        B = [K.ps("B%d" % i, [128, 512], F32) for i in range(4)]
        tpb = [K.ps("tpb%d" % i, [128, 8, 128], BF16) for i in range(2)]
        psy = [K.ps("psy%d" % i, [128, 512], F32) for i in range(2)]

        K.dma(identf[:], ident_d)
        K.copy(identb[:], identf[:])
        K.dma(trif[:], tri_d)
        K.copy(trib[:], trif[:])
        K.memset(oneb[:], 1.0)
        K.dma(io384[:], io384_d)
        K.dma(io8[:], io8_d)
        K.dma(pnf[:], pn_d)
        K.copy(pnb[:], pnf[:])
        K.dma(g2col[:], col_view(gffn_d), allow_slow_non_contiguous=True)
        K.dma(g8[:], V(g8_d.ap.partition_broadcast(128), g8_d.res))
        K.dma(Mt[:], mg_d.rr("(n p) e -> p n e", p=128))
        K.dma(rt[:], rt_d.rr("(n p) k -> p n k", p=128))
        K.copy(Mb[:], Mt[:])
        K.memset(Rf[:, 0, :], 0.0)
        for n in range(1, NTT):
            K.tt(Rf[:, n, :], Rf[:, n - 1, :], Mt[:, n - 1, :], ALU.add)
        K.copy(Rb[:], Rf[:])
        sv = B[0][:].rr("p (n e) -> p n e", e=8)
        for n in range(NTT):
            K.mm(sv[:, n, :], trib[:], Mb[:, n, :], start=True, stop=False)
            K.mm(sv[:, n, :], oneb[:], Rb[:, n, :], start=False, stop=True)
        K.copy(slot[:], sv)
        io8_3 = io8[:].un(1).bc([128, NTT, 8])
        for k in range(2):
            K.ts(rel[:], rt[:, :, k], g8[:, 0:1], None, ALU.subtract)
            K.tt(oh[:], io8_3, rel[:].un(2).bc([128, NTT, 8]), ALU.is_equal)
            K.tt(oh[:], oh[:], slot[:], ALU.mult)
            K.reduce(sl[:], oh[:], ALU.add)
            K.stt(dls[:, :, k], rel[:], float(CE), sl[:], ALU.mult, ALU.add)
        K.dma(dl_d.rr("(n p) k -> p n k", p=128), dls[:])
        psi = [B[1 + s_][:, 0:16].rr("p (e k) -> p e k", k=2) for s_ in range(3)]
        oc = 0
        for e in range(8):
            for n in range(NTT):
                o_ = OHb[oc % 4]
                oc += 1
                K.ts(o_[:], io384[:], slot[:, n, e:e + 1], Mt[:, n, e:e + 1], ALU.is_equal, ALU.mult)
                for s_ in range(3):
                    K.mm(psi[s_][:, e, :], o_[:, s_ * 128:(s_ + 1) * 128], pnb[:, n, :], start=(n == 0), stop=(n == NTT - 1))
        for s_ in range(3):
            K.copy(idxf[:, s_, :, :], psi[s_])
        K.stt(idxv[:], idxf[:, :, :, 1], 128.0, idxf[:, :, :, 0], ALU.mult, ALU.add)
        K.copy(idxi[:], idxv[:])
        XeT = K.sb("XeT", [128, 32, CE], BF16)
        wgb = [K.sb("wgb%d" % i, [128, 32, 384], BF16) for i in range(2)]
        wub = [K.sb("wub%d" % i, [128, 32, 384], BF16) for i in range(2)]
        wdb = K.sb("wdb", [128, 3, D], BF16)
        xg = [K.sb("xg%d" % i, [128, D], BF16) for i in range(2)]
        ysb = K.sb("ysb", [128, D], F32)
        actT = K.sb("actT", [128, 3, CE], BF16)
        sil = [K.sb("sil%d" % i, [128, CE], F32) for i in range(2)]
        gc = 0
        for e in range(8):
            K.dma(wgb[e % 2][:], wg_d[e].rr("(c p) f -> p c f", p=128), eng="gpsimd")
            K.dma(wub[e % 2][:], wu_d[e].rr("(c p) f -> p c f", p=128), eng="gpsimd")
            for s_ in range(3):
                x_ = xg[gc % 2]
                gc += 1
                K.gather(x_[:], xn2_d, idxi[:, s_, e:e + 1])
                for k in range(4):
                    tp = tpb[k % 2]
                    for c in range(8):
                        cc = k * 8 + c
                        K.tr(tp[:, c, :], x_[:, cc * 128:(cc + 1) * 128], identb[:])
                    K.tt(XeT.sub(k, (slice(None), slice(k * 8, k * 8 + 8), slice(s_ * 128, (s_ + 1) * 128))),
                         tp[:], g2col[:, k * 8:k * 8 + 8].un(2).bc([128, 8, 128]), ALU.mult)
            K.dma(wdb[:], wd_d[e].rr("(c p) n -> p c n", p=128), eng="gpsimd")
            for f in range(3):
                pg, pu = B[f % 2], B[2 + f % 2]
                for c in range(32):
                    K.mm(pg[:, 0:CE], wgb[e % 2][:, c, f * 128:(f + 1) * 128],
                         XeT.sub(c // 8, (slice(None), c, slice(None))), start=(c == 0), stop=(c == 31))
                for c in range(32):
                    K.mm(pu[:, 0:CE], wub[e % 2][:, c, f * 128:(f + 1) * 128],
                         XeT.sub(c // 8, (slice(None), c, slice(None))), start=(c == 0), stop=(c == 31))
                K.act(sil[f % 2][:], pg[:, 0:CE], AF.Silu)
                K.tt(actT[:, f, :], sil[f % 2][:], pu[:, 0:CE], ALU.mult)
            for s_ in range(3):
                for j in range(8):
                    py = psy[j % 2]
                    for f in range(3):
                        K.mm(py[:], actT[:, f, s_ * 128:(s_ + 1) * 128], wdb[:, f, j * 512:(j + 1) * 512],
                             start=(f == 0), stop=(f == 2))
                    K.copy(ysb[:, j * 512:(j + 1) * 512], py[:], eng=("scalar" if j % 2 else "vector"))
                r0 = e * CE + s_ * 128
                K.dma(y_d[r0:r0 + 128, :], ysb[:])
        K.S.emit()
    return nc


def build_e(final):
    nc = bass.Bass("TRN2", target_bir_lowering=False)
    with contextlib.ExitStack() as st:
        K = KB(nc, st)
        I = lambda n, s, dt=F32: K.dram(n, s, dt, "ExternalInput")
        xm_d, y_d = I("xmid", [NT, D]), I("yall", [64 * CE, D])
        dl8_d, rt_d = I("destl8", [8, NT, 2]), I("rt", [NT, 4])
        lo8_d, base8_d, gfin_d = I("lo8", [128, 8]), I("base8", [128, 8]), I("gfin", [D])
        out_d = K.dram("xout", [NT, D], F32, "ExternalOutput")
        lo8 = K.sb("lo8s", [128, 8], F32)
        base8 = K.sb("base8s", [128, 8], F32)
        K.dma(lo8[:], lo8_d)
        K.dma(base8[:], base8_d)
        gb = K.sb("gb", [128, D], F32)
        if final:
            K.dma(gb[:], V(gfin_d.ap.partition_broadcast(128), gfin_d.res))
        xm = K.sb("xm", [128, D], F32)
        ya = K.sb("ya", [128, D], F32)
        yb = K.sb("yb", [128, D], F32)
        acc = K.sb("acc", [128, D], F32)
        junk = K.sb("junk", [128, D], BF16)
        rts = K.sb("rts", [128, 4], F32)
        dl8 = K.sb("dl8", [128, 8, 2], F32)
        g1 = K.sb("g1", [128, 8], F32)
        g2 = K.sb("g2", [128, 8], F32)
        tmp8 = K.sb("tmp8", [128, 8], F32)
        df = K.sb("df", [128, 2], F32)
        di = K.sb("di", [128, 2], I32)
        ss = K.sb("ss", [128, 1], F32)
        tmp = K.sb("tmp", [128, 1], F32)
        rs = K.sb("rs", [128, 1], F32)
        for n in range(NT // 128):
            r0 = n * 128
            K.dma(xm[:], xm_d[r0:r0 + 128, :])
            K.dma(rts[:], rt_d[r0:r0 + 128, :])
            K.dma(dl8[:], dl8_d[:, r0:r0 + 128, :].rr("g t k -> t g k"))
            K.ts(g1[:], lo8[:], rts[:, 0:1], None, ALU.is_le)
            K.ts(g2[:], lo8[:], 8.0, rts[:, 0:1], ALU.add, ALU.is_gt)
            K.tt(g1[:], g1[:], g2[:], ALU.mult)
            for k in range(2):
                K.tt(tmp8[:], dl8[:, :, k], base8[:], ALU.add)
                K.tt(tmp8[:], tmp8[:], g1[:], ALU.mult)
                K.reduce(df[:, k:k + 1], tmp8[:], ALU.add)
            K.copy(di[:], df[:])
            K.gather(ya[:], y_d, di[:, 0:1])
            K.gather(yb[:], y_d, di[:, 1:2])
            K.stt(acc[:], ya[:], rts[:, 2:3], xm[:], ALU.mult, ALU.add)
            K.stt(acc[:], yb[:], rts[:, 3:4], acc[:], ALU.mult, ALU.add)
            if final:
                K.act(junk[:], acc[:], AF.Square, accum=ss[:])
                K.rstd(rs[:], ss[:], float(D), tmp[:])
                K.stt(acc[:], acc[:], rs[:, 0:1], gb[:], ALU.mult, ALU.mult)
            K.dma(out_d[r0:r0 + 128, :], acc[:])
        K.S.emit()
    return nc


_PROGS = {}
_DBG = None
_DBG_STOP = False


def _prog(name, fn):
    if name not in _PROGS:
        _PROGS[name] = fn()
    return _PROGS[name]


def _run(name, fn, in_maps):
    res = run_bass_kernel_spmd(_prog(name, fn), in_maps, core_ids=list(range(8)))
    return res.results


CE2 = 1024
CG = 4096


def build_d2():
    nc = bass.Bass("TRN2", target_bir_lowering=False)
    NS = CE2 // 128
    with contextlib.ExitStack() as st:
        K = KB(nc, st)
        I = lambda n, s, dt=F32: K.dram(n, s, dt, "ExternalInput")
        mg_d, rt_d, g8_d = I("mg", [NTOK, 8]), I("rtall", [NTOK, 4]), I("g8", [1])
        xn2_d = I("xn2", [NTOK, D], BF16)
        gffn_d = I("gffn", [D])
        wg_d, wu_d, wd_d = I("wg", [8, D, 384]), I("wu", [8, D, 384]), I("wd", [8, 384, D])
        tri_d, tok_d = I("tri", [128, 128]), I("tokid", [128, NTT])
        ident_d, io8_d, lo8_d = I("ident", [128, 128]), I("iota8", [128, 8]), I("lo8", [128, 8])
        tr1_d, tr2_d = I("trash1", [128, 1]), I("trash2", [128, 1])
        y_d = K.dram("y", [8 * CE2, D], F32, "ExternalOutput")
        z_d = K.dram("z", [CG, D], F32, "ExternalOutput")
        inv_d = K.dram("inv", [128, NTT], I32, "ExternalOutput")
        info_d = K.dram("info", [NTOK, 4], F32, "ExternalOutput")
        idx_d = K.dram("idxl", [8 * CE2 + 128, 2], I32, "ExternalOutput")
        tl_d = K.dram("tokl", [CG + 128, 2], I32, "ExternalOutput")

        identf = K.sb("identf", [128, 128], F32)
        identb = K.sb("identb", [128, 128], BF16)
        trif = K.sb("trif", [128, 128], F32)
        trib = K.sb("trib", [128, 128], BF16)
        oneb = K.sb("oneb", [128, 128], BF16)
        io8 = K.sb("io8", [128, 8], F32)
        lo8 = K.sb("lo8s", [128, 8], F32)
        tokf = K.sb("tokf", [128, NTT], F32)
        toki = K.sb("toki", [128, NTT, 2], I32)
        g2col = K.sb("g2col", [128, 32], F32)
        g8 = K.sb("g8s", [128, 1], F32)
        tr1 = K.sb("tr1", [128, 1], F32)
        tr2 = K.sb("tr2", [128, 1], F32)
        Mt = K.sb("Mt", [128, NTT, 8], F32)
        Mb = K.sb("Mb", [128, NTT, 8], BF16)
        Rf = K.sb("Rf", [128, NTT, 8], F32)
        Rb = K.sb("Rb", [128, NTT, 8], BF16)
        slot = K.sb("slot", [128, NTT, 8], F32)
        G8 = K.sb("G8", [128, NTT, 8], F32)
        posa = K.sb("posa", [128, NTT, 8], F32)
        oh = K.sb("ohd", [128, NTT, 8], F32)
        oh2 = K.sb("ohd2", [128, NTT, 8], F32)
        rt = K.sb("rt", [128, NTT, 4], F32)
        rel = K.sb("rel", [128, NTT], F32)
        sl = K.sb("sl", [128, NTT], F32)
        okk = K.sb("okk", [128, NTT], F32)
        ok2 = K.sb("ok2", [128, NTT], F32)
        ing = K.sb("ing", [128, NTT], F32)
        dtmp = K.sb("dtmp", [128, NTT], F32)
        info = K.sb("infos", [128, NTT, 4], F32)
        di = [K.sb("di%d" % k, [128, NTT], I32) for k in range(3)]
        invf = K.sb("invf", [128, NTT], F32)
        invi = K.sb("invi", [128, NTT], I32)
        zt = K.sb("zt", [128, (8 * CE2 + 128) // 128, 2], I32)
        pgp = K.ps("pgp", [128, CE2], F32)
        pup = K.ps("pup", [128, CE2], F32)
        tpb = [K.ps("tpb%d" % i, [128, 8, 128], BF16) for i in range(2)]
        psy = [K.ps("psy%d" % i, [128, 512], F32) for i in range(2)]

        K.dma(identf[:], ident_d)
        K.copy(identb[:], identf[:])
        K.dma(trif[:], tri_d)
        K.copy(trib[:], trif[:])
        K.memset(oneb[:], 1.0)
        K.dma(io8[:], io8_d)
        K.dma(lo8[:], lo8_d)
        K.dma(tokf[:], tok_d)
        K.copy(toki[:, :, 0], tokf[:])
        K.copy(toki[:, :, 1], tokf[:])
        K.dma(tr1[:], tr1_d)
        K.dma(tr2[:], tr2_d)
        K.dma(g2col[:], col_view(gffn_d), allow_slow_non_contiguous=True)
        K.dma(g8[:], V(g8_d.ap.partition_broadcast(128), g8_d.res))
        K.dma(Mt[:], mg_d.rr("(n p) e -> p n e", p=128))
        K.dma(rt[:], rt_d.rr("(n p) k -> p n k", p=128))
        K.memset(zt[:], 0)
        idx_rs = [Res("idxr%d" % i) for i in range(6)]
        tl_rs = [Res("tlr%d" % i) for i in range(3)]
        K.dma(idx_d.rr("(s p) o -> p s o", p=128), zt[:], xw=idx_rs)
        K.dma(tl_d.rr("(s p) o -> p s o", p=128), zt[:, 0:(CG + 128) // 128, :], xw=tl_rs)

        def excl_cumsum(dst, src_f, pv):
            K.copy(Mb[:], src_f)
            K.memset(Rf[:, 0, :], 0.0)
            for n in range(1, NTT):
                K.tt(Rf[:, n, :], Rf[:, n - 1, :], src_f[:, n - 1, :], ALU.add)
            K.copy(Rb[:], Rf[:])
            for n in range(NTT):
                K.mm(pv[:, n, :], trib[:], Mb[:, n, :], start=True, stop=False)
                K.mm(pv[:, n, :], oneb[:], Rb[:, n, :], start=False, stop=True)
            K.copy(dst, pv)
        sv = psy[0][:].rr("p (n e) -> p n e", e=8)
        excl_cumsum(slot[:], Mt[:], sv)
        io8_3 = io8[:].un(1).bc([128, NTT, 8])
        lo8_3 = lo8[:].un(1).bc([128, NTT, 8])
        ea3 = rt[:, :, 0].un(2).bc([128, NTT, 8])
        K.tt(G8[:], lo8_3, ea3, ALU.is_le)
        K.ts(oh[:], lo8_3, 8.0, None, ALU.add)
        K.tt(oh[:], oh[:], ea3, ALU.is_gt)
        K.tt(G8[:], G8[:], oh[:], ALU.mult)
        sv2 = psy[1][:].rr("p (n e) -> p n e", e=8)
        excl_cumsum(posa[:], G8[:], sv2)
        K.tt(oh[:], G8[:], posa[:], ALU.mult)
        K.reduce(sl[:], oh[:], ALU.add)
        K.tt(oh[:], G8[:], io8_3, ALU.mult)
        K.reduce(dtmp[:], oh[:], ALU.add)
        K.stt(invf[:], dtmp[:], float(CG), sl[:], ALU.mult, ALU.add)
        K.copy(invi[:], invf[:])
        K.dma(inv_d, invi[:])
        K.ts(rel[:], rt[:, :, 0], g8[:, 0:1], None, ALU.subtract)
        K.ts(ing[:], rel[:], 0.0, None, ALU.is_ge)
        K.ts(ok2[:], rel[:], 8.0, None, ALU.is_lt)
        K.tt(ing[:], ing[:], ok2[:], ALU.mult)
        K.ts(ok2[:], sl[:], float(CG), None, ALU.is_lt)
        K.tt(ok2[:], ok2[:], ing[:], ALU.mult)
        K.ts(dtmp[:], sl[:], tr2[:, 0:1], None, ALU.subtract)
        K.tt(dtmp[:], dtmp[:], ok2[:], ALU.mult)
        K.ts(dtmp[:], dtmp[:], tr2[:, 0:1], None, ALU.add)
        K.copy(di[2][:], dtmp[:])
        for k in range(2):
            K.ts(rel[:], rt[:, :, k], g8[:, 0:1], None, ALU.subtract)
            K.tt(oh2[:], io8_3, rel[:].un(2).bc([128, NTT, 8]), ALU.is_equal)
            K.tt(oh2[:], oh2[:], slot[:], ALU.mult)
            K.reduce(sl[:], oh2[:], ALU.add)
            K.stt(info[:, :, k], rel[:], float(CE2), sl[:], ALU.mult, ALU.add)
            K.ts(okk[:], sl[:], float(CE2), None, ALU.is_lt)
            K.tt(info[:, :, 2 + k], rt[:, :, 2 + k], okk[:], ALU.mult)
            K.tt(okk[:], okk[:], ing[:], ALU.mult)
            K.ts(dtmp[:], info[:, :, k], tr1[:, 0:1], None, ALU.subtract)
            K.tt(dtmp[:], dtmp[:], okk[:], ALU.mult)
            K.ts(dtmp[:], dtmp[:], tr1[:, 0:1], None, ALU.add)
            K.copy(di[k][:], dtmp[:])
        K.dma(info_d.rr("(n p) k -> p n k", p=128), info[:])
        for n in range(NTT):
            for k in range(2):
                K.scatter(V(idx_d.ap, idx_rs[(2 * n + k) % 6]), di[k][:, n:n + 1], toki[:, n, :])
            K.scatter(V(tl_d.ap, tl_rs[n % 3]), di[2][:, n:n + 1], toki[:, n, :])
        XeT = K.sb("XeT", [128, 32, CE2], BF16)
        wgb = K.sb("wgb", [128, 32, 384], BF16)
        wub = K.sb("wub", [128, 32, 384], BF16)
        wdb = K.sb("wdb", [128, 3, D], BF16)
        xg = [K.sb("xg%d" % i, [128, D], BF16) for i in range(2)]
        ysb = K.sb("ysb", [128, D], F32)
        actT = K.sb("actT", [128, 3, CE2], BF16)
        sil = K.sb("sil", [128, CE2], F32)
        idxt = K.sb("idxt", [128, NS, 2], I32)
        gc = 0
        for e in range(8):
            K.dma(wgb[:], wg_d[e].rr("(c p) f -> p c f", p=128), eng="gpsimd")
            K.dma(wub[:], wu_d[e].rr("(c p) f -> p c f", p=128), eng="gpsimd")
            K.dma(idxt[:], idx_d[e * CE2:(e + 1) * CE2, :].rr("(s p) o -> p s o", p=128), xr=idx_rs)
            for s_ in range(NS):
                x_ = xg[gc % 2]
                gc += 1
                K.gather(x_[:], xn2_d, idxt[:, s_, 0:1])
                for k in range(4):
                    tp = tpb[k % 2]
                    for c in range(8):
                        cc = k * 8 + c
                        K.tr(tp[:, c, :], x_[:, cc * 128:(cc + 1) * 128], identb[:])
                    K.tt(XeT.sub(k, (slice(None), slice(k * 8, k * 8 + 8), slice(s_ * 128, (s_ + 1) * 128))),
                         tp[:], g2col[:, k * 8:k * 8 + 8].un(2).bc([128, 8, 128]), ALU.mult)
            K.dma(wdb[:], wd_d[e].rr("(c p) n -> p c n", p=128), eng="gpsimd")
            for f in range(3):
                for (wb_, pp) in ((wgb, pgp), (wub, pup)):
                    for hh in range(CE2 // 512):
                        for c in range(32):
                            K.mm(pp.sub(hh, (slice(None), slice(hh * 512, (hh + 1) * 512))), wb_[:, c, f * 128:(f + 1) * 128],
                                 XeT.sub(c // 8, (slice(None), c, slice(hh * 512, (hh + 1) * 512))), start=(c == 0), stop=(c == 31))
                for hh in range(CE2 // 512):
                    hs = slice(hh * 512, (hh + 1) * 512)
                    K.act(sil[:, hs], pgp.sub(hh, (slice(None), hs)), AF.Silu)
                    K.tt(actT[:, f, hs], sil[:, hs], pup.sub(hh, (slice(None), hs)), ALU.mult)
            for s_ in range(NS):
                for j in range(8):
                    py = psy[j % 2]
                    for f in range(3):
                        K.mm(py[:], actT[:, f, s_ * 128:(s_ + 1) * 128], wdb[:, f, j * 512:(j + 1) * 512],
                             start=(f == 0), stop=(f == 2))
                    K.copy(ysb[:, j * 512:(j + 1) * 512], py[:], eng=("scalar" if j % 2 else "vector"))
                r0 = e * CE2 + s_ * 128
                K.dma(y_d[r0:r0 + 128, :], ysb[:])
        tlt = K.sb("tlt", [128, CG // 128, 2], I32)
        inf = [K.sb("inf%d" % i, [128, 4], F32) for i in range(2)]
        ii = [K.sb("ii%d" % i, [128, 2], I32) for i in range(2)]
        cb = [V(XeT.h[:, 8 * k:8 * k + 8, :].rearrange("p c t -> p (c t)").bitcast(F32), XeT.sub(k).res) for k in range(4)]
        K.dma(tlt[:], tl_d[0:CG, :].rr("(s p) o -> p s o", p=128), xr=tl_rs)
        for j in range(CG // 128):
            f_ = inf[j % 2]
            i_ = ii[j % 2]
            ya, yb = cb[(2 * j) % 4], cb[(2 * j + 1) % 4]
            K.gather(f_[:], info_d, tlt[:, j, 0:1])
            K.ts(f_[:, 0:2], f_[:, 0:2], 0.0, float(8 * CE2 - 1), ALU.max, ALU.min)
            K.copy(i_[:], f_[:, 0:2])
            K.gather(ya[:], y_d, i_[:, 0:1])
            K.gather(yb[:], y_d, i_[:, 1:2])
            K.ts(ya[:], ya[:], f_[:, 2:3], None, ALU.mult)
            K.stt(ya[:], yb[:], f_[:, 3:4], ya[:], ALU.mult, ALU.add)
            K.dma(z_d[j * 128:(j + 1) * 128, :], ya[:])
        K.S.emit()
    return nc


def build_f():
    nc = bass.Bass("TRN2", target_bir_lowering=False)
    with contextlib.ExitStack() as st:
        K = KB(nc, st)
        x_d = K.dram("x", [NT, D], F32, "ExternalInput")
        xa_d = K.dram("xadd", [NT, D], F32, "ExternalInput")
        g_d = K.dram("g", [D], F32, "ExternalInput")
        o_d = K.dram("xout", [NT, D], F32, "ExternalOutput")
        gb = K.sb("gb", [128, D], F32)
        K.dma(gb[:], V(g_d.ap.partition_broadcast(128), g_d.res))
        xa = [K.sb("xa%d" % i, [128, D], F32) for i in range(2)]
        xb = [K.sb("xb%d" % i, [128, D], F32) for i in range(2)]
        junk = K.sb("junk", [128, D], BF16)
        ss = [K.sb("ss%d" % i, [128, 1], F32) for i in range(2)]
        tmp = [K.sb("tmp%d" % i, [128, 1], F32) for i in range(2)]
        rs = [K.sb("rs%d" % i, [128, 1], F32) for i in range(2)]
        for n in range(NT // 128):
            a, b = xa[n % 2], xb[n % 2]
            K.dma(a[:], x_d[n * 128:(n + 1) * 128, :])
            K.dma(b[:], xa_d[n * 128:(n + 1) * 128, :])
            K.tt(a[:], a[:], b[:], ALU.add)
            K.act(junk[:], a[:], AF.Square, accum=ss[n % 2][:])
            K.rstd(rs[n % 2][:], ss[n % 2][:], float(D), tmp[n % 2][:])
            K.stt(b[:], a[:], rs[n % 2][:, 0:1], gb[:], ALU.mult, ALU.mult)
            K.dma(o_d[n * 128:(n + 1) * 128, :], b[:])
        K.S.emit()
    return nc


def kernel(**P):
    P = {k: np.asarray(v) for k, v in P.items()}
    xmid = _c(P["x"]).reshape(NTOK, D)
    moe = np.zeros((NTOK, D), np.float32)
    ident = np.eye(128, dtype=np.float32)
    i128 = np.arange(128)
    iota64 = _c(np.tile(np.arange(64)[None, :], (128, 1)))
    tri = _c(np.triu(np.ones((128, 128)), 1))
    iota8 = _c(np.tile(np.arange(8)[None, :], (128, 1)))
    lo8 = _c(iota8 * 8.0)
    tokid = _c(np.arange(NTT)[None, :] * 128 + i128[:, None])
    trash1 = _c((8 * CE2 + i128)[:, None])
    trash2 = _c((CG + i128)[:, None])
    pos = np.arange(S, dtype=np.float32)
    inv = (10000.0 ** (-np.arange(0, 64, 2, dtype=np.float32) / 64)).astype(np.float32)
    ang = pos[:, None] * inv[None, :]
    cosT, sinT = np.cos(ang).astype(np.float32), np.sin(ang).astype(np.float32)
    bconst = b_const_inputs()
    sh = lambda a, c: a[c * NT:(c + 1) * NT]
    for l in range(2):
        r = _run("a", build_a, [dict(x=_c(sh(xmid, c)), xadd=_c(sh(moe, c)), g=_c(P["norm_mix_g"][l]), w=_c(P["w_in"][l]),
                                     ident=ident) for c in range(8)])
        proj = np.concatenate([r[c]["proj"] for c in range(8)], axis=0)
        x = np.concatenate([r[c]["xsum"] for c in range(8)], axis=0)
        r = _run("a2", build_a2, [dict(cq=_c(sh(proj, c)[:, 5664:6432]), ckv=_c(sh(proj, c)[:, 6432:6944]),
                                       kr=_c(sh(proj, c)[:, 6944:7008]), gq=_c(P["mla_q_norm_g"][l]),
                                       gkv=_c(P["mla_kv_norm_g"][l]), wuq=_c(P["mla_w_uq"][l]), wukv=_c(P["mla_w_ukv"][l]),
                                       cos=_c(cosT[(c % 4) * NT:(c % 4 + 1) * NT]), sin=_c(sinT[(c % 4) * NT:(c % 4 + 1) * NT]),
                                       ident=ident) for c in range(8)])
        qm = np.concatenate([r[c]["q"] for c in range(8)], axis=0)
        kvm = np.concatenate([r[c]["kv"] for c in range(8)], axis=0)
        krm = np.concatenate([r[c]["kro"] for c in range(8)], axis=0)
        maps = []
        for c in range(8):
            b, hq = c // 4, c % 4
            bs = slice(b * S, (b + 1) * S)
            d = dict(bconst)
            d.update(b_core_inputs(proj[bs], qm[bs], kvm[bs], krm[bs], P, l, hq))
            maps.append(d)
        r = _run("b", build_b, maps)
        o_all = np.empty((NTOK, D), np.float32)
        for c in range(8):
            b, hq = c // 4, c % 4
            o = r[c]["out"]
            for gi in range(4):
                o_all[b * S:(b + 1) * S, gi * 1024 + hq * 256:gi * 1024 + (hq + 1) * 256] = o[:, gi * 256:(gi + 1) * 256]
        rw = _c(np.concatenate([P["router_group_w"][l], P["router_expert_w"][l]], axis=1))
        rb = _c(np.concatenate([P["router_group_b"][l], P["router_expert_b"][l]]))
        r = _run("c", build_c, [dict(o=_c(sh(o_all, c)), x=_c(sh(x, c)), gout=_c(P["out_norm_g"][l]), wout=_c(P["w_out"][l]),
                                     gffn=_c(P["norm_ffn_g"][l]), rw=rw, rb=rb, ident=ident, iota64=iota64) for c in range(8)])
        xmid = np.concatenate([r[c]["xmid"] for c in range(8)], axis=0)
        xn2 = np.concatenate([r[c]["xn2"] for c in range(8)], axis=0)
        mall = np.concatenate([r[c]["mroute"] for c in range(8)], axis=0)
        rtall = np.concatenate([r[c]["route"] for c in range(8)], axis=0)
        r = _run("d", build_d2, [dict(mg=_c(mall[:, 8 * g:8 * g + 8]), rtall=_c(rtall), g8=np.array([8.0 * g], np.float32),
                                      xn2=np.ascontiguousarray(xn2), gffn=_c(P["norm_ffn_g"][l]),
                                      wg=_c(P["exp_w_gate"][l][8 * g:8 * g + 8]), wu=_c(P["exp_w_up"][l][8 * g:8 * g + 8]),
                                      wd=_c(P["exp_w_down"][l][8 * g:8 * g + 8]), tri=tri, tokid=tokid, ident=ident,
                                      iota8=iota8, lo8=lo8, trash1=trash1, trash2=trash2) for g in range(8)])
        zall = np.concatenate([r[g]["z"] for g in range(8)], axis=0)
        moe = np.take(zall, r[0]["inv"].T.reshape(-1), axis=0, mode="clip")
        if _DBG is not None:
            _DBG.append(dict(proj=proj, o_all=o_all, xmid=xmid, moe=moe, rtall=rtall, mall=mall))
            if _DBG_STOP:
                return xmid + moe
    r = _run("f", build_f, [dict(x=_c(sh(xmid, c)), xadd=_c(sh(moe, c)), g=_c(P["final_norm_g"])) for c in range(8)])
    out = np.concatenate([r[c]["xout"] for c in range(8)], axis=0)
    return out.reshape(2, S, D).astype(np.float32)
```

```python
import numpy as np
import ml_dtypes
import concourse.bass as bass
import concourse.mybir as mybir
from concourse.bass_utils import run_bass_kernel_spmd

F32 = mybir.dt.float32
BF16 = mybir.dt.bfloat16
I32 = mybir.dt.int32
AF = mybir.ActivationFunctionType
ALU = mybir.AluOpType
AX = mybir.AxisListType

ENGS = ("tensor", "vector", "scalar", "gpsimd", "sync")
DMA_SLOTS = 8


class Res:
    __slots__ = ("name", "lw", "rd")

    def __init__(self, name):
        self.name = name
        self.lw = None
        self.rd = {}


class Op:
    __slots__ = ("eng", "fn", "dma", "deps", "sig", "sem", "val", "idx")

    def __init__(self, eng, fn, dma):
        self.eng = eng
        self.fn = fn
        self.dma = dma
        self.deps = []
        self.sig = False
        self.sem = None
        self.val = 0


class Sched:
    def __init__(self, nc):
        self.nc = nc
        self.ops = []
        self.dma_cnt = {e: 0 for e in ENGS}
        self.dma_last = {}
        self.out_dmas = []

    def op(self, eng, fn, reads=(), writes=(), dma=False):
        o = Op(eng, fn, dma)
        o.idx = len(self.ops)
        deps = {}
        for r in reads:
            if r.lw is not None:
                deps[r.lw.idx] = (r.lw, "raw")
        for w in writes:
            if w.lw is not None and w.lw.idx not in deps:
                deps[w.lw.idx] = (w.lw, "waw")
            for rr in w.rd.values():
                if rr.idx not in deps:
                    deps[rr.idx] = (rr, "war")
        if dma:
            n = self.dma_cnt[eng]
            self.dma_cnt[eng] = n + 1
            slot = n % DMA_SLOTS
            o.sem = ("dma", eng, slot)
            o.val = 16 * (n // DMA_SLOTS + 1)
            o.sig = True
            prev = self.dma_last.get((eng, slot))
            if prev is not None and prev.idx not in deps:
                deps[prev.idx] = (prev, "slot")
            self.dma_last[(eng, slot)] = o
        for d, kind in deps.values():
            if not d.dma and d.eng == eng:
                if kind != "raw" or eng == "tensor":
                    continue
            o.deps.append(d)
            d.sig = True
        for r in reads:
            r.rd[o.sem if dma else eng] = o
        for w in writes:
            w.lw = o
            w.rd = {}
        self.ops.append(o)
        return o

    def emit(self):
        nc = self.nc
        cnt = {e: 0 for e in ENGS}
        for o in self.ops:
            if not o.dma and o.sig:
                cnt[o.eng] += 1
                o.sem = ("eng", o.eng)
                o.val = cnt[o.eng]
        semkeys = [("eng", e) for e in ENGS]
        for e in ENGS:
            for s in range(min(DMA_SLOTS, self.dma_cnt[e])):
                semkeys.append(("dma", e, s))
        import contextlib
        with contextlib.ExitStack() as st:
            sems = {k: st.enter_context(nc.semaphore("s_" + "_".join(str(x) for x in k))) for k in semkeys}
            block = st.enter_context(nc.Block())
            per = {e: [o for o in self.ops if o.eng == e] for e in ENGS}
            final = {}
            for (eng, slot), o in self.dma_last.items():
                final.setdefault(eng, []).append((sems[o.sem], o.val))

            def run(e, engobj):
                seen = {}
                for o in per[e]:
                    for d in o.deps:
                        if seen.get(d.sem, 0) < d.val:
                            engobj.wait_ge(sems[d.sem], d.val)
                            seen[d.sem] = d.val
                    ins = o.fn(engobj)
                    if o.sig:
                        ins.then_inc(sems[o.sem], 16 if o.dma else 1)
                for s, v in final.get(e, []):
                    engobj.wait_ge(s, v)

            @block.tensor
            def _(eng):
                run("tensor", eng)

            @block.vector
            def _(eng):
                run("vector", eng)

            @block.scalar
            def _(eng):
                run("scalar", eng)

            @block.gpsimd
            def _(eng):
                run("gpsimd", eng)

            @block.sync
            def _(eng):
                run("sync", eng)


class V:
    __slots__ = ("ap", "res")

    def __init__(self, ap, res):
        self.ap = ap
        self.res = res

    def __getitem__(self, idx):
        return V(self.ap[idx], self.res)

    def rr(self, s, **kw):
        return V(self.ap.rearrange(s, **kw), self.res)

    def bc(self, shape):
        return V(self.ap.broadcast_to(shape), self.res)

    def un(self, axis):
        return V(self.ap.unsqueeze(axis), self.res)


class Tile:
    def __init__(self, handle, name):
        self.h = handle
        self.name = name
        self.res = Res(name)
        self.subs = {}

    def __getitem__(self, idx):
        return V(self.h[idx], self.res)

    def sub(self, key, idx=None):
        r = self.subs.get(key)
        if r is None:
            r = self.subs[key] = Res("%s/%s" % (self.name, key))
        return V(self.h[idx] if idx is not None else self.h[:], r)


def _aps(x):
    return x.ap if isinstance(x, V) else x


class KB:
    def __init__(self, nc, st):
        self.nc = nc
        self.st = st
        self.S = Sched(nc)
        self.n = 0

    def sb(self, name, shape, dt):
        return Tile(self.st.enter_context(self.nc.sbuf_tensor(name, list(shape), dt)), name)

    def ps(self, name, shape, dt):
        return Tile(self.st.enter_context(self.nc.psum_tensor(name, list(shape), dt)), name)

    def dram(self, name, shape, dt, kind):
        ap = self.nc.dram_tensor(name, list(shape), dt, kind=kind).ap()
        return V(ap, Res(name))

    def _op(self, eng, fn, reads, writes, dma=False):
        rs = [r.res if isinstance(r, V) else r for r in reads if isinstance(r, (V, Res))]
        ws = [w.res if isinstance(w, V) else w for w in writes if isinstance(w, (V, Res))]
        return self.S.op(eng, fn, rs, ws, dma)

    def dma(self, out, in_, eng="sync", xr=(), xw=(), **kw):
        return self._op(eng, lambda e: e.dma_start(out=out.ap, in_=in_.ap, **kw), [in_] + list(xr), [out] + list(xw),
                        dma=True)

    def gather(self, out, table, idx, **kw):
        return self._op("gpsimd", lambda e: e.indirect_dma_start(
            out=out.ap, out_offset=None, in_=table.ap,
            in_offset=bass.IndirectOffsetOnAxis(ap=idx.ap, axis=0), **kw), [table, idx], [out], dma=True)

    def scatter(self, table, idx, in_, **kw):
        return self._op("gpsimd", lambda e: e.indirect_dma_start(
            out=table.ap, out_offset=bass.IndirectOffsetOnAxis(ap=idx.ap, axis=0),
            in_=in_.ap, in_offset=None, **kw), [in_, idx], [table], dma=True)

    def mm(self, out, lhsT, rhs, start=True, stop=True):
        return self._op("tensor", lambda e: e.matmul(out.ap, lhsT=lhsT.ap, rhs=rhs.ap, start=start, stop=stop),
                        [lhsT, rhs], [out])

    def tr(self, out, in_, ident):
        return self._op("tensor", lambda e: e.transpose(out.ap, in_.ap, ident.ap), [in_, ident], [out])

    def act(self, out, in_, func, bias=0.0, scale=1.0, accum=None, eng="scalar"):
        kw = {}
        if accum is not None:
            kw["accum_out"] = accum.ap
        ws = [out] + ([accum] if accum is not None else [])
        return self._op(eng, lambda e: e.activation(out=out.ap, in_=in_.ap, func=func, bias=_aps(bias),
                                                    scale=_aps(scale), **kw), [in_, bias, scale], ws)

    def tt(self, out, in0, in1, op, eng="vector"):
        return self._op(eng, lambda e: e.tensor_tensor(out=out.ap, in0=in0.ap, in1=in1.ap, op=op), [in0, in1], [out])

    def ts(self, out, in0, s1, s2, op0, op1=None, accum=None, eng="vector"):
        kw = {}
        if op1 is not None:
            kw["op1"] = op1
        if accum is not None:
            kw["accum_out"] = accum.ap
        ws = [out] + ([accum] if accum is not None else [])
        return self._op(eng, lambda e: e.tensor_scalar(out=out.ap, in0=in0.ap, scalar1=_aps(s1), scalar2=_aps(s2),
                                                       op0=op0, **kw), [in0, s1, s2], ws)

    def stt(self, out, in0, scalar, in1, op0, op1, eng="vector"):
        return self._op(eng, lambda e: e.scalar_tensor_tensor(out=out.ap, in0=in0.ap, scalar=_aps(scalar), in1=in1.ap,
                                                              op0=op0, op1=op1), [in0, scalar, in1], [out])

    def copy(self, out, in_, eng="vector"):
        if eng == "scalar":
            return self._op(eng, lambda e: e.copy(out=out.ap, in_=in_.ap), [in_], [out])
        return self._op(eng, lambda e: e.tensor_copy(out=out.ap, in_=in_.ap), [in_], [out])

    def memset(self, out, val, eng="vector"):
        return self._op(eng, lambda e: e.memset(out.ap, val), [], [out])

    def recip(self, out, in_):
        return self._op("vector", lambda e: e.reciprocal(out=out.ap, in_=in_.ap), [in_], [out])

    def reduce(self, out, in_, op, axis=AX.X):
        return self._op("vector", lambda e: e.tensor_reduce(out=out.ap, in_=in_.ap, axis=axis, op=op), [in_], [out])

    def max8(self, out, in_):
        return self._op("vector", lambda e: e.max(out=out.ap, in_=in_.ap), [in_], [out])

    def match_replace(self, out, rep, vals, imm):
        return self._op("vector", lambda e: e.match_replace(out=out.ap, in_to_replace=rep.ap, in_values=vals.ap,
                                                            imm_value=imm), [rep, vals], [out])

    def rstd(self, out, ss, n, tmp):
        self.ts(tmp, ss, 1.0 / n, 1e-6, ALU.mult, ALU.add)
        self.act(tmp, tmp, AF.Sqrt)
        self.recip(out, tmp)


import contextlib

D = 4096
NT = 1024
EPS = 1e-6
BIG = 30000.0


def col_view(v, p=128):
    return v.rr("(c p) -> p c", p=p)


def build_c():
    nc = bass.Bass("TRN2", target_bir_lowering=False)
    with contextlib.ExitStack() as st:
        K = KB(nc, st)
        o_d = K.dram("o", [NT, D], F32, "ExternalInput")
        x_d = K.dram("x", [NT, D], F32, "ExternalInput")
        gout_d = K.dram("gout", [D], F32, "ExternalInput")
        wout_d = K.dram("wout", [D, D], F32, "ExternalInput")
        gffn_d = K.dram("gffn", [D], F32, "ExternalInput")
        rw_d = K.dram("rw", [D, 72], F32, "ExternalInput")
        rb_d = K.dram("rb", [72], F32, "ExternalInput")
        ident_d = K.dram("ident", [128, 128], F32, "ExternalInput")
        xmid_d = K.dram("xmid", [NT, D], F32, "ExternalOutput")
        xn2_d = K.dram("xn2", [NT, D], BF16, "ExternalOutput")
        m_d = K.dram("mroute", [NT, 64], F32, "ExternalOutput")
        gw_d = K.dram("gwroute", [NT, 64], F32, "ExternalOutput")
        rt_d = K.dram("route", [NT, 4], F32, "ExternalOutput")
        iota_d = K.dram("iota64", [128, 64], F32, "ExternalInput")
        xmid_res = [Res("xmid%d" % n) for n in range(NT // 128)]

        identf = K.sb("identf", [128, 128], F32)
        identb = K.sb("identb", [128, 128], BF16)
        gcol = K.sb("gcol", [128, 32], F32)
        g2col = K.sb("g2col", [128, 32], F32)
        wr = K.sb("wr", [128, 32, 72], F32)
        rbb = K.sb("rbb", [128, 72], F32)
        iota = K.sb("iota", [128, 64], F32)
        rto = K.sb("rto", [128, 4], F32)
        rtmp = K.sb("rtmp", [128, 64], F32)
        K.dma(iota[:], iota_d)
        K.dma(identf[:], ident_d)
        K.copy(identb[:], identf[:])
        K.dma(gcol[:], col_view(gout_d), allow_slow_non_contiguous=True)
        K.dma(g2col[:], col_view(gffn_d), allow_slow_non_contiguous=True)
        K.dma(wr[:], rw_d.rr("(c p) n -> p c n", p=128))
        K.dma(rbb[:], V(rb_d.ap.partition_broadcast(128), rb_d.res))
        K.tt(wr[:], wr[:], g2col[:].un(2).bc([128, 32, 72]), ALU.mult)

        mixedT = K.sb("mixedT", [128, 32, 512], BF16)
        wbf = [K.sb("wbf%d" % i, [128, 32, 512], BF16) for i in range(2)]
        ot = K.sb("ot", [128, D], F32)
        onb = K.sb("onb", [128, D], BF16)
        junk = K.sb("junk", [128, 1024], BF16)
        xnb = K.sb("xnb", [128, D], BF16)
        ss = K.sb("ss", [128, 4], F32)
        tmp4 = K.sb("tmp4", [128, 4], F32)
        rs4 = K.sb("rs4", [128, 4], F32)
        xt = [K.sb("xt%d" % i, [128, 4, 512], F32) for i in range(2)]
        ysb = [K.sb("ysb%d" % i, [128, 512], F32) for i in range(2)]
        xmT = K.sb("xmT", [128, 32, 128], F32)
        lg = K.sb("lg", [128, 72], F32)
        sm = K.sb("sm", [128, 16], F32)
        oh = K.sb("oh", [128, 8], F32)
        msk = K.sb("msk", [128, 64], F32)
        m8 = K.sb("m8", [128, 8], F32)
        mo = K.sb("mo", [128, 64], F32)
        gwo = K.sb("gwo", [128, 64], F32)
        tpb = [K.ps("tpb%d" % i, [128, 8, 128], BF16) for i in range(2)]
        yps = [K.ps("yps%d" % i, [128, 512], F32) for i in range(2)]
        tpf = [K.ps("tpf%d" % i, [128, 4, 128], F32) for i in range(2)]
        lgp = K.ps("lgp", [128, 72], F32)

        wv = wout_d.rr("(c p) n -> p c n", p=128)
        wcount = 0
        for hf in range(NT // 512):
            for n in range(4):
                r0 = hf * 512 + n * 128
                K.dma(ot[:], o_d[r0:r0 + 128, :])
                for gi in range(4):
                    K.act(junk[:], ot[:, gi * 1024:(gi + 1) * 1024], AF.Square, accum=ss[:, gi:gi + 1])
                K.rstd(rs4[:], ss[:], 1024.0, tmp4[:])
                for gi in range(4):
                    K.ts(onb.sub(gi, (slice(None), slice(gi * 1024, (gi + 1) * 1024))),
                         ot[:, gi * 1024:(gi + 1) * 1024], rs4[:, gi:gi + 1], None, ALU.mult)
                for k in range(4):
                    tp = tpb[k % 2]
                    for c in range(8):
                        cc = k * 8 + c
                        K.tr(tp[:, c, :], onb.sub(cc // 8, (slice(None), slice(cc * 128, (cc + 1) * 128))), identb[:])
                    K.tt(mixedT.sub((n, k), (slice(None), slice(k * 8, k * 8 + 8), slice(n * 128, (n + 1) * 128))),
                         tp[:], gcol[:, k * 8:k * 8 + 8].un(2).bc([128, 8, 128]), ALU.mult)
            for j in range(8):
                wb = wbf[wcount % 2]
                wcount += 1
                K.dma(wb[:], wv[:, :, j * 512:(j + 1) * 512], eng="gpsimd")
                xb = xt[j % 2]
                K.dma(xb[:], x_d[hf * 512:(hf + 1) * 512, j * 512:(j + 1) * 512].rr("(n p) c -> p n c", p=128))
                for n in range(4):
                    yp = yps[n % 2]
                    for c in range(32):
                        K.mm(yp[:], mixedT.sub((n, c // 8), (slice(None), c, slice(n * 128, (n + 1) * 128))),
                             wb[:, c, :], start=(c == 0), stop=(c == 31))
                    yb = ysb[n % 2]
                    K.tt(yb[:], yp[:], xb[:, n, :], ALU.add)
                    r0 = hf * 512 + n * 128
                    K.dma(V(xmid_d.ap[r0:r0 + 128, j * 512:(j + 1) * 512], xmid_res[hf * 4 + n]), yb[:])
            for n in range(4):
                r0 = hf * 512 + n * 128
                K.dma(ot[:], V(xmid_d.ap[r0:r0 + 128, :], xmid_res[hf * 4 + n]))
                K.act(xnb[:], ot[:], AF.Square, accum=ss[:, 0:1])
                K.rstd(rs4[:, 0:1], ss[:, 0:1], float(D), tmp4[:, 0:1])
                K.ts(xnb[:], ot[:], rs4[:, 0:1], None, ALU.mult)
                K.dma(xn2_d[r0:r0 + 128, :], xnb[:])
                for k in range(8):
                    tp = tpf[k % 2]
                    for c in range(4):
                        cc = k * 4 + c
                        K.tr(tp[:, c, :], ot[:, cc * 128:(cc + 1) * 128], identf[:])
                    K.copy(xmT.sub(k, (slice(None), slice(k * 4, k * 4 + 4), slice(None))), tp[:],
                           eng=("scalar" if k % 2 else "vector"))
                for c in range(32):
                    K.mm(lgp[:], xmT.sub(c // 4, (slice(None), c, slice(None))), wr[:, c, :],
                         start=(c == 0), stop=(c == 31))
                K.stt(lg[:], lgp[:], rs4[:, 0:1], rbb[:], ALU.mult, ALU.add)
                router_math(K, lg, sm, oh, msk, m8, mo, gwo, iota, rto, rtmp)
                K.dma(rt_d[r0:r0 + 128, :], rto[:])
                K.dma(m_d[r0:r0 + 128, :], mo[:])
                K.dma(gw_d[r0:r0 + 128, :], gwo[:])
        K.S.emit()
    return nc


def router_math(K, lg, sm, oh, msk, m8, mo, gwo, iota, rto, rtmp):
    K.reduce(sm[:, 0:1], lg[:, 0:8], ALU.max)
    K.ts(oh[:], lg[:, 0:8], sm[:, 0:1], None, ALU.is_ge)
    K.ts(sm[:, 1:2], sm[:, 0:1], -1.0, None, ALU.mult)
    K.act(msk[:, 0:8], lg[:, 0:8], AF.Exp, bias=sm[:, 1:2], accum=sm[:, 2:3])
    K.recip(sm[:, 3:4], sm[:, 2:3])
    K.ts(oh[:], oh[:], BIG, -BIG, ALU.mult, ALU.add)
    K.tt(msk[:].rr("p (g e) -> p g e", g=8), lg[:, 8:72].rr("p (g e) -> p g e", g=8),
         oh[:].un(2).bc([128, 8, 8]), ALU.add)
    K.max8(m8[:], msk[:])
    K.ts(mo[:], msk[:], m8[:, 1:2], None, ALU.is_ge)
    K.tt(sm[:, 4:5], m8[:, 0:1], m8[:, 1:2], ALU.add)
    K.ts(sm[:, 4:5], sm[:, 4:5], -1.0, None, ALU.mult)
    K.act(gwo[:], msk[:], AF.Sigmoid, bias=sm[:, 4:5], scale=2.0)
    K.stt(gwo[:], gwo[:], sm[:, 3:4], mo[:], ALU.mult, ALU.mult)
    K.stt(rtmp[:], iota[:], 1.0, mo[:], ALU.add, ALU.mult)
    K.reduce(rto[:, 1:2], rtmp[:], ALU.max)
    K.ts(rto[:, 1:2], rto[:, 1:2], -1.0, None, ALU.add)
    K.ts(rtmp[:], iota[:], -1.0, 64.0, ALU.mult, ALU.add)
    K.tt(rtmp[:], rtmp[:], mo[:], ALU.mult)
    K.reduce(rto[:, 0:1], rtmp[:], ALU.max)
    K.ts(rto[:, 0:1], rto[:, 0:1], -1.0, 64.0, ALU.mult, ALU.add)
    for k in range(2):
        K.ts(rtmp[:], iota[:], rto[:, k:k + 1], None, ALU.is_equal)
        K.tt(rtmp[:], rtmp[:], gwo[:], ALU.mult)
        K.reduce(rto[:, 2 + k:3 + k], rtmp[:], ALU.add)


CE = 384
NTOK = 8192
NTT = NTOK // 128
OOB = 1.0e6


def build_d1():
    nc = bass.Bass("TRN2", target_bir_lowering=False)
    with contextlib.ExitStack() as st:
        K = KB(nc, st)
        m_d = K.dram("mall", [NTOK, 64], F32, "ExternalInput")
        rt_d = K.dram("rtall", [NTOK, 4], F32, "ExternalInput")
        gb_d = K.dram("gbase", [1], F32, "ExternalInput")
        tri_d = K.dram("tri", [128, 128], F32, "ExternalInput")
        tok_d = K.dram("tokid", [128, NTT], F32, "ExternalInput")
        iota_d = K.dram("iota64", [128, 64], F32, "ExternalInput")
        idx_d = K.dram("idxlist", [8 * CE, 2], I32, "ExternalOutput")
        dg_d = K.dram("destg", [NTOK, 2], I32, "ExternalOutput")

        Mt = K.sb("Mt", [128, NTT, 64], F32)
        Mb = K.sb("Mb", [128, NTT, 64], BF16)
        Rf = K.sb("Rf", [128, NTT, 64], F32)
        Rb = K.sb("Rb", [128, NTT, 64], BF16)
        slot = K.sb("slot", [128, NTT, 64], F32)
        oh = K.sb("ohd", [128, NTT, 64], F32)
        trif = K.sb("trif", [128, 128], F32)
        trib = K.sb("trib", [128, 128], BF16)
        oneb = K.sb("oneb", [128, 128], BF16)
        iota = K.sb("iota", [128, 64], F32)
        tokf = K.sb("tokf", [128, NTT], F32)
        toki = K.sb("toki", [128, NTT, 2], I32)
        rt = K.sb("rt", [128, NTT, 4], F32)
        gb = K.sb("gb", [128, 1], F32)
        sl = K.sb("sl", [128, NTT], F32)
        dgl = K.sb("dgl", [128, NTT], F32)
        dl = K.sb("dl", [128, NTT], F32)
        ok = K.sb("ok", [128, NTT], F32)
        ok2 = K.sb("ok2", [128, NTT], F32)
        di = [K.sb("di%d" % k, [128, NTT], I32) for k in range(2)]
        dgi = K.sb("dgi", [128, NTT, 2], I32)
        zt = K.sb("zt", [128, 8 * CE // 128, 2], I32)
        pss = [K.ps("pss%d" % i, [128, 8, 64], F32) for i in range(8)]

        K.dma(Mt[:], m_d.rr("(n p) e -> p n e", p=128))
        K.dma(rt[:], rt_d.rr("(n p) k -> p n k", p=128))
        K.dma(trif[:], tri_d)
        K.dma(tokf[:], tok_d)
        K.dma(iota[:], iota_d)
        K.dma(gb[:], V(gb_d.ap.partition_broadcast(128), gb_d.res))
        K.copy(trib[:], trif[:])
        K.memset(oneb[:], 1.0)
        K.copy(toki[:, :, 0], tokf[:])
        K.copy(toki[:, :, 1], tokf[:])
        K.memset(zt[:], 0)
        K.dma(idx_d.rr("(s p) o -> p s o", p=128), zt[:])
        K.copy(Mb[:], Mt[:])
        K.memset(Rf[:, 0, :], 0.0)
        for n in range(1, NTT):
            K.tt(Rf[:, n, :], Rf[:, n - 1, :], Mt[:, n - 1, :], ALU.add)
        K.copy(Rb[:], Rf[:])
        for n in range(NTT):
            p = pss[n // 8]
            K.mm(p[:, n % 8, :], trib[:], Mb[:, n, :], start=True, stop=False)
            K.mm(p[:, n % 8, :], oneb[:], Rb[:, n, :], start=False, stop=True)
        for b in range(8):
            K.copy(slot[:, b * 8:(b + 1) * 8, :], pss[b][:], eng=("scalar" if b % 2 else "vector"))
        iota3 = iota[:].un(1).bc([128, NTT, 64])
        for k in range(2):
            K.tt(oh[:], iota3, rt[:, :, k].un(2).bc([128, NTT, 64]), ALU.is_equal)
            K.tt(oh[:], oh[:], slot[:], ALU.mult)
            K.reduce(sl[:], oh[:], ALU.add)
            K.stt(dgl[:], rt[:, :, k], float(CE), sl[:], ALU.mult, ALU.add)
            K.copy(dgi[:, :, k], dgl[:])
            K.ts(ok[:], sl[:], float(CE), None, ALU.is_lt)
            K.ts(dl[:], dgl[:], gb[:, 0:1], None, ALU.subtract)
            K.ts(ok2[:], dl[:], 0.0, None, ALU.is_ge)
            K.tt(ok[:], ok[:], ok2[:], ALU.mult)
            K.ts(ok2[:], dl[:], float(8 * CE), None, ALU.is_lt)
            K.tt(ok[:], ok[:], ok2[:], ALU.mult)
            K.ts(dl[:], dl[:], -OOB, None, ALU.add)
            K.tt(dl[:], dl[:], ok[:], ALU.mult)
            K.ts(dl[:], dl[:], OOB, None, ALU.add)
            K.copy(di[k][:], dl[:])
        K.dma(dg_d.rr("(n p) k -> p n k", p=128), dgi[:])
        for n in range(NTT):
            for k in range(2):
                K.scatter(idx_d, di[k][:, n:n + 1], toki[:, n, :], bounds_check=8 * CE - 1, oob_is_err=False)
        K.S.emit()
    return nc


NPROJ = 8288


def ot_pre(K):
    if not hasattr(K, "_otp"):
        K._otp = K.sb("otp", [128, D], F32)
    return K._otp


def norm_transpose(K, src_d, r0, width, gcols, ot, onb, ss, rs, tmp, tpb, identb, dstT, n, junk):
    nch = width // 128
    K.dma(ot[:, 0:width], src_d[r0:r0 + 128, :])
    K.act(junk[:, 0:width], ot[:, 0:width], AF.Square, accum=ss[:, 0:1])
    K.rstd(rs[:, 0:1], ss[:, 0:1], float(width), tmp[:, 0:1])
    K.ts(onb[:, 0:width], ot[:, 0:width], rs[:, 0:1], None, ALU.mult)
    for k in range((nch + 7) // 8):
        tp = tpb[k % 2]
        m = min(8, nch - k * 8)
        for c in range(m):
            cc = k * 8 + c
            K.tr(tp[:, c, :], onb[:, cc * 128:(cc + 1) * 128], identb[:])
        K.tt(dstT.sub((n, k), (slice(None), slice(k * 8, k * 8 + m), slice(n * 128, (n + 1) * 128))),
             tp[:, 0:m, :], gcols[:, k * 8:k * 8 + m].un(2).bc([128, m, 128]), ALU.mult)


def build_a():
    nc = bass.Bass("TRN2", target_bir_lowering=False)
    with contextlib.ExitStack() as st:
        K = KB(nc, st)
        x_d = K.dram("x", [NT, D], F32, "ExternalInput")
        xa_d = K.dram("xadd", [NT, D], F32, "ExternalInput")
        g_d = K.dram("g", [D], F32, "ExternalInput")
        w_d = K.dram("w", [D, NPROJ], F32, "ExternalInput")
        ident_d = K.dram("ident", [128, 128], F32, "ExternalInput")
        p_d = K.dram("proj", [NT, NPROJ], F32, "ExternalOutput")
        xs_d = K.dram("xsum", [NT, D], F32, "ExternalOutput")
        gq_d = K.dram("gq", [768], F32, "ExternalInput")
        gkv_d = K.dram("gkv", [512], F32, "ExternalInput")
        wuq_d = K.dram("wuq", [768, 1536], F32, "ExternalInput")
        wukv_d = K.dram("wukv", [512, 2048], F32, "ExternalInput")
        cos_d = K.dram("cos", [NT, 32], F32, "ExternalInput")
        sin_d = K.dram("sin", [NT, 32], F32, "ExternalInput")
        q_d = K.dram("q", [NT, 1536], F32, "ExternalOutput")
        kv_d = K.dram("kv", [NT, 2048], F32, "ExternalOutput")
        kro_d = K.dram("kro", [NT, 64], F32, "ExternalOutput")
        xs_res = [Res("xs%d" % n) for n in range(NT // 128)]
        xa = K.sb("xa", [128, D], F32)
        for n in range(NT // 128):
            K.dma(ot_pre(K)[:], x_d[n * 128:(n + 1) * 128, :])
            K.dma(xa[:], xa_d[n * 128:(n + 1) * 128, :])
            K.tt(xa[:], xa[:], ot_pre(K)[:], ALU.add)
            K.dma(V(xs_d.ap[n * 128:(n + 1) * 128, :], xs_res[n]), xa[:])
        identf = K.sb("identf", [128, 128], F32)
        identb = K.sb("identb", [128, 128], BF16)
        gcol = K.sb("gcol", [128, 32], F32)
        K.dma(identf[:], ident_d)
        K.copy(identb[:], identf[:])
        K.dma(gcol[:], col_view(g_d), allow_slow_non_contiguous=True)
        xT = K.sb("xT", [128, 32, 512], BF16)
        wbf = [K.sb("wbf%d" % i, [128, 32, 512], BF16) for i in range(2)]
        ot = K.sb("ot", [128, D], F32)
        onb = K.sb("onb", [128, D], BF16)
        junk = K.sb("junk", [128, D], BF16)
        ss = K.sb("ss", [128, 1], F32)
        tmp = K.sb("tmp", [128, 1], F32)
        rs = K.sb("rs", [128, 1], F32)
        ysb = [K.sb("ysb%d" % i, [128, 512], F32) for i in range(4)]
        tpb = [K.ps("tpb%d" % i, [128, 8, 128], BF16) for i in range(2)]
        yps = [K.ps("yps%d" % i, [128, 512], F32) for i in range(4)]
        wv = w_d.rr("(c p) n -> p c n", p=128)
        chunks = [(j * 512, 512) for j in range(16)] + [(8192, 96)]
        wcount = 0
        ycount = 0
        for hf in range(NT // 512):
            for n in range(4):
                norm_transpose(K, V(xs_d.ap, xs_res[hf * 4 + n]), hf * 512 + n * 128, D, gcol, ot, onb, ss, rs, tmp, tpb,
                               identb, xT, n, junk)
            for (c0, cw) in chunks:
                wb = wbf[wcount % 2]
                wcount += 1
                K.dma(wb[:, :, 0:cw], wv[:, :, c0:c0 + cw], eng="gpsimd")
                for n in range(4):
                    yp = yps[ycount % 4]
                    yb = ysb[ycount % 4]
                    for c in range(32):
                        K.mm(yp[:, 0:cw], xT.sub((n, c // 8), (slice(None), c, slice(n * 128, (n + 1) * 128))),
                             wb[:, c, 0:cw], start=(c == 0), stop=(c == 31))
                    K.copy(yb[:, 0:cw], yp[:, 0:cw], eng=("scalar" if ycount % 2 else "vector"))
                    ycount += 1
                    r0 = hf * 512 + n * 128
                    K.dma(p_d[r0:r0 + 128, c0:c0 + cw], yb[:, 0:cw])
        gqc = K.sb("gqc", [128, 6], F32)
        gkvc = K.sb("gkvc", [128, 4], F32)
        K.dma(gqc[:], col_view(gq_d), allow_slow_non_contiguous=True)
        K.dma(gkvc[:], col_view(gkv_d), allow_slow_non_contiguous=True)
        wuq = V(wbf[0].h[:].rearrange("p c n -> p (c n)")[:, 0:6 * 1536].rearrange("p (c n) -> p c n", c=6), wbf[0].res)
        wukv = V(wbf[1].h[:].rearrange("p c n -> p (c n)")[:, 0:4 * 2048].rearrange("p (c n) -> p c n", c=4), wbf[1].res)
        K.dma(wuq, wuq_d.rr("(c p) n -> p c n", p=128), eng="gpsimd")
        K.dma(wukv, wukv_d.rr("(c p) n -> p c n", p=128), eng="gpsimd")
        cqT = K.sb("cqT", [128, 6, 128], BF16)
        ckvT = K.sb("ckvT", [128, 4, 128], BF16)
        qsb = xa[:, 0:1536]
        kvsb = xa[:, 1536:3584]
        krs = K.sb("krs", [128, 64], F32)
        cs = K.sb("cs", [128, 32], F32)
        sn = K.sb("sn", [128, 32], F32)
        t1 = K.sb("t1", [128, 8, 32], F32)
        t2 = K.sb("t2", [128, 8, 32], F32)
        t3 = K.sb("t3", [128, 8, 32], F32)
        t4 = K.sb("t4", [128, 8, 32], F32)

        def rope(buf3, nh):
            x1 = buf3[:, :, 0:32]
            x2 = buf3[:, :, 32:64]
            cb = cs[:].un(1).bc([128, nh, 32])
            sb_ = sn[:].un(1).bc([128, nh, 32])
            K.tt(t1[:, 0:nh, :], x1, cb, ALU.mult)
            K.tt(t2[:, 0:nh, :], x2, sb_, ALU.mult)
            K.tt(t3[:, 0:nh, :], x2, cb, ALU.mult)
            K.tt(t4[:, 0:nh, :], x1, sb_, ALU.mult)
            K.tt(x1, t1[:, 0:nh, :], t2[:, 0:nh, :], ALU.subtract)
            K.tt(x2, t3[:, 0:nh, :], t4[:, 0:nh, :], ALU.add)
        yc = 0
        for n in range(NT // 128):
            r0 = n * 128
            norm_transpose(K, p_d[:, 5664:6432], r0, 768, gqc, ot, onb, ss, rs, tmp, tpb, identb, cqT, 0, junk)
            norm_transpose(K, p_d[:, 6432:6944], r0, 512, gkvc, ot, onb, ss, rs, tmp, tpb, identb, ckvT, 0, junk)
            K.dma(krs[:], p_d[r0:r0 + 128, 6944:7008])
            K.dma(cs[:], cos_d[r0:r0 + 128, :])
            K.dma(sn[:], sin_d[r0:r0 + 128, :])
            for j in range(3):
                yp = yps[yc % 4]
                yc += 1
                for c in range(6):
                    K.mm(yp[:], cqT.sub((0, 0), (slice(None), c, slice(None))), wuq[:, c, j * 512:(j + 1) * 512],
                         start=(c == 0), stop=(c == 5))
                K.copy(qsb[:, j * 512:(j + 1) * 512], yp[:], eng=("scalar" if j % 2 else "vector"))
            for j in range(4):
                yp = yps[yc % 4]
                yc += 1
                for c in range(4):
                    K.mm(yp[:], ckvT.sub((0, 0), (slice(None), c, slice(None))), wukv[:, c, j * 512:(j + 1) * 512],
                         start=(c == 0), stop=(c == 3))
                K.copy(kvsb[:, j * 512:(j + 1) * 512], yp[:], eng=("scalar" if j % 2 else "vector"))
            rope(qsb.rr("p (h d) -> p h d", d=192)[:, :, 128:192], 8)
            rope(krs[:].rr("p (h d) -> p h d", d=64), 1)
            K.dma(q_d[r0:r0 + 128, :], qsb)
            K.dma(kv_d[r0:r0 + 128, :], kvsb)
            K.dma(kro_d[r0:r0 + 128, :], krs[:])
        K.S.emit()
    return nc


def build_a2():
    nc = bass.Bass("TRN2", target_bir_lowering=False)
    with contextlib.ExitStack() as st:
        K = KB(nc, st)
        cq_d = K.dram("cq", [NT, 768], F32, "ExternalInput")
        ckv_d = K.dram("ckv", [NT, 512], F32, "ExternalInput")
        kr_d = K.dram("kr", [NT, 64], F32, "ExternalInput")
        gq_d = K.dram("gq", [768], F32, "ExternalInput")
        gkv_d = K.dram("gkv", [512], F32, "ExternalInput")
        wuq_d = K.dram("wuq", [768, 1536], F32, "ExternalInput")
        wukv_d = K.dram("wukv", [512, 2048], F32, "ExternalInput")
        cos_d = K.dram("cos", [NT, 32], F32, "ExternalInput")
        sin_d = K.dram("sin", [NT, 32], F32, "ExternalInput")
        ident_d = K.dram("ident", [128, 128], F32, "ExternalInput")
        q_d = K.dram("q", [NT, 1536], F32, "ExternalOutput")
        kv_d = K.dram("kv", [NT, 2048], F32, "ExternalOutput")
        kro_d = K.dram("kro", [NT, 64], F32, "ExternalOutput")
        identf = K.sb("identf", [128, 128], F32)
        identb = K.sb("identb", [128, 128], BF16)
        gqc = K.sb("gqc", [128, 6], F32)
        gkvc = K.sb("gkvc", [128, 4], F32)
        wuq = K.sb("wuqs", [128, 6, 1536], BF16)
        wukv = K.sb("wukvs", [128, 4, 2048], BF16)
        K.dma(identf[:], ident_d)
        K.copy(identb[:], identf[:])
        K.dma(gqc[:], col_view(gq_d), allow_slow_non_contiguous=True)
        K.dma(gkvc[:], col_view(gkv_d), allow_slow_non_contiguous=True)
        K.dma(wuq[:], wuq_d.rr("(c p) n -> p c n", p=128), eng="gpsimd")
        K.dma(wukv[:], wukv_d.rr("(c p) n -> p c n", p=128), eng="gpsimd")
        cqT = K.sb("cqT", [128, 6, 128], BF16)
        ckvT = K.sb("ckvT", [128, 4, 128], BF16)
        ot = K.sb("ot", [128, 768], F32)
        onb = K.sb("onb", [128, 768], BF16)
        junk = K.sb("junk", [128, 768], BF16)
        ss = K.sb("ss", [128, 1], F32)
        tmp = K.sb("tmp", [128, 1], F32)
        rs = K.sb("rs", [128, 1], F32)
        qsb = K.sb("qsb", [128, 1536], F32)
        kvsb = K.sb("kvsb", [128, 2048], F32)
        krs = K.sb("krs", [128, 64], F32)
        cs = K.sb("cs", [128, 32], F32)
        sn = K.sb("sn", [128, 32], F32)
        t1 = K.sb("t1", [128, 8, 32], F32)
        t2 = K.sb("t2", [128, 8, 32], F32)
        t3 = K.sb("t3", [128, 8, 32], F32)
        t4 = K.sb("t4", [128, 8, 32], F32)
        tpb = [K.ps("tpb%d" % i, [128, 8, 128], BF16) for i in range(2)]
        yps = [K.ps("yps%d" % i, [128, 512], F32) for i in range(4)]

        def rope(buf3, nh):
            x1 = buf3[:, :, 0:32]
            x2 = buf3[:, :, 32:64]
            cb = cs[:].un(1).bc([128, nh, 32])
            sb_ = sn[:].un(1).bc([128, nh, 32])
            K.tt(t1[:, 0:nh, :], x1, cb, ALU.mult)
            K.tt(t2[:, 0:nh, :], x2, sb_, ALU.mult)
            K.tt(t3[:, 0:nh, :], x2, cb, ALU.mult)
            K.tt(t4[:, 0:nh, :], x1, sb_, ALU.mult)
            K.tt(x1, t1[:, 0:nh, :], t2[:, 0:nh, :], ALU.subtract)
            K.tt(x2, t3[:, 0:nh, :], t4[:, 0:nh, :], ALU.add)

        yc = 0
        for n in range(NT // 128):
            r0 = n * 128
            norm_transpose(K, cq_d, r0, 768, gqc, ot, onb, ss, rs, tmp, tpb, identb, cqT, 0, junk)
            norm_transpose(K, ckv_d, r0, 512, gkvc, ot, onb, ss, rs, tmp, tpb, identb, ckvT, 0, junk)
            K.dma(krs[:], kr_d[r0:r0 + 128, :])
            K.dma(cs[:], cos_d[r0:r0 + 128, :])
            K.dma(sn[:], sin_d[r0:r0 + 128, :])
            for j in range(3):
                yp = yps[yc % 4]
                yc += 1
                for c in range(6):
                    K.mm(yp[:], cqT.sub((0, 0), (slice(None), c, slice(None))), wuq[:, c, j * 512:(j + 1) * 512],
                         start=(c == 0), stop=(c == 5))
                K.copy(qsb[:, j * 512:(j + 1) * 512], yp[:], eng=("scalar" if j % 2 else "vector"))
            for j in range(4):
                yp = yps[yc % 4]
                yc += 1
                for c in range(4):
                    K.mm(yp[:], ckvT.sub((0, 0), (slice(None), c, slice(None))), wukv[:, c, j * 512:(j + 1) * 512],
                         start=(c == 0), stop=(c == 3))
                K.copy(kvsb[:, j * 512:(j + 1) * 512], yp[:], eng=("scalar" if j % 2 else "vector"))
            rope(qsb[:].rr("p (h d) -> p h d", d=192)[:, :, 128:192], 8)
            rope(krs[:].rr("p (h d) -> p h d", d=64), 1)
            K.dma(q_d[r0:r0 + 128, :], qsb[:])
            K.dma(kv_d[r0:r0 + 128, :], kvsb[:])
            K.dma(kro_d[r0:r0 + 128, :], krs[:])
        K.S.emit()
    return nc


S = 4096
NQT = S // 128


def nsa_tables():
    c = np.arange(256)
    c_end = 16 * c + 31
    t = np.arange(S)
    cm = (t[None, :] >= c_end[:, None]) & (c[:, None] < 255)
    cmpmask = cm.reshape(2, 128, NQT, 128).transpose(1, 0, 2, 3).astype(np.float32)
    j = np.arange(64)
    c_start = 16 * c
    ovl = ((c_start[:, None] <= j[None, :] * 64 + 63) & (c_end[:, None] >= j[None, :] * 64) & (c[:, None] < 255))
    ovl = ovl.reshape(2, 128, 64).transpose(1, 0, 2).astype(np.float32)
    cur = (t // 64)[:, None]
    forced = (j[None, :] == 0) | (j[None, :] == cur) | (j[None, :] == cur - 1)
    future = j[None, :] > cur
    keep = (~(forced | future)).astype(np.float32)
    fill = np.where(forced, 1e6 + j[None, :] * 16.0, 0.0) + np.where(future, -1e6 - j[None, :] * 16.0, 0.0)
    keep = keep.reshape(NQT, 128, 64).transpose(1, 0, 2)
    fill = fill.reshape(NQT, 128, 64).transpose(1, 0, 2).astype(np.float32)
    E = (np.arange(S)[None, :] // 64 == j[:, None]).astype(np.float32)
    return cmpmask, ovl, np.ascontiguousarray(keep), np.ascontiguousarray(fill), E


class ACtx:
    pass


def attn_head(K, C, qparts, kparts, vaug, vw, klist_fn, scale, bias_fn, extra_fn, epilogue):
    LOOK = 3
    jobs = []
    for qt in range(NQT):
        kl = klist_fn(qt)
        for i, (kt, mk) in enumerate(kl):
            jobs.append((qt, kt, mk, i == 0, i == len(kl) - 1))
    st = {}
    po_of = {}

    def front(j):
        qt, kt, mk, first, last = jobs[j]
        pss = C.ps_s[C.sc % 4]
        pt = C.pT[C.sc % 4]
        C.sc += 1
        ex = extra_fn(kt, qt) if extra_fn else None
        npart = len(qparts)
        for p in range(npart):
            K.mm(pss[:, 0:128], kparts[p][:, kt * 128:(kt + 1) * 128], qparts[p][:, qt * 128:(qt + 1) * 128],
                 start=(p == 0), stop=(p == npart - 1 and ex is None))
        if ex is not None:
            K.mm(pss[:, 0:128], ex[0], ex[1], start=False, stop=True)
        b = bias_fn(kt, qt) if bias_fn else 0.0
        K.act(pt[:], pss[:, 0:128], AF.Exp, bias=b, scale=scale)
        if mk is not None:
            K.tt(pt[:], pt[:], mk, ALU.mult)
        st[j] = pt

    def back(j):
        qt, kt, mk, first, last = jobs[j]
        if first:
            po_of[qt] = C.ps_o[C.oc % 2]
            C.oc += 1
        po = po_of[qt]
        K.mm(po[:, 0:vw], st.pop(j)[:], vaug[:, kt, 0:vw], start=first, stop=last)
        if last:
            epilogue(qt, po)
            del po_of[qt]
    n = len(jobs)
    for j in range(n + LOOK):
        if j < n:
            front(j)
        if j - LOOK >= 0:
            back(j - LOOK)


def build_b():
    cmpmask_np = nsa_tables()[0]
    nc = bass.Bass("TRN2", target_bir_lowering=False)
    with contextlib.ExitStack() as st:
        K = KB(nc, st)
        I = lambda n, s: K.dram(n, s, F32, "ExternalInput")
        swa_q, swa_k, swa_v = I("swa_qT", [4, 64, S]), I("swa_kT", [64, S]), I("swa_v", [S, 64])
        swa_alb, swa_sink = I("swa_alb", [128, 4, 2]), I("swa_sink", [128, 4])
        fox_q, fox_k, fox_v = I("fox_qT", [2, 128, S]), I("fox_kT", [2, 128, S]), I("fox_v", [2, S, 128])
        fox_fl, fox_fb = I("fox_fl", [2, S]), I("fox_fb", [2, 1])
        mla_qn, mla_qr, mla_kn = I("mla_qnT", [2, 128, S]), I("mla_qrT", [2, 64, S]), I("mla_knT", [2, 128, S])
        mla_kr, mla_v = I("mla_krT", [64, S]), I("mla_v", [2, S, 128])
        nsa_q = I("nsa_qT", [4, 128, S])
        nsa_kc, nsa_vc = I("nsa_kcT", [128, S]), I("nsa_vcT", [128, S])
        nsa_ks, nsa_vs = I("nsa_ksT", [128, S]), I("nsa_vs", [S, 128])
        nsa_kw, nsa_vw = I("nsa_kwT", [128, S]), I("nsa_vw", [S, 128])
        nsa_gl = I("nsa_gl", [S, 6])
        kc_pos, kc_w1, kc_w2 = I("kc_posT", [128, 32]), I("kc_w1", [S, 128]), I("kc_w2", [128, 128])
        vc_pos, vc_w1, vc_w2 = I("vc_posT", [128, 32]), I("vc_w1", [S, 128]), I("vc_w2", [128, 128])
        nsa_alb = I("nsa_alb", [128, 4, NQT])
        cmpb_d = I("cmpb", [128, 4, 2, NQT])
        cmpmask_d = I("cmpmask", [128, 2, NQT, 128])
        ovl_d, keep_d, fill_d, E_d = I("ovl", [128, 2, 64]), I("keep", [128, NQT, 64]), I("fill", [128, NQT, 64]), I("Emat", [64, S])
        mdiag_d, medge_d, ident_d = I("mdiag", [128, 128]), I("medge", [128, 128]), I("ident", [128, 128])
        out_d = K.dram("out", [S, 1024], F32, "ExternalOutput")
        cfox_d = K.dram("cfox", [2, S], F32, "ExternalOutput")

        def cload(name, src, shape, dt=BF16):
            t = K.sb(name, shape, dt)
            K.dma(t[:], src, eng=("gpsimd" if dt == BF16 else "sync"))
            return t
        mdiag = cload("mdiag_s", mdiag_d, [128, 128])
        medge = cload("medge_s", medge_d, [128, 128])
        identf = cload("identf", ident_d, [128, 128], F32)
        cmpmask = cload("cmpmask_s", cmpmask_d, [128, 2, NQT, 128])
        Emat = cload("Emat_s", E_d, [64, S])
        keep = cload("keep_s", keep_d, [128, NQT, 64], F32)
        fill = cload("fill_s", fill_d, [128, NQT, 64], F32)
        cmpb = cload("cmpb_s", cmpb_d, [128, 4, 2, NQT], F32)
        nalb = cload("nalb_s", nsa_alb, [128, 4, NQT], F32)
        salb = cload("salb_s", swa_alb, [128, 4, 2], F32)
        ssink = cload("ssink_s", swa_sink, [128, 4], F32)

        C = ACtx()
        C.oc = 0
        C.sc = 0
        C.ps_s = [K.ps("pss%d" % i, [128, 512], F32) for i in range(4)]
        C.ps_o = [K.ps("pso%d" % i, [128, 512], F32) for i in range(2)]
        C.pT = [K.sb("pT%d" % i, [128, 128], BF16) for i in range(4)]
        psx = [K.ps("psx%d" % i, [128, 512], F32) for i in range(2)]
        qb = [K.sb("qb%d" % i, [128, S], BF16) for i in range(2)]
        kb = [K.sb("kb%d" % i, [128, S], BF16) for i in range(1)] * 2
        q2b = [K.sb("q2b%d" % i, [64, S], BF16) for i in range(1)] * 2
        k2b = K.sb("k2b", [64, S], BF16)
        vb = [K.sb("vb%d" % i, [128, NQT, 129], BF16) for i in range(2)]
        ob = [K.sb("ob%d" % i, [128, NQT, 128], F32) for i in range(1)] * 2
        onsa = [K.sb("onsa%d" % i, [128, NQT, 128], F32) for i in range(2)]
        flt = [onsa[i][0:2].rr("p n d -> p (n d)") for i in range(2)]
        rinv = [K.sb("rinv%d" % i, [128, 1], F32) for i in range(4)]
        cnt = {"r": 0, "h": 0}

        def nr():
            cnt["r"] += 1
            return rinv[cnt["r"] % 4]

        def load_v(vbt, src, dv):
            K.dma(vbt[:, :, 0:dv], src.rr("(n p) d -> p n d", p=128), eng="gpsimd")
            K.memset(vbt[:, :, dv:dv + 1], 1.0)

        def causal(qt):
            return [(kt, (mdiag[:] if kt == qt else None)) for kt in range(qt + 1)]

        def band(nprev):
            def f(qt):
                l = []
                for kt in range(max(0, qt - nprev), qt + 1):
                    l.append((kt, mdiag[:] if kt == qt else (medge[:] if kt == qt - nprev else None)))
                return l
            return f

        def plain_epi(obuf, dv):
            def f(qt, po):
                r = nr()
                K.recip(r[:], po[:, dv:dv + 1])
                K.ts(obuf[:, qt, 0:dv], po[:, 0:dv], r[:, 0:1], None, ALU.mult)
            return f

        def store(obuf, col0, dv):
            K.dma(out_d[:, col0:col0 + dv].rr("(n p) d -> p n d", p=128), obuf[:, :, 0:dv])

        K.dma(k2b[:], swa_k, eng="gpsimd")
        load_v(vb[0], swa_v, 64)
        sinkc = K.sb("sinkc", [128, 4], F32)
        for h in range(4):
            K.act(sinkc[:, h:h + 1], salb[:, h, 0:1], AF.Exp, bias=ssink[:, h:h + 1])
        for h in range(4):
            qt_ = q2b[h % 2]
            K.dma(qt_[:], swa_q[h], eng="gpsimd")
            obuf = ob[cnt["h"] % 2]
            cnt["h"] += 1

            def epi(qt, po, h=h, obuf=obuf):
                r = nr()
                K.tt(r[:], po[:, 64:65], sinkc[:, h:h + 1], ALU.add)
                K.recip(r[:], r[:])
                K.ts(obuf[:, qt, 0:64], po[:, 0:64], r[:, 0:1], None, ALU.mult)
            attn_head(K, C, [qt_], [k2b], vb[0], 65, band(1), 0.125,
                      lambda kt, qt, h=h: salb[:, h, (qt - kt):(qt - kt) + 1], None, epi)
            store(obuf, 768 + h * 64, 64)

        fl, fl2 = flt
        fbn = K.sb("fbn", [2, 1], F32)
        K.dma(fl[:], fox_fl)
        K.dma(fbn[:], fox_fb)
        K.ts(fbn[:], fbn[:], -1.0, None, ALU.mult)
        K.act(fl[:], fl[:], AF.Exp, bias=fbn[:, 0:1], scale=-1.0)
        K.act(fl[:], fl[:], AF.Ln, bias=1.0)
        a, b_ = fl, fl2
        s = 1
        while s < S:
            K.copy(b_[:, 0:s], a[:, 0:s], eng="gpsimd")
            K.tt(b_[:, s:S], a[:, s:S], a[:, 0:S - s], ALU.add)
            a, b_ = b_, a
            s *= 2
        K.dma(cfox_d, a[:])
        cK = K.sb("cK", [128, 2, NQT], F32)
        cR = K.sb("cR", [128, 2, NQT], F32)
        for h in range(2):
            K.dma(cK[:, h, :], cfox_d[h].rr("(n p) -> p n", p=128), allow_slow_non_contiguous=True)
            K.dma(cR[:, h, :], V(cfox_d.ap[h, 127:S:128].partition_broadcast(128), cfox_d.res), allow_slow_non_contiguous=True)
        fbias = [K.sb("fbias%d" % i, [128, NQT], F32) for i in range(2)]
        for h in range(2):
            K.dma(qb[h % 2][:], fox_q[h], eng="gpsimd")
            K.dma(kb[h % 2][:], fox_k[h], eng="gpsimd")
            load_v(vb[(h + 1) % 2], fox_v[h], 128)
            obuf = ob[cnt["h"] % 2]
            cnt["h"] += 1
            fbs = {}

            def fb(kt, qt, h=h, fbs=fbs):
                if qt not in fbs:
                    t = fbias[qt % 2]
                    K.ts(t[:, 0:qt + 1], cK[:, h, 0:qt + 1], cR[:, h, qt:qt + 1], None, ALU.subtract)
                    fbs[qt] = t
                return fbs[qt][:, kt:kt + 1]
            attn_head(K, C, [qb[h % 2]], [kb[h % 2]], vb[(h + 1) % 2], 129, causal, 128 ** -0.5, fb, None,
                      plain_epi(obuf, 128))
            store(obuf, 256 + h * 128, 128)

        K.dma(k2b[:], mla_kr, eng="gpsimd")
        for h in range(2):
            K.dma(qb[h % 2][:], mla_qn[h], eng="gpsimd")
            K.dma(kb[h % 2][:], mla_kn[h], eng="gpsimd")
            K.dma(q2b[h % 2][:], mla_qr[h], eng="gpsimd")
            load_v(vb[(h + 1) % 2], mla_v[h], 128)
            obuf = ob[cnt["h"] % 2]
            cnt["h"] += 1
            attn_head(K, C, [qb[h % 2], q2b[h % 2]], [kb[h % 2], k2b], vb[(h + 1) % 2], 129, causal, 192 ** -0.5,
                      None, None, plain_epi(obuf, 128))
            store(obuf, 512 + h * 128, 128)

        sg = K.sb("sg", [128, NQT, 6], F32)
        K.dma(sg[:], nsa_gl.rr("(n p) c -> p n c", p=128))
        K.act(sg[:], sg[:], AF.Sigmoid)
        kcT = K.sb("kcT", [128, 256], BF16)
        vca = K.sb("vca", [128, 2, 193], BF16)
        w1b = K.sb("w1b", [128, 32, 128], BF16)
        w2b = K.sb("w2b", [128, 128], BF16)
        posT = K.sb("posT", [128, 32], F32)
        win = [K.sb("win%d" % i, [128, 255], BF16) for i in range(3)]
        hsb = K.sb("hsb", [128, 256], F32)
        h2 = K.sb("h2", [128, 256], F32)
        gT = K.sb("gT", [128, 256], BF16)
        K.memset(gT[:], 0.0)
        K.memset(kcT[:], 0.0)
        K.memset(vca[:], 0.0)
        K.dma(vca[:, :, 0:64], ovl_d, eng="gpsimd")
        K.memset(vca[:, 0, 64:65], 1.0)
        K.memset(vca[0:127, 1, 64:65], 1.0)
        for which, srcT, pos_d, w1_d, w2_d in (("k", nsa_kc, kc_pos, kc_w1, kc_w2), ("v", nsa_vc, vc_pos, vc_w1, vc_w2)):
            xT = qb[0]
            K.dma(xT[:], srcT, eng="gpsimd")
            K.dma(posT[:], pos_d)
            K.dma(w1b[:], w1_d.rr("(w d) n -> d w n", d=128), eng="gpsimd")
            K.dma(w2b[:], w2_d, eng="gpsimd")
            ph = psx[0]
            for w in range(32):
                wt = win[w % 3]
                K.ts(wt[:], xT[:, w:w + 16 * 254 + 1:16], posT[:, w:w + 1], None, ALU.add)
                K.mm(ph[:, 0:255], w1b[:, w, :], wt[:], start=(w == 0), stop=(w == 31))
            K.copy(hsb[:, 0:255], ph[:, 0:255])
            K.act(h2[:, 0:255], hsb[:, 0:255], AF.Square)
            K.ts(h2[:, 0:255], h2[:, 0:255], 0.044715, 1.0, ALU.mult, ALU.add)
            K.tt(h2[:, 0:255], h2[:, 0:255], hsb[:, 0:255], ALU.mult)
            K.act(h2[:, 0:255], h2[:, 0:255], AF.Tanh, scale=0.7978845608028654)
            K.ts(h2[:, 0:255], h2[:, 0:255], 1.0, None, ALU.add)
            K.stt(gT[:, 0:255], hsb[:, 0:255], 0.5, h2[:, 0:255], ALU.mult, ALU.mult)
            if which == "k":
                p2 = psx[1]
                K.mm(p2[:, 0:256], w2b[:], gT[:], start=True, stop=True)
                K.copy(kcT[:], p2[:, 0:256])
            else:
                for ct in range(2):
                    p2 = psx[1]
                    K.mm(p2[:, 0:128], gT[:, ct * 128:(ct + 1) * 128], w2b[:], start=True, stop=True)
                    K.copy(vca[:, ct, 65:193], p2[:, 0:128])
        imp = K.sb("imp", [128, NQT, 64], F32)

        def cmp_klist(qt):
            l = []
            for ct in range(2):
                m = cmpmask_np[:, ct, qt, :]
                if not m.any():
                    continue
                l.append((ct, None if m.all() else cmpmask[:, ct, qt, :]))
            return l
        for h in range(4):
            K.dma(qb[(h + 1) % 2][:], nsa_q[h], eng="gpsimd")

            def epi(qt, po, h=h):
                r = nr()
                K.ts(r[:], po[:, 64:65], 1e-30, None, ALU.max)
                K.recip(r[:], r[:])
                if h == 0:
                    K.ts(imp[:, qt, :], po[:, 0:64], r[:, 0:1], None, ALU.mult)
                else:
                    K.stt(imp[:, qt, :], po[:, 0:64], r[:, 0:1], imp[:, qt, :], ALU.mult, ALU.add)
                if h < 2:
                    K.tt(r[:], r[:], sg[:, qt, h:h + 1], ALU.mult)
                    K.ts(onsa[h][:, qt, :], po[:, 65:193], r[:, 0:1], None, ALU.mult)
            attn_head(K, C, [qb[(h + 1) % 2]], [kcT], vca, 193, cmp_klist, 128 ** -0.5,
                      lambda kt, qt, h=h: cmpb[:, h, kt, qt:qt + 1], None, epi)
        K.tt(imp[:], imp[:], keep[:], ALU.mult)
        K.tt(imp[:], imp[:], fill[:], ALU.add)
        selbT = K.sb("selbT", [64, S], BF16)
        m8 = [K.sb("m8_%d" % i, [128, 16], F32) for i in range(2)]
        wk = [K.sb("wk%d" % i, [128, 64], F32) for i in range(2)]
        sel = [K.sb("sel%d" % i, [128, 64], F32) for i in range(2)]
        for qt in range(NQT):
            m = m8[qt % 2]
            w_ = wk[qt % 2]
            sl = sel[qt % 2]
            K.max8(m[:, 0:8], imp[:, qt, :])
            K.match_replace(w_[:], m[:, 0:8], imp[:, qt, :], -3.0e6)
            K.max8(m[:, 8:16], w_[:])
            K.ts(sl[:], imp[:, qt, :], m[:, 15:16], None, ALU.is_ge)
            K.ts(sl[:], sl[:], BIG, -BIG, ALU.mult, ALU.add)
            pt_ = psx[qt % 2]
            K.tr(pt_[0:64, 0:128], sl[:], identf[:])
            K.copy(selbT[:, qt * 128:(qt + 1) * 128], pt_[0:64, 0:128], eng="scalar")
        for h in range(2):
            K.dma(qb[h % 2][:], nsa_q[h], eng="gpsimd")
            for br, kT_d, v_d, klf in ((1, nsa_ks, nsa_vs, causal), (2, nsa_kw, nsa_vw, band(4))):
                kbt = kb[br % 2]
                vbt = vb[br % 2]
                K.dma(kbt[:], kT_d, eng="gpsimd")
                load_v(vbt, v_d, 128)

                def epi(qt, po, h=h, br=br):
                    r = nr()
                    K.recip(r[:], po[:, 128:129])
                    K.tt(r[:], r[:], sg[:, qt, br * 2 + h:br * 2 + h + 1], ALU.mult)
                    K.stt(onsa[h][:, qt, :], po[:, 0:128], r[:, 0:1], onsa[h][:, qt, :], ALU.mult, ALU.add)
                ex = None
                if br == 1:
                    ex = lambda kt, qt: (Emat[:, kt * 128:(kt + 1) * 128], selbT[:, qt * 128:(qt + 1) * 128])
                attn_head(K, C, [qb[h % 2]], [kbt], vbt, 129, klf, 128 ** -0.5,
                          lambda kt, qt, h=h: nalb[:, h, (qt - kt):(qt - kt) + 1], ex, epi)
            store(onsa[h], h * 128, 128)
        K.S.emit()
    return nc


def _c(a):
    return np.ascontiguousarray(a, dtype=np.float32)


def b_const_inputs():
    cmpmask, ovl, keep, fill, E = nsa_tables()
    i = np.arange(128)
    return dict(cmpmask=_c(cmpmask), ovl=_c(ovl), keep=_c(keep), fill=_c(fill), Emat=_c(E),
                mdiag=_c(i[:, None] <= i[None, :]), medge=_c(i[:, None] > i[None, :]), ident=np.eye(128, dtype=np.float32))


def b_core_inputs(proj, q_mla, kv_mla, kro, P, l, hq):
    T = lambda a: _c(np.asarray(a).T)
    d = {}
    i = np.arange(128, dtype=np.float64)
    g = hq // 2
    heads = [4 * hq + r for r in range(4)]
    d["swa_qT"] = _c(np.stack([proj[:, 7008 + h * 64:7008 + (h + 1) * 64].T for h in heads]))
    d["swa_kT"] = T(proj[:, 8032 + g * 64:8032 + (g + 1) * 64])
    d["swa_v"] = _c(proj[:, 8032 + (2 + g) * 64:8032 + (3 + g) * 64])
    sl = np.array([2.0 ** (-(h + 1) / 2.0) for h in heads])
    d["swa_alb"] = _c(sl[None, :, None] * (i[:, None, None] - 63.5 - 128.0 * np.arange(2)[None, None, :]))
    d["swa_sink"] = _c(np.tile(P["swa_sinks"][l][heads][None, :], (128, 1)))
    fh = [2 * hq, 2 * hq + 1]
    bq = lambda w, h: proj[:, 2584 + (w * 8 + h) * 128:2584 + (w * 8 + h + 1) * 128]
    d["fox_qT"] = _c(np.stack([bq(0, h).T for h in fh]))
    d["fox_kT"] = _c(np.stack([bq(1, h).T for h in fh]))
    d["fox_v"] = _c(np.stack([bq(2, h) for h in fh]))
    d["fox_fl"] = _c(np.stack([proj[:, 5656 + h] for h in fh]))
    d["fox_fb"] = _c(P["fox_f_bias"][l][fh][:, None])
    q3 = q_mla.reshape(S, 8, 192)
    kv3 = kv_mla.reshape(S, 8, 256)
    d["mla_qnT"] = _c(np.stack([q3[:, h, 0:128].T for h in fh]))
    d["mla_qrT"] = _c(np.stack([q3[:, h, 128:192].T for h in fh]))
    d["mla_knT"] = _c(np.stack([kv3[:, h, 0:128].T for h in fh]))
    d["mla_krT"] = T(kro)
    d["mla_v"] = _c(np.stack([kv3[:, h, 128:256] for h in fh]))
    mine = [2 * hq, 2 * hq + 1]
    nh = mine + [h for h in range(4 * g, 4 * g + 4) if h not in mine]
    d["nsa_qT"] = _c(np.stack([proj[:, h * 128:(h + 1) * 128].T for h in nh]))
    akv = lambda br, kvi: proj[:, 1024 + ((br * 2 + kvi) * 2 + g) * 128:1024 + ((br * 2 + kvi) * 2 + g + 1) * 128]
    d["nsa_kcT"], d["nsa_vcT"] = T(akv(0, 0)), T(akv(0, 1))
    d["nsa_ksT"], d["nsa_vs"] = T(akv(1, 0)), _c(akv(1, 1))
    d["nsa_kwT"], d["nsa_vw"] = T(akv(2, 0)), _c(akv(2, 1))
    d["nsa_gl"] = _c(np.stack([proj[:, 2560 + br * 8 + h] for br in range(3) for h in mine], axis=1))
    d["kc_posT"], d["kc_w1"], d["kc_w2"] = T(P["nsa_kc_pos"][l]), _c(P["nsa_kc_w1"][l]), _c(P["nsa_kc_w2"][l])
    d["vc_posT"], d["vc_w1"], d["vc_w2"] = T(P["nsa_vc_pos"][l]), _c(P["nsa_vc_w1"][l]), _c(P["nsa_vc_w2"][l])
    nsl = np.array([2.0 ** (-(h + 1)) for h in nh])
    d["nsa_alb"] = _c(nsl[None, :, None] * (i[:, None, None] - 63.5 - 128.0 * np.arange(NQT)[None, None, :]))
    cend = 16.0 * (np.arange(2)[None, :, None] * 128 + i[:, None, None]) + 31.0
    tref = np.arange(NQT)[None, None, :] * 128 + 63.5
    cb = nsl[None, :, None, None] * (cend - tref)[:, None, :, :]
    d["cmpb"] = _c(np.minimum(cb, 40.0))
    return d


def build_d():
    nc = bass.Bass("TRN2", target_bir_lowering=False)
    with contextlib.ExitStack() as st:
        K = KB(nc, st)
        I = lambda n, s, dt=F32: K.dram(n, s, dt, "ExternalInput")
        mg_d, rt_d, g8_d = I("mg", [NTOK, 8]), I("rtall", [NTOK, 4]), I("g8", [1])
        xn2_d = I("xn2", [NTOK, D], BF16)
        gffn_d = I("gffn", [D])
        wg_d, wu_d, wd_d = I("wg", [8, D, 384]), I("wu", [8, D, 384]), I("wd", [8, 384, D])
        tri_d, io384_d, pn_d = I("tri", [128, 128]), I("iota384", [128, 384]), I("pn", [128, NTT, 2])
        ident_d, io8_d = I("ident", [128, 128]), I("iota8", [128, 8])
        y_d = K.dram("y", [8 * CE, D], F32, "ExternalOutput")
        dl_d = K.dram("destl", [NTOK, 2], F32, "ExternalOutput")

        identf = K.sb("identf", [128, 128], F32)
        identb = K.sb("identb", [128, 128], BF16)
        trif = K.sb("trif", [128, 128], F32)
        trib = K.sb("trib", [128, 128], BF16)
        oneb = K.sb("oneb", [128, 128], BF16)
        io384 = K.sb("io384", [128, 384], F32)
        io8 = K.sb("io8", [128, 8], F32)
        pnf = K.sb("pnf", [128, NTT, 2], F32)
        pnb = K.sb("pnb", [128, NTT, 2], BF16)
        g2col = K.sb("g2col", [128, 32], F32)
        g8 = K.sb("g8s", [128, 1], F32)
        Mt = K.sb("Mt", [128, NTT, 8], F32)
        Mb = K.sb("Mb", [128, NTT, 8], BF16)
        Rf = K.sb("Rf", [128, NTT, 8], F32)
        Rb = K.sb("Rb", [128, NTT, 8], BF16)
        slot = K.sb("slot", [128, NTT, 8], F32)
        oh = K.sb("ohd", [128, NTT, 8], F32)
        rt = K.sb("rt", [128, NTT, 4], F32)
        rel = K.sb("rel", [128, NTT], F32)
        sl = K.sb("sl", [128, NTT], F32)
        dls = K.sb("dls", [128, NTT, 2], F32)
        OHb = [K.sb("OHb%d" % i, [128, 384], BF16) for i in range(4)]
        idxf = K.sb("idxf", [128, 3, 8, 2], F32)
        idxv = K.sb("idxv", [128, 3, 8], F32)
        idxi = K.sb("idxi", [128, 3, 8], I32)
        B = [K.ps("B%d" % i, [128, 512], F32) for i in range(4)]
        tpb = [K.ps("tpb%d" % i, [128, 8, 128], BF16) for i in range(2)]
        psy = [K.ps("psy%d" % i, [128, 512], F32) for i in range(2)]

        K.dma(identf[:], ident_d)
        K.copy(identb[:], identf[:])
        K.dma(trif[:], tri_d)
        K.copy(trib[:], trif[:])
        K.memset(oneb[:], 1.0)
        K.dma(io384[:], io384_d)
        K.dma(io8[:], io8_d)
        K.dma(pnf[:], pn_d)
        K.copy(pnb[:], pnf[:])
        K.dma(g2col[:], col_view(gffn_d), allow_slow_non_contiguous=True)
        K.dma(g8[:], V(g8_d.ap.partition_broadcast(128), g8_d.res))
        K.dma(Mt[:], mg_d.rr("(n p) e -> p n e", p=128))
        K.dma(rt[:], rt_d.rr("(n p) k -> p n k", p=128))
        K.copy(Mb[:], Mt[:])
        K.memset(Rf[:, 0, :], 0.0)
        for n in range(1, NTT):
            K.tt(Rf[:, n, :], Rf[:, n - 1, :], Mt[:, n - 1, :], ALU.add)
        K.copy(Rb[:], Rf[:])
        sv = B[0][:].rr("p (n e) -> p n e", e=8)
        for n in range(NTT):
            K.mm(sv[:, n, :], trib[:], Mb[:, n, :], start=True, stop=False)
            K.mm(sv[:, n, :], oneb[:], Rb[:, n, :], start=False, stop=True)
        K.copy(slot[:], sv)
        io8_3 = io8[:].un(1).bc([128, NTT, 8])
        for k in range(2):
            K.ts(rel[:], rt[:, :, k], g8[:, 0:1], None, ALU.subtract)
            K.tt(oh[:], io8_3, rel[:].un(2).bc([128, NTT, 8]), ALU.is_equal)
            K.tt(oh[:], oh[:], slot[:], ALU.mult)
            K.reduce(sl[:], oh[:], ALU.add)
            K.stt(dls[:, :, k], rel[:], float(CE), sl[:], ALU.mult, ALU.add)
        K.dma(dl_d.rr("(n p) k -> p n k", p=128), dls[:])
        psi = [B[1 + s_][:, 0:16].rr("p (e k) -> p e k", k=2) for s_ in range(3)]
        oc = 0
        for e in range(8):
            for n in range(NTT):
                o_ = OHb[oc % 4]
                oc += 1
                K.ts(o_[:], io384[:], slot[:, n, e:e + 1], Mt[:, n, e:e + 1], ALU.is_equal, ALU.mult)
                for s_ in range(3):
                    K.mm(psi[s_][:, e, :], o_[:, s_ * 128:(s_ + 1) * 128], pnb[:, n, :], start=(n == 0), stop=(n == NTT - 1))
        for s_ in range(3):
            K.copy(idxf[:, s_, :, :], psi[s_])
        K.stt(idxv[:], idxf[:, :, :, 1], 128.0, idxf[:, :, :, 0], ALU.mult, ALU.add)
        K.copy(idxi[:], idxv[:])
        XeT = K.sb("XeT", [128, 32, CE], BF16)
        wgb = [K.sb("wgb%d" % i, [128, 32, 384], BF16) for i in range(2)]
        wub = [K.sb("wub%d" % i, [128, 32, 384], BF16) for i in range(2)]
        wdb = K.sb("wdb", [128, 3, D], BF16)
        xg = [K.sb("xg%d" % i, [128, D], BF16) for i in range(2)]
        ysb = K.sb("ysb", [128, D], F32)
        actT = K.sb("actT", [128, 3, CE], BF16)
        sil = [K.sb("sil%d" % i, [128, CE], F32) for i in range(2)]
        gc = 0
        for e in range(8):
            K.dma(wgb[e % 2][:], wg_d[e].rr("(c p) f -> p c f", p=128), eng="gpsimd")
            K.dma(wub[e % 2][:], wu_d[e].rr("(c p) f -> p c f", p=128), eng="gpsimd")
            for s_ in range(3):
                x_ = xg[gc % 2]
                gc += 1
                K.gather(x_[:], xn2_d, idxi[:, s_, e:e + 1])
                for k in range(4):
                    tp = tpb[k % 2]
                    for c in range(8):
                        cc = k * 8 + c
                        K.tr(tp[:, c, :], x_[:, cc * 128:(cc + 1) * 128], identb[:])
                    K.tt(XeT.sub(k, (slice(None), slice(k * 8, k * 8 + 8), slice(s_ * 128, (s_ + 1) * 128))),
                         tp[:], g2col[:, k * 8:k * 8 + 8].un(2).bc([128, 8, 128]), ALU.mult)
            K.dma(wdb[:], wd_d[e].rr("(c p) n -> p c n", p=128), eng="gpsimd")
            for f in range(3):
                pg, pu = B[f % 2], B[2 + f % 2]
                for c in range(32):
                    K.mm(pg[:, 0:CE], wgb[e % 2][:, c, f * 128:(f + 1) * 128],
                         XeT.sub(c // 8, (slice(None), c, slice(None))), start=(c == 0), stop=(c == 31))
                for c in range(32):
                    K.mm(pu[:, 0:CE], wub[e % 2][:, c, f * 128:(f + 1) * 128],
                         XeT.sub(c // 8, (slice(None), c, slice(None))), start=(c == 0), stop=(c == 31))
                K.act(sil[f % 2][:], pg[:, 0:CE], AF.Silu)
                K.tt(actT[:, f, :], sil[f % 2][:], pu[:, 0:CE], ALU.mult)
            for s_ in range(3):
                for j in range(8):
                    py = psy[j % 2]
                    for f in range(3):
                        K.mm(py[:], actT[:, f, s_ * 128:(s_ + 1) * 128], wdb[:, f, j * 512:(j + 1) * 512],
                             start=(f == 0), stop=(f == 2))
                    K.copy(ysb[:, j * 512:(j + 1) * 512], py[:], eng=("scalar" if j % 2 else "vector"))
                r0 = e * CE + s_ * 128
                K.dma(y_d[r0:r0 + 128, :], ysb[:])
        K.S.emit()
    return nc


def build_e(final):
    nc = bass.Bass("TRN2", target_bir_lowering=False)
    with contextlib.ExitStack() as st:
        K = KB(nc, st)
        I = lambda n, s, dt=F32: K.dram(n, s, dt, "ExternalInput")
        xm_d, y_d = I("xmid", [NT, D]), I("yall", [64 * CE, D])
        dl8_d, rt_d = I("destl8", [8, NT, 2]), I("rt", [NT, 4])
        lo8_d, base8_d, gfin_d = I("lo8", [128, 8]), I("base8", [128, 8]), I("gfin", [D])
        out_d = K.dram("xout", [NT, D], F32, "ExternalOutput")
        lo8 = K.sb("lo8s", [128, 8], F32)
        base8 = K.sb("base8s", [128, 8], F32)
        K.dma(lo8[:], lo8_d)
        K.dma(base8[:], base8_d)
        gb = K.sb("gb", [128, D], F32)
        if final:
            K.dma(gb[:], V(gfin_d.ap.partition_broadcast(128), gfin_d.res))
        xm = K.sb("xm", [128, D], F32)
        ya = K.sb("ya", [128, D], F32)
        yb = K.sb("yb", [128, D], F32)
        acc = K.sb("acc", [128, D], F32)
        junk = K.sb("junk", [128, D], BF16)
        rts = K.sb("rts", [128, 4], F32)
        dl8 = K.sb("dl8", [128, 8, 2], F32)
        g1 = K.sb("g1", [128, 8], F32)
        g2 = K.sb("g2", [128, 8], F32)
        tmp8 = K.sb("tmp8", [128, 8], F32)
        df = K.sb("df", [128, 2], F32)
        di = K.sb("di", [128, 2], I32)
        ss = K.sb("ss", [128, 1], F32)
        tmp = K.sb("tmp", [128, 1], F32)
        rs = K.sb("rs", [128, 1], F32)
        for n in range(NT // 128):
            r0 = n * 128
            K.dma(xm[:], xm_d[r0:r0 + 128, :])
            K.dma(rts[:], rt_d[r0:r0 + 128, :])
            K.dma(dl8[:], dl8_d[:, r0:r0 + 128, :].rr("g t k -> t g k"))
            K.ts(g1[:], lo8[:], rts[:, 0:1], None, ALU.is_le)
            K.ts(g2[:], lo8[:], 8.0, rts[:, 0:1], ALU.add, ALU.is_gt)
            K.tt(g1[:], g1[:], g2[:], ALU.mult)
            for k in range(2):
                K.tt(tmp8[:], dl8[:, :, k], base8[:], ALU.add)
                K.tt(tmp8[:], tmp8[:], g1[:], ALU.mult)
                K.reduce(df[:, k:k + 1], tmp8[:], ALU.add)
            K.copy(di[:], df[:])
            K.gather(ya[:], y_d, di[:, 0:1])
            K.gather(yb[:], y_d, di[:, 1:2])
            K.stt(acc[:], ya[:], rts[:, 2:3], xm[:], ALU.mult, ALU.add)
            K.stt(acc[:], yb[:], rts[:, 3:4], acc[:], ALU.mult, ALU.add)
            if final:
                K.act(junk[:], acc[:], AF.Square, accum=ss[:])
                K.rstd(rs[:], ss[:], float(D), tmp[:])
                K.stt(acc[:], acc[:], rs[:, 0:1], gb[:], ALU.mult, ALU.mult)
            K.dma(out_d[r0:r0 + 128, :], acc[:])
        K.S.emit()
    return nc


_PROGS = {}
_DBG = None
_DBG_STOP = False


def _prog(name, fn):
    if name not in _PROGS:
        _PROGS[name] = fn()
    return _PROGS[name]


def _run(name, fn, in_maps):
    res = run_bass_kernel_spmd(_prog(name, fn), in_maps, core_ids=list(range(8)))
    return res.results


CE2 = 1024
CG = 4096


def build_d2():
    nc = bass.Bass("TRN2", target_bir_lowering=False)
    NS = CE2 // 128
    with contextlib.ExitStack() as st:
        K = KB(nc, st)
        I = lambda n, s, dt=F32: K.dram(n, s, dt, "ExternalInput")
        mg_d, rt_d, g8_d = I("mg", [NTOK, 8]), I("rtall", [NTOK, 4]), I("g8", [1])
        xn2_d = I("xn2", [NTOK, D], BF16)
        gffn_d = I("gffn", [D])
        wg_d, wu_d, wd_d = I("wg", [8, D, 384]), I("wu", [8, D, 384]), I("wd", [8, 384, D])
        tri_d, tok_d = I("tri", [128, 128]), I("tokid", [128, NTT])
        ident_d, io8_d, lo8_d = I("ident", [128, 128]), I("iota8", [128, 8]), I("lo8", [128, 8])
        tr1_d, tr2_d = I("trash1", [128, 1]), I("trash2", [128, 1])
        y_d = K.dram("y", [8 * CE2, D], F32, "ExternalOutput")
        z_d = K.dram("z", [CG, D], F32, "ExternalOutput")
        inv_d = K.dram("inv", [128, NTT], I32, "ExternalOutput")
        info_d = K.dram("info", [NTOK, 4], F32, "ExternalOutput")
        idx_d = K.dram("idxl", [8 * CE2 + 128, 2], I32, "ExternalOutput")
        tl_d = K.dram("tokl", [CG + 128, 2], I32, "ExternalOutput")

        identf = K.sb("identf", [128, 128], F32)
        identb = K.sb("identb", [128, 128], BF16)
        trif = K.sb("trif", [128, 128], F32)
        trib = K.sb("trib", [128, 128], BF16)
        oneb = K.sb("oneb", [128, 128], BF16)
        io8 = K.sb("io8", [128, 8], F32)
        lo8 = K.sb("lo8s", [128, 8], F32)
        tokf = K.sb("tokf", [128, NTT], F32)
        toki = K.sb("toki", [128, NTT, 2], I32)
        g2col = K.sb("g2col", [128, 32], F32)
        g8 = K.sb("g8s", [128, 1], F32)
        tr1 = K.sb("tr1", [128, 1], F32)
        tr2 = K.sb("tr2", [128, 1], F32)
        Mt = K.sb("Mt", [128, NTT, 8], F32)
        Mb = K.sb("Mb", [128, NTT, 8], BF16)
        Rf = K.sb("Rf", [128, NTT, 8], F32)
        Rb = K.sb("Rb", [128, NTT, 8], BF16)
        slot = K.sb("slot", [128, NTT, 8], F32)
        G8 = K.sb("G8", [128, NTT, 8], F32)
        posa = K.sb("posa", [128, NTT, 8], F32)
        oh = K.sb("ohd", [128, NTT, 8], F32)
        oh2 = K.sb("ohd2", [128, NTT, 8], F32)
        rt = K.sb("rt", [128, NTT, 4], F32)
        rel = K.sb("rel", [128, NTT], F32)
        sl = K.sb("sl", [128, NTT], F32)
        okk = K.sb("okk", [128, NTT], F32)
        ok2 = K.sb("ok2", [128, NTT], F32)
        ing = K.sb("ing", [128, NTT], F32)
        dtmp = K.sb("dtmp", [128, NTT], F32)
        info = K.sb("infos", [128, NTT, 4], F32)
        di = [K.sb("di%d" % k, [128, NTT], I32) for k in range(3)]
        invf = K.sb("invf", [128, NTT], F32)
        invi = K.sb("invi", [128, NTT], I32)
        zt = K.sb("zt", [128, (8 * CE2 + 128) // 128, 2], I32)
        pgp = K.ps("pgp", [128, CE2], F32)
        pup = K.ps("pup", [128, CE2], F32)
        tpb = [K.ps("tpb%d" % i, [128, 8, 128], BF16) for i in range(2)]
        psy = [K.ps("psy%d" % i, [128, 512], F32) for i in range(2)]

        K.dma(identf[:], ident_d)
        K.copy(identb[:], identf[:])
        K.dma(trif[:], tri_d)
        K.copy(trib[:], trif[:])
        K.memset(oneb[:], 1.0)
        K.dma(io8[:], io8_d)
        K.dma(lo8[:], lo8_d)
        K.dma(tokf[:], tok_d)
        K.copy(toki[:, :, 0], tokf[:])
        K.copy(toki[:, :, 1], tokf[:])
        K.dma(tr1[:], tr1_d)
        K.dma(tr2[:], tr2_d)
        K.dma(g2col[:], col_view(gffn_d), allow_slow_non_contiguous=True)
        K.dma(g8[:], V(g8_d.ap.partition_broadcast(128), g8_d.res))
        K.dma(Mt[:], mg_d.rr("(n p) e -> p n e", p=128))
        K.dma(rt[:], rt_d.rr("(n p) k -> p n k", p=128))
        K.memset(zt[:], 0)
        idx_rs = [Res("idxr%d" % i) for i in range(6)]
        tl_rs = [Res("tlr%d" % i) for i in range(3)]
        K.dma(idx_d.rr("(s p) o -> p s o", p=128), zt[:], xw=idx_rs)
        K.dma(tl_d.rr("(s p) o -> p s o", p=128), zt[:, 0:(CG + 128) // 128, :], xw=tl_rs)

        def excl_cumsum(dst, src_f, pv):
            K.copy(Mb[:], src_f)
            K.memset(Rf[:, 0, :], 0.0)
            for n in range(1, NTT):
                K.tt(Rf[:, n, :], Rf[:, n - 1, :], src_f[:, n - 1, :], ALU.add)
            K.copy(Rb[:], Rf[:])
            for n in range(NTT):
                K.mm(pv[:, n, :], trib[:], Mb[:, n, :], start=True, stop=False)
                K.mm(pv[:, n, :], oneb[:], Rb[:, n, :], start=False, stop=True)
            K.copy(dst, pv)
        sv = psy[0][:].rr("p (n e) -> p n e", e=8)
        excl_cumsum(slot[:], Mt[:], sv)
        io8_3 = io8[:].un(1).bc([128, NTT, 8])
        lo8_3 = lo8[:].un(1).bc([128, NTT, 8])
        ea3 = rt[:, :, 0].un(2).bc([128, NTT, 8])
        K.tt(G8[:], lo8_3, ea3, ALU.is_le)
        K.ts(oh[:], lo8_3, 8.0, None, ALU.add)
        K.tt(oh[:], oh[:], ea3, ALU.is_gt)
        K.tt(G8[:], G8[:], oh[:], ALU.mult)
        sv2 = psy[1][:].rr("p (n e) -> p n e", e=8)
        excl_cumsum(posa[:], G8[:], sv2)
        K.tt(oh[:], G8[:], posa[:], ALU.mult)
        K.reduce(sl[:], oh[:], ALU.add)
        K.tt(oh[:], G8[:], io8_3, ALU.mult)
        K.reduce(dtmp[:], oh[:], ALU.add)
        K.stt(invf[:], dtmp[:], float(CG), sl[:], ALU.mult, ALU.add)
        K.copy(invi[:], invf[:])
        K.dma(inv_d, invi[:])
        K.ts(rel[:], rt[:, :, 0], g8[:, 0:1], None, ALU.subtract)
        K.ts(ing[:], rel[:], 0.0, None, ALU.is_ge)
        K.ts(ok2[:], rel[:], 8.0, None, ALU.is_lt)
        K.tt(ing[:], ing[:], ok2[:], ALU.mult)
        K.ts(ok2[:], sl[:], float(CG), None, ALU.is_lt)
        K.tt(ok2[:], ok2[:], ing[:], ALU.mult)
        K.ts(dtmp[:], sl[:], tr2[:, 0:1], None, ALU.subtract)
        K.tt(dtmp[:], dtmp[:], ok2[:], ALU.mult)
        K.ts(dtmp[:], dtmp[:], tr2[:, 0:1], None, ALU.add)
        K.copy(di[2][:], dtmp[:])
        for k in range(2):
            K.ts(rel[:], rt[:, :, k], g8[:, 0:1], None, ALU.subtract)
            K.tt(oh2[:], io8_3, rel[:].un(2).bc([128, NTT, 8]), ALU.is_equal)
            K.tt(oh2[:], oh2[:], slot[:], ALU.mult)
            K.reduce(sl[:], oh2[:], ALU.add)
            K.stt(info[:, :, k], rel[:], float(CE2), sl[:], ALU.mult, ALU.add)
            K.ts(okk[:], sl[:], float(CE2), None, ALU.is_lt)
            K.tt(info[:, :, 2 + k], rt[:, :, 2 + k], okk[:], ALU.mult)
            K.tt(okk[:], okk[:], ing[:], ALU.mult)
            K.ts(dtmp[:], info[:, :, k], tr1[:, 0:1], None, ALU.subtract)
            K.tt(dtmp[:], dtmp[:], okk[:], ALU.mult)
            K.ts(dtmp[:], dtmp[:], tr1[:, 0:1], None, ALU.add)
            K.copy(di[k][:], dtmp[:])
        K.dma(info_d.rr("(n p) k -> p n k", p=128), info[:])
        for n in range(NTT):
            for k in range(2):
                K.scatter(V(idx_d.ap, idx_rs[(2 * n + k) % 6]), di[k][:, n:n + 1], toki[:, n, :])
            K.scatter(V(tl_d.ap, tl_rs[n % 3]), di[2][:, n:n + 1], toki[:, n, :])
        XeT = K.sb("XeT", [128, 32, CE2], BF16)
        wgb = K.sb("wgb", [128, 32, 384], BF16)
        wub = K.sb("wub", [128, 32, 384], BF16)
        wdb = K.sb("wdb", [128, 3, D], BF16)
        xg = [K.sb("xg%d" % i, [128, D], BF16) for i in range(2)]
        ysb = K.sb("ysb", [128, D], F32)
        actT = K.sb("actT", [128, 3, CE2], BF16)
        sil = K.sb("sil", [128, CE2], F32)
        idxt = K.sb("idxt", [128, NS, 2], I32)
        gc = 0
        for e in range(8):
            K.dma(wgb[:], wg_d[e].rr("(c p) f -> p c f", p=128), eng="gpsimd")
            K.dma(wub[:], wu_d[e].rr("(c p) f -> p c f", p=128), eng="gpsimd")
            K.dma(idxt[:], idx_d[e * CE2:(e + 1) * CE2, :].rr("(s p) o -> p s o", p=128), xr=idx_rs)
            for s_ in range(NS):
                x_ = xg[gc % 2]
                gc += 1
                K.gather(x_[:], xn2_d, idxt[:, s_, 0:1])
                for k in range(4):
                    tp = tpb[k % 2]
                    for c in range(8):
                        cc = k * 8 + c
                        K.tr(tp[:, c, :], x_[:, cc * 128:(cc + 1) * 128], identb[:])
                    K.tt(XeT.sub(k, (slice(None), slice(k * 8, k * 8 + 8), slice(s_ * 128, (s_ + 1) * 128))),
                         tp[:], g2col[:, k * 8:k * 8 + 8].un(2).bc([128, 8, 128]), ALU.mult)
            K.dma(wdb[:], wd_d[e].rr("(c p) n -> p c n", p=128), eng="gpsimd")
            for f in range(3):
                for (wb_, pp) in ((wgb, pgp), (wub, pup)):
                    for hh in range(CE2 // 512):
                        for c in range(32):
                            K.mm(pp.sub(hh, (slice(None), slice(hh * 512, (hh + 1) * 512))), wb_[:, c, f * 128:(f + 1) * 128],
                                 XeT.sub(c // 8, (slice(None), c, slice(hh * 512, (hh + 1) * 512))), start=(c == 0), stop=(c == 31))
                for hh in range(CE2 // 512):
                    hs = slice(hh * 512, (hh + 1) * 512)
                    K.act(sil[:, hs], pgp.sub(hh, (slice(None), hs)), AF.Silu)
                    K.tt(actT[:, f, hs], sil[:, hs], pup.sub(hh, (slice(None), hs)), ALU.mult)
            for s_ in range(NS):
                for j in range(8):
                    py = psy[j % 2]
                    for f in range(3):
                        K.mm(py[:], actT[:, f, s_ * 128:(s_ + 1) * 128], wdb[:, f, j * 512:(j + 1) * 512],
                             start=(f == 0), stop=(f == 2))
                    K.copy(ysb[:, j * 512:(j + 1) * 512], py[:], eng=("scalar" if j % 2 else "vector"))
                r0 = e * CE2 + s_ * 128
                K.dma(y_d[r0:r0 + 128, :], ysb[:])
        tlt = K.sb("tlt", [128, CG // 128, 2], I32)
        inf = [K.sb("inf%d" % i, [128, 4], F32) for i in range(2)]
        ii = [K.sb("ii%d" % i, [128, 2], I32) for i in range(2)]
        cb = [V(XeT.h[:, 8 * k:8 * k + 8, :].rearrange("p c t -> p (c t)").bitcast(F32), XeT.sub(k).res) for k in range(4)]
        K.dma(tlt[:], tl_d[0:CG, :].rr("(s p) o -> p s o", p=128), xr=tl_rs)
        for j in range(CG // 128):
            f_ = inf[j % 2]
            i_ = ii[j % 2]
            ya, yb = cb[(2 * j) % 4], cb[(2 * j + 1) % 4]
            K.gather(f_[:], info_d, tlt[:, j, 0:1])
            K.ts(f_[:, 0:2], f_[:, 0:2], 0.0, float(8 * CE2 - 1), ALU.max, ALU.min)
            K.copy(i_[:], f_[:, 0:2])
            K.gather(ya[:], y_d, i_[:, 0:1])
            K.gather(yb[:], y_d, i_[:, 1:2])
            K.ts(ya[:], ya[:], f_[:, 2:3], None, ALU.mult)
            K.stt(ya[:], yb[:], f_[:, 3:4], ya[:], ALU.mult, ALU.add)
            K.dma(z_d[j * 128:(j + 1) * 128, :], ya[:])
        K.S.emit()
    return nc


def build_f():
    nc = bass.Bass("TRN2", target_bir_lowering=False)
    with contextlib.ExitStack() as st:
        K = KB(nc, st)
        x_d = K.dram("x", [NT, D], F32, "ExternalInput")
        xa_d = K.dram("xadd", [NT, D], F32, "ExternalInput")
        g_d = K.dram("g", [D], F32, "ExternalInput")
        o_d = K.dram("xout", [NT, D], F32, "ExternalOutput")
        gb = K.sb("gb", [128, D], F32)
        K.dma(gb[:], V(g_d.ap.partition_broadcast(128), g_d.res))
        xa = [K.sb("xa%d" % i, [128, D], F32) for i in range(2)]
        xb = [K.sb("xb%d" % i, [128, D], F32) for i in range(2)]
        junk = K.sb("junk", [128, D], BF16)
        ss = [K.sb("ss%d" % i, [128, 1], F32) for i in range(2)]
        tmp = [K.sb("tmp%d" % i, [128, 1], F32) for i in range(2)]
        rs = [K.sb("rs%d" % i, [128, 1], F32) for i in range(2)]
        for n in range(NT // 128):
            a, b = xa[n % 2], xb[n % 2]
            K.dma(a[:], x_d[n * 128:(n + 1) * 128, :])
            K.dma(b[:], xa_d[n * 128:(n + 1) * 128, :])
            K.tt(a[:], a[:], b[:], ALU.add)
            K.act(junk[:], a[:], AF.Square, accum=ss[n % 2][:])
            K.rstd(rs[n % 2][:], ss[n % 2][:], float(D), tmp[n % 2][:])
            K.stt(b[:], a[:], rs[n % 2][:, 0:1], gb[:], ALU.mult, ALU.mult)
            K.dma(o_d[n * 128:(n + 1) * 128, :], b[:])
        K.S.emit()
    return nc


def kernel(**P):
    P = {k: np.asarray(v) for k, v in P.items()}
    xmid = _c(P["x"]).reshape(NTOK, D)
    moe = np.zeros((NTOK, D), np.float32)
    ident = np.eye(128, dtype=np.float32)
    i128 = np.arange(128)
    iota64 = _c(np.tile(np.arange(64)[None, :], (128, 1)))
    tri = _c(np.triu(np.ones((128, 128)), 1))
    iota8 = _c(np.tile(np.arange(8)[None, :], (128, 1)))
    lo8 = _c(iota8 * 8.0)
    tokid = _c(np.arange(NTT)[None, :] * 128 + i128[:, None])
    trash1 = _c((8 * CE2 + i128)[:, None])
    trash2 = _c((CG + i128)[:, None])
    pos = np.arange(S, dtype=np.float32)
    inv = (10000.0 ** (-np.arange(0, 64, 2, dtype=np.float32) / 64)).astype(np.float32)
    ang = pos[:, None] * inv[None, :]
    cosT, sinT = np.cos(ang).astype(np.float32), np.sin(ang).astype(np.float32)
    bconst = b_const_inputs()
    sh = lambda a, c: a[c * NT:(c + 1) * NT]
    for l in range(2):
        r = _run("a", build_a, [dict(x=_c(sh(xmid, c)), xadd=_c(sh(moe, c)), g=_c(P["norm_mix_g"][l]), w=_c(P["w_in"][l]),
                                     ident=ident, gq=_c(P["mla_q_norm_g"][l]), gkv=_c(P["mla_kv_norm_g"][l]),
                                     wuq=_c(P["mla_w_uq"][l]), wukv=_c(P["mla_w_ukv"][l]),
                                     cos=_c(cosT[(c % 4) * NT:(c % 4 + 1) * NT]), sin=_c(sinT[(c % 4) * NT:(c % 4 + 1) * NT]))
                                for c in range(8)])
        proj = np.concatenate([r[c]["proj"] for c in range(8)], axis=0)
        x = np.concatenate([r[c]["xsum"] for c in range(8)], axis=0)
        qm = np.concatenate([r[c]["q"] for c in range(8)], axis=0)
        kvm = np.concatenate([r[c]["kv"] for c in range(8)], axis=0)
        krm = np.concatenate([r[c]["kro"] for c in range(8)], axis=0)
        maps = []
        for c in range(8):
            b, hq = c // 4, c % 4
            bs = slice(b * S, (b + 1) * S)
            d = dict(bconst)
            d.update(b_core_inputs(proj[bs], qm[bs], kvm[bs], krm[bs], P, l, hq))
            maps.append(d)
        r = _run("b", build_b, maps)
        o_all = np.empty((NTOK, D), np.float32)
        for c in range(8):
            b, hq = c // 4, c % 4
            o = r[c]["out"]
            for gi in range(4):
                o_all[b * S:(b + 1) * S, gi * 1024 + hq * 256:gi * 1024 + (hq + 1) * 256] = o[:, gi * 256:(gi + 1) * 256]
        rw = _c(np.concatenate([P["router_group_w"][l], P["router_expert_w"][l]], axis=1))
        rb = _c(np.concatenate([P["router_group_b"][l], P["router_expert_b"][l]]))
        r = _run("c", build_c, [dict(o=_c(sh(o_all, c)), x=_c(sh(x, c)), gout=_c(P["out_norm_g"][l]), wout=_c(P["w_out"][l]),
                                     gffn=_c(P["norm_ffn_g"][l]), rw=rw, rb=rb, ident=ident, iota64=iota64) for c in range(8)])
        xmid = np.concatenate([r[c]["xmid"] for c in range(8)], axis=0)
        xn2 = np.concatenate([r[c]["xn2"] for c in range(8)], axis=0)
        mall = np.concatenate([r[c]["mroute"] for c in range(8)], axis=0)
        rtall = np.concatenate([r[c]["route"] for c in range(8)], axis=0)
        r = _run("d", build_d2, [dict(mg=_c(mall[:, 8 * g:8 * g + 8]), rtall=_c(rtall), g8=np.array([8.0 * g], np.float32),
                                      xn2=np.ascontiguousarray(xn2), gffn=_c(P["norm_ffn_g"][l]),
                                      wg=_c(P["exp_w_gate"][l][8 * g:8 * g + 8]), wu=_c(P["exp_w_up"][l][8 * g:8 * g + 8]),
                                      wd=_c(P["exp_w_down"][l][8 * g:8 * g + 8]), tri=tri, tokid=tokid, ident=ident,
                                      iota8=iota8, lo8=lo8, trash1=trash1, trash2=trash2) for g in range(8)])
        zall = np.concatenate([r[g]["z"] for g in range(8)], axis=0)
        moe = np.take(zall, r[0]["inv"].T.reshape(-1), axis=0, mode="clip")
        if _DBG is not None:
            _DBG.append(dict(proj=proj, o_all=o_all, xmid=xmid, moe=moe, rtall=rtall, mall=mall))
            if _DBG_STOP:
                return xmid + moe
    r = _run("f", build_f, [dict(x=_c(sh(xmid, c)), xadd=_c(sh(moe, c)), g=_c(P["final_norm_g"])) for c in range(8)])
    out = np.concatenate([r[c]["xout"] for c in range(8)], axis=0)
    return out.reshape(2, S, D).astype(np.float32)
```
